# Optimizing a Trainium2 kernel written in Bass

```python
import math
import jax, jax.numpy as jnp
from jax import lax
import numpy as np

D_MODEL = 1024
BATCH = 8
SEQ = 4096
DEPTH = 2

CTX_LEN = 256
GRID_W = 64
N_BRANCH = 4
N_HEADS = 4
HEAD_DIM = 64
CONV_CH = 256
CONV_WIDTH = 31
Q_LORA = 192
KV_LORA = 128
QK_NOPE = 64
QK_ROPE = 32
V_HEAD = 64
DIFF_DIM = 32
DIFF_V = 2 * DIFF_DIM
NA_KH = 8
NA_KW = 16
NA_QB = 16
ROPE_DIM = 32
ROPE_BASE = 10000.0
QBLOCK = 128
N_EXPERTS = 32
TOP_K = 4
D_FF = 1024
SWIGLU_LIMIT = 7.0
SWIGLU_ALPHA = 1.702
EPS = 1e-6
NEG_INF = -1e30

A_IN = 2 * CONV_CH
B_IN = Q_LORA + KV_LORA + QK_ROPE
C_IN = N_HEADS * (4 * DIFF_DIM + DIFF_V)
D_IN = N_HEADS * 3 * HEAD_DIM
OFF_B = A_IN
OFF_C = OFF_B + B_IN
OFF_D = OFF_C + C_IN
IN_WIDTH = OFF_D + D_IN

kernel_name = 'hybrid_gated_mixer_moe_dit'

F32 = jnp.float32


def rmsnorm(x, g):
    xf = x.astype(F32)
    y = xf * lax.rsqrt(jnp.mean(xf * xf, axis=-1, keepdims=True) + EPS)
    return (y * g.astype(F32)).astype(x.dtype)


def layernorm(x, g, b):
    xf = x.astype(F32)
    mu = jnp.mean(xf, axis=-1, keepdims=True)
    var = jnp.mean(jnp.square(xf - mu), axis=-1, keepdims=True)
    y = (xf - mu) * lax.rsqrt(var + EPS)
    return (y * g.astype(F32) + b.astype(F32)).astype(x.dtype)


def rope(x, cos, sin):
    half = x.shape[-1] // 2
    x1, x2 = x[..., :half], x[..., half:]
    return jnp.concatenate([x1 * cos - x2 * sin, x2 * cos + x1 * sin], axis=-1)


def axial_rope_tables(n_tokens, dtype):
    t = jnp.arange(n_tokens, dtype=jnp.int32)
    rows = (t // GRID_W).astype(F32)
    cols = (t % GRID_W).astype(F32)
    axis_dim = ROPE_DIM // 2
    inv = ROPE_BASE ** (-jnp.arange(0, axis_dim, 2, dtype=F32) / axis_dim)
    theta = jnp.concatenate([rows[:, None] * inv, cols[:, None] * inv], axis=-1)
    return jnp.cos(theta).astype(dtype), jnp.sin(theta).astype(dtype)


def merge_heads(o):
    b, h, n, d = o.shape
    return o.transpose(0, 2, 1, 3).reshape(b, n, h * d)


def sweep_query_blocks(fn, q):
    b, h, n = q.shape[:3]
    nb = n // QBLOCK
    qb = jnp.moveaxis(q.reshape(b, h, nb, QBLOCK, *q.shape[3:]), 2, 0)
    out = lax.map(fn, qb)
    return jnp.moveaxis(out, 0, 2).reshape(b, h, n, out.shape[-1])


def softmax_attend(q, k, v):
    s = jnp.einsum('bhqd,bhkd->bhqk', q, k).astype(F32) * (q.shape[-1] ** -0.5)
    p = jax.nn.softmax(s, axis=-1).astype(v.dtype)
    return jnp.einsum('bhqk,bhkd->bhqd', p, v)


def dense_attention(q, k, v):
    return sweep_query_blocks(lambda qb: softmax_attend(qb, k, v), q)


def diff_attend(q, k, v, lam):
    s = jnp.einsum('bhqcd,bhkcd->bhcqk', q, k).astype(F32) * (q.shape[-1] ** -0.5)
    p = jax.nn.softmax(s, axis=-1)
    w = (p[:, :, 0] - lam * p[:, :, 1]).astype(v.dtype)
    return jnp.einsum('bhqk,bhkd->bhqd', w, v)


def conformer_branch(pa, p):
    u = pa[..., :CONV_CH] * jax.nn.sigmoid(pa[..., CONV_CH:])
    u = lax.conv_general_dilated(
        u, p['conv_w'][:, None, :], window_strides=(1,),
        padding=[(CONV_WIDTH // 2, CONV_WIDTH // 2)],
        dimension_numbers=('NWC', 'WIO', 'NWC'), feature_group_count=CONV_CH) + p['conv_b']
    u = jax.nn.silu(layernorm(u, p['conv_ln_g'], p['conv_ln_b']))
    return u @ p['conv_out']


def mla_qkv(pb, p, cos, sin):
    b, n = pb.shape[:2]
    c_q = rmsnorm(pb[..., :Q_LORA], p['mla_cq_g'])
    c_kv = rmsnorm(pb[..., Q_LORA:Q_LORA + KV_LORA], p['mla_ckv_g'])
    k_pe = pb[..., Q_LORA + KV_LORA:]
    q = (c_q @ p['mla_w_uq']).reshape(b, n, N_HEADS, QK_NOPE + QK_ROPE)
    kv = (c_kv @ p['mla_w_ukv']).reshape(b, n, N_HEADS, QK_NOPE + V_HEAD)
    k = jnp.concatenate(
        [kv[..., :QK_NOPE], jnp.broadcast_to(k_pe[:, :, None, :], (b, n, N_HEADS, QK_ROPE))], axis=-1)
    q = rmsnorm(q, p['mla_qn_g']).transpose(0, 2, 1, 3)
    k = rmsnorm(k, p['mla_kn_g']).transpose(0, 2, 1, 3)
    v = kv[..., QK_NOPE:].transpose(0, 2, 1, 3)
    if cos is not None:
        q = jnp.concatenate([q[..., :QK_NOPE], rope(q[..., QK_NOPE:], cos, sin)], axis=-1)
        k = jnp.concatenate([k[..., :QK_NOPE], rope(k[..., QK_NOPE:], cos, sin)], axis=-1)
    return q, k, v


def diff_qkv(pc, p, cos, sin):
    b, n = pc.shape[:2]
    t = pc.reshape(b, n, N_HEADS, 4 * DIFF_DIM + DIFF_V)
    q = t[..., :2 * DIFF_DIM].reshape(b, n, N_HEADS, 2, DIFF_DIM)
    k = t[..., 2 * DIFF_DIM:4 * DIFF_DIM].reshape(b, n, N_HEADS, 2, DIFF_DIM)
    v = t[..., 4 * DIFF_DIM:].transpose(0, 2, 1, 3)
    q = rmsnorm(q, p['diff_qn_g']).transpose(0, 2, 1, 3, 4)
    k = rmsnorm(k, p['diff_kn_g']).transpose(0, 2, 1, 3, 4)
    if cos is not None:
        q = rope(q, cos[:, None], sin[:, None])
        k = rope(k, cos[:, None], sin[:, None])
    return q, k, v


def diff_project(o, p, lam_init):
    o = rmsnorm(o, p['diff_subln_g']) * (1.0 - lam_init)
    return merge_heads(o) @ p['diff_out']


def na_qkv(pd, p):
    b, n = pd.shape[:2]
    t = pd.reshape(b, n, N_HEADS, 3 * HEAD_DIM)
    q = rmsnorm(t[..., :HEAD_DIM], p['na_qn_g']).transpose(0, 2, 1, 3)
    k = rmsnorm(t[..., HEAD_DIM:2 * HEAD_DIM], p['na_kn_g']).transpose(0, 2, 1, 3)
    v = t[..., 2 * HEAD_DIM:].transpose(0, 2, 1, 3)
    return q, k, v


def na_index_tables(rows):
    kh = min(NA_KH, rows)
    ncb = GRID_W // NA_QB
    band_w = NA_QB + NA_KW
    r = np.arange(rows)
    row_start = np.clip(r - kh // 2, 0, rows - kh)
    key_rows = row_start[:, None] + np.arange(kh)
    row_off = key_rows - r[:, None] + NA_KH - 1
    cb = np.arange(ncb)
    col_start = np.clip(cb * NA_QB - NA_KW // 2, 0, GRID_W - band_w)
    key_cols = col_start[:, None] + np.arange(band_w)
    q_cols = cb[:, None] * NA_QB + np.arange(NA_QB)
    win_start = np.clip(q_cols - NA_KW // 2, 0, GRID_W - NA_KW)
    d = key_cols[:, None, :] - win_start[:, :, None]
    col_valid = (d >= 0) & (d < NA_KW)
    col_off = np.clip(key_cols[:, None, :] - q_cols[:, :, None] + NA_KW - 1, 0, 2 * NA_KW - 2)
    return key_rows.astype(np.int32), row_off, key_cols, col_off, col_valid


def neighbourhood_attention(q, k, v, kc, vc, rpb):
    b, h, n, d = q.shape
    rows = n // GRID_W
    key_rows, row_off, key_cols, col_off, col_valid = na_index_tables(rows)
    kh = key_rows.shape[1]
    ncb, band_w = key_cols.shape
    bias = rpb.astype(F32)[:, row_off[:, None, None, :, None], col_off[None, :, :, None, :]]
    bias = jnp.where(col_valid[None, None, :, :, None, :], bias, NEG_INF)
    bias = jnp.moveaxis(bias.reshape(h, rows, ncb, NA_QB, kh * band_w), 1, 0)
    scale = d ** -0.5
    kg = k.reshape(b, h, rows, GRID_W, d)
    vg = v.reshape(b, h, rows, GRID_W, v.shape[-1])
    qr = jnp.moveaxis(q.reshape(b, h, rows, ncb, NA_QB, d), 2, 0)
    n_ctx = kc.shape[2]

    def gather_band(t, rows_r):
        t = jnp.take(t, rows_r, axis=2)[:, :, :, key_cols]
        return t.transpose(0, 1, 3, 2, 4, 5).reshape(b, h, ncb, kh * band_w, t.shape[-1])

    def row_block(args):
        q_r, rows_r, bias_r = args
        k_band = gather_band(kg, rows_r)
        v_band = gather_band(vg, rows_r)
        s_loc = jnp.einsum('bhnqd,bhnkd->bhnqk', q_r, k_band).astype(F32) * scale + bias_r
        s_ctx = jnp.einsum('bhnqd,bhcd->bhnqc', q_r, kc).astype(F32) * scale
        pr = jax.nn.softmax(jnp.concatenate([s_ctx, s_loc], axis=-1), axis=-1).astype(v.dtype)
        return (jnp.einsum('bhnqc,bhcd->bhnqd', pr[..., :n_ctx], vc)
                + jnp.einsum('bhnqk,bhnkd->bhnqd', pr[..., n_ctx:], v_band))

    out = lax.map(row_block, (qr, jnp.asarray(key_rows), bias))
    return jnp.moveaxis(out, 0, 2).reshape(b, h, n, out.shape[-1])


def gated_merge(h, y_conv, y_mla, y_diff, y_na, p):
    g = jax.nn.sigmoid((h @ p['gate_w'] + p['gate_b']).astype(F32)).astype(h.dtype)
    g = g.reshape(*h.shape[:-1], N_BRANCH, D_MODEL)
    y = (g[..., 0, :] * y_conv + g[..., 1, :] * y_mla
         + g[..., 2, :] * y_diff + g[..., 3, :] * y_na)
    return y @ p['w_o']


def token_mixer(h, hc, p, lam_init, cos, sin, with_ctx_out):
    proj = h @ p['w_in']
    projc = hc @ p['w_in']
    lam_vec = p['diff_lam'].astype(F32)
    lam = jnp.exp(jnp.sum(lam_vec[0] * lam_vec[1])) - jnp.exp(jnp.sum(lam_vec[2] * lam_vec[3])) + lam_init

    y_conv = conformer_branch(proj[..., :OFF_B], p)

    q_b, k_b, v_b = mla_qkv(proj[..., OFF_B:OFF_C], p, cos, sin)
    qc_b, kc_b, vc_b = mla_qkv(projc[..., OFF_B:OFF_C], p, None, None)
    y_mla = merge_heads(dense_attention(
        q_b, jnp.concatenate([kc_b, k_b], axis=2), jnp.concatenate([vc_b, v_b], axis=2))) @ p['mla_out']

    q_c, k_c, v_c = diff_qkv(proj[..., OFF_C:OFF_D], p, cos, sin)
    qc_c, kc_c, vc_c = diff_qkv(projc[..., OFF_C:OFF_D], p, None, None)
    k_all = jnp.concatenate([kc_c, k_c], axis=2)
    v_all = jnp.concatenate([vc_c, v_c], axis=2)
    y_diff = diff_project(sweep_query_blocks(lambda qb: diff_attend(qb, k_all, v_all, lam), q_c), p, lam_init)

    q_d, k_d, v_d = na_qkv(proj[..., OFF_D:], p)
    qc_d, kc_d, vc_d = na_qkv(projc[..., OFF_D:], p)
    y_na = merge_heads(neighbourhood_attention(q_d, k_d, v_d, kc_d, vc_d, p['na_rpb'])) @ p['na_out']

    y = gated_merge(h, y_conv, y_mla, y_diff, y_na, p)
    if not with_ctx_out:
        return y, None
    yc_conv = conformer_branch(projc[..., :OFF_B], p)
    yc_mla = merge_heads(dense_attention(qc_b, kc_b, vc_b)) @ p['mla_out']
    yc_diff = diff_project(sweep_query_blocks(lambda qb: diff_attend(qb, kc_c, vc_c, lam), qc_c), p, lam_init)
    yc_na = merge_heads(dense_attention(qc_d, kc_d, vc_d)) @ p['na_out']
    return y, gated_merge(hc, yc_conv, yc_mla, yc_diff, yc_na, p)


def moe_ffn(h, p):
    shp = h.shape
    t = h.reshape(-1, shp[-1])
    logits = (t @ p['router_w'] + p['router_b']).astype(F32)
    top_val, top_idx = lax.top_k(logits, TOP_K)
    w = jax.nn.softmax(top_val, axis=-1)
    gates = jnp.einsum('nk,nke->ne', w, jax.nn.one_hot(top_idx, N_EXPERTS, dtype=F32)).astype(h.dtype)
    out = jnp.zeros_like(t)
    for e in range(N_EXPERTS):
        gu = t @ p['exp_w_gu'][e] + p['exp_b_gu'][e]
        g = jnp.minimum(gu[:, :D_FF], SWIGLU_LIMIT)
        u = jnp.clip(gu[:, D_FF:], -SWIGLU_LIMIT, SWIGLU_LIMIT)
        y = ((u + 1.0) * (g * jax.nn.sigmoid(SWIGLU_ALPHA * g))) @ p['exp_w_down'][e] + p['exp_b_down'][e]
        out = out + gates[:, e:e + 1] * y
    return out.reshape(shp)


def modulate(x, g, shift, scale):
    return rmsnorm(x, g) * (1.0 + scale) + shift


def setup_inputs(seed: int = 0) -> dict:
    key = jax.random.key(seed)
    ks = iter(jax.random.split(key, 64))
    L, D = DEPTH, D_MODEL

    def nrm(shape, scale):
        return jax.random.normal(next(ks), shape, F32) * scale

    def gain(shape):
        return 1.0 + nrm(shape, 0.02)

    return {
        'x': nrm((BATCH, SEQ, D), 1.0),
        'c': nrm((BATCH, D), 1.0),
        'ctx': nrm((BATCH, CTX_LEN, D), 1.0),
        'c_ctx': nrm((D,), 1.0),
        'ada_w': nrm((L, D, 6 * D), 0.5 * D ** -0.5),
        'ada_b': nrm((L, 6 * D), 0.02),
        'norm1_g': gain((L, D)),
        'norm2_g': gain((L, D)),
        'w_in': nrm((L, D, IN_WIDTH), D ** -0.5),
        'conv_w': nrm((L, CONV_WIDTH, CONV_CH), CONV_WIDTH ** -0.5),
        'conv_b': nrm((L, CONV_CH), 0.02),
        'conv_ln_g': gain((L, CONV_CH)),
        'conv_ln_b': nrm((L, CONV_CH), 0.02),
        'conv_out': nrm((L, CONV_CH, D), CONV_CH ** -0.5),
        'mla_cq_g': gain((L, Q_LORA)),
        'mla_ckv_g': gain((L, KV_LORA)),
        'mla_w_uq': nrm((L, Q_LORA, N_HEADS * (QK_NOPE + QK_ROPE)), Q_LORA ** -0.5),
        'mla_w_ukv': nrm((L, KV_LORA, N_HEADS * (QK_NOPE + V_HEAD)), KV_LORA ** -0.5),
        'mla_qn_g': gain((L, QK_NOPE + QK_ROPE)),
        'mla_kn_g': gain((L, QK_NOPE + QK_ROPE)),
        'mla_out': nrm((L, N_HEADS * V_HEAD, D), (N_HEADS * V_HEAD) ** -0.5),
        'diff_qn_g': gain((L, DIFF_DIM)),
        'diff_kn_g': gain((L, DIFF_DIM)),
        'diff_lam': nrm((L, 4, DIFF_DIM), 0.1),
        'diff_subln_g': gain((L, DIFF_V)),
        'diff_out': nrm((L, N_HEADS * DIFF_V, D), (N_HEADS * DIFF_V) ** -0.5),
        'na_qn_g': gain((L, HEAD_DIM)),
        'na_kn_g': gain((L, HEAD_DIM)),
        'na_rpb': nrm((L, N_HEADS, 2 * NA_KH - 1, 2 * NA_KW - 1), 0.1),
        'na_out': nrm((L, N_HEADS * HEAD_DIM, D), (N_HEADS * HEAD_DIM) ** -0.5),
        'gate_w': nrm((L, D, N_BRANCH * D), D ** -0.5),
        'gate_b': nrm((L, N_BRANCH * D), 0.02),
        'w_o': nrm((L, D, D), D ** -0.5),
        'router_w': nrm((L, D, N_EXPERTS), D ** -0.5),
        'router_b': nrm((L, N_EXPERTS), 0.01),
        'exp_w_gu': nrm((L, N_EXPERTS, D, 2 * D_FF), D ** -0.5),
        'exp_b_gu': nrm((L, N_EXPERTS, 2 * D_FF), 0.02),
        'exp_w_down': nrm((L, N_EXPERTS, D_FF, D), D_FF ** -0.5),
        'exp_b_down': nrm((L, N_EXPERTS, D), 0.02),
    }


def reference(x, c, ctx, c_ctx, ada_w, ada_b, norm1_g, norm2_g, w_in, conv_w, conv_b, conv_ln_g,
              conv_ln_b, conv_out, mla_cq_g, mla_ckv_g, mla_w_uq, mla_w_ukv, mla_qn_g, mla_kn_g,
              mla_out, diff_qn_g, diff_kn_g, diff_lam, diff_subln_g, diff_out, na_qn_g, na_kn_g,
              na_rpb, na_out, gate_w, gate_b, w_o, router_w, router_b, exp_w_gu, exp_b_gu,
              exp_w_down, exp_b_down):
    cos, sin = axial_rope_tables(x.shape[1], x.dtype)
    xc = ctx
    for l in range(DEPTH):
        p = dict(
            w_in=w_in[l], conv_w=conv_w[l], conv_b=conv_b[l], conv_ln_g=conv_ln_g[l],
            conv_ln_b=conv_ln_b[l], conv_out=conv_out[l], mla_cq_g=mla_cq_g[l],
            mla_ckv_g=mla_ckv_g[l], mla_w_uq=mla_w_uq[l], mla_w_ukv=mla_w_ukv[l],
            mla_qn_g=mla_qn_g[l], mla_kn_g=mla_kn_g[l], mla_out=mla_out[l],
            diff_qn_g=diff_qn_g[l], diff_kn_g=diff_kn_g[l], diff_lam=diff_lam[l],
            diff_subln_g=diff_subln_g[l], diff_out=diff_out[l], na_qn_g=na_qn_g[l],
            na_kn_g=na_kn_g[l], na_rpb=na_rpb[l], na_out=na_out[l], gate_w=gate_w[l],
            gate_b=gate_b[l], w_o=w_o[l], router_w=router_w[l], router_b=router_b[l],
            exp_w_gu=exp_w_gu[l], exp_b_gu=exp_b_gu[l], exp_w_down=exp_w_down[l],
            exp_b_down=exp_b_down[l])
        last = l == DEPTH - 1
        lam_init = 0.8 - 0.6 * math.exp(-0.3 * l)
        sh1, sc1, g1, sh2, sc2, g2 = jnp.split(jax.nn.silu(c) @ ada_w[l] + ada_b[l], 6, axis=-1)
        shc1, scc1, gc1, shc2, scc2, gc2 = jnp.split(jax.nn.silu(c_ctx) @ ada_w[l] + ada_b[l], 6, axis=-1)
        h = modulate(x, norm1_g[l], sh1[:, None], sc1[:, None])
        hc = modulate(xc, norm1_g[l], shc1, scc1)
        y, yc = token_mixer(h, hc, p, lam_init, cos, sin, not last)
        x = x + g1[:, None] * y
        x = x + g2[:, None] * moe_ffn(modulate(x, norm2_g[l], sh2[:, None], sc2[:, None]), p)
        if not last:
            xc = xc + gc1 * yc
            xc = xc + gc2 * moe_ffn(modulate(xc, norm2_g[l], shc2, scc2), p)
    return x
```

```python
import math
from contextlib import ExitStack
import numpy as np
import ml_dtypes
import concourse.bass as bass
import concourse.mybir as mybir
from concourse.bass_utils import run_bass_kernel_spmd

F32 = mybir.dt.float32
BF16 = mybir.dt.bfloat16
AF = mybir.ActivationFunctionType
ALU = mybir.AluOpType
AX = mybir.AxisListType

D = 1024
SEQ = 4096
CTX = 256
TOK = SEQ + CTX
NE = 32
DFF = 1024
EPS = 1e-6
DEPTH = 2
NKT = TOK // 128

ENGS = ['pe', 'act', 'dve', 'pool', 'sp']
import os as _os
NOBAR = _os.environ.get('NOBAR', 'p1,cv,na,at,mg').split(',')
POOLP1 = _os.environ.get('POOLP1', '0') == '1'
NDSEM = 8


class V:
    __slots__ = ('ap', 'keys')

    def __init__(self, ap, keys):
        self.ap = ap
        self.keys = keys


class Buf:
    def __init__(self, name, ap, nsub=0):
        self.name = name
        self.apx = ap
        self.nsub = nsub

    def __getitem__(self, idx):
        keys = [self.name] if self.nsub == 0 else [(self.name, i) for i in range(self.nsub)]
        return V(self.apx[idx], keys)

    def s(self, i, idx=None):
        ap = self.apx if idx is None else self.apx[idx]
        return V(ap, [(self.name, i)])


class Sched:
    def __init__(self):
        self.ops = {e: [] for e in ENGS}
        self.ccnt = {e: 0 for e in ENGS}
        self.dcnt = {e: 0 for e in ENGS}
        self.last_w = {}
        self.readers = {}
        self.waited = {e: {} for e in ENGS}
        self.semmax = {}
        self.out_tokens = []

    def _need(self, eng, tok, waits):
        sem, val, teng, is_dma = tok
        if (not is_dma) and teng == eng and eng == 'pe':
            return
        if self.waited[eng].get(sem, 0) >= val:
            return
        self.waited[eng][sem] = val
        waits.append((sem, val))

    def add(self, eng, fn, reads, writes, dma=False, is_out=False):
        waits = []
        if dma:
            i = self.dcnt[eng]
            self.dcnt[eng] += 1
            sem = 'd_%s_%d' % (eng, i % NDSEM)
            val = 16 * (i // NDSEM + 1)
            if i >= NDSEM:
                self._need(eng, (sem, val - 16, eng, True), waits)
            tok = (sem, val, eng, True)
            inc = (sem, 16)
        else:
            self.ccnt[eng] += 1
            sem = 'c_' + eng
            tok = (sem, self.ccnt[eng], eng, False)
            inc = (sem, 1)
        self.semmax[sem] = tok[1]
        for k in reads:
            t = self.last_w.get(k)
            if t is not None:
                self._need(eng, t, waits)
        for k in writes:
            t = self.last_w.get(k)
            if t is not None:
                self._need(eng, t, waits)
            for (rs, (rv, re, rd)) in self.readers.get(k, {}).items():
                self._need(eng, (rs, rv, re, rd), waits)
        for k in writes:
            self.last_w[k] = tok
            self.readers[k] = {}
        for k in reads:
            r = self.readers.setdefault(k, {})
            r[tok[0]] = (tok[1], tok[2], tok[3])
        self.ops[eng].append((fn, waits, inc))
        if is_out:
            self.out_tokens.append(tok)

    def barrier(self, skip_pool_dma=False):
        for e in ENGS:
            waits = []
            for sem, val in self.semmax.items():
                if skip_pool_dma and sem.startswith('d_pool'):
                    continue
                if self.waited[e].get(sem, 0) < val:
                    self.waited[e][sem] = val
                    waits.append((sem, val))
            if waits:
                self.ops[e].append((None, waits, None))
        if not skip_pool_dma:
            self.last_w = {}
            self.readers = {}


class Ctx:
    def __init__(self, nc):
        self.nc = nc
        self.S = Sched()
        self.n_in = {}
        self.stack = None

    def dram_in(self, name, shape, dtype=F32, nsub=0):
        t = self.nc.dram_tensor(name, list(shape), dtype, kind="ExternalInput").ap()
        return Buf(name, t, nsub)

    def dram_out(self, name, shape, dtype=F32, nsub=0):
        t = self.nc.dram_tensor(name, list(shape), dtype, kind="ExternalOutput").ap()
        return Buf(name, t, nsub)

    def dram(self, name, shape, dtype, nsub=0):
        t = self.nc.dram_tensor(name, list(shape), dtype, kind="Internal").ap()
        return Buf(name, t, nsub)

    def sb(self, name, shape, dtype, nsub=0):
        self.uid = getattr(self, 'uid', 0) + 1
        name = 'sb%d_%s' % (self.uid, name)
        t = self.stack.enter_context(self.nc.sbuf_tensor(name, list(shape), dtype))
        return Buf(name, t, nsub)

    def ps(self, name, shape, dtype=F32, nsub=0):
        self.uid = getattr(self, 'uid', 0) + 1
        name = 'ps%d_%s' % (self.uid, name)
        t = self.stack.enter_context(self.nc.psum_tensor(name, list(shape), dtype))
        return Buf(name, t, nsub)

    def _rk(self, *vs):
        ks = []
        for v in vs:
            if isinstance(v, V):
                ks += v.keys
        return ks

    def mm(self, out, lhsT, rhs, start=True, stop=True, **kw):
        o, l, r = out.ap, lhsT.ap, rhs.ap
        rd = self._rk(lhsT, rhs) + ([] if start else out.keys)
        self.S.add('pe', lambda e: e.matmul(o, l, r, start=start, stop=stop, **kw), rd, out.keys)

    def tr(self, out, in_, ident):
        o, i, d = out.ap, in_.ap, ident.ap
        self.S.add('pe', lambda e: e.transpose(o, i, d), self._rk(in_, ident), out.keys)

    def act(self, out, in_, func, bias=0.0, scale=1.0, accum=None):
        o, i = out.ap, in_.ap
        b = bias.ap if isinstance(bias, V) else bias
        s = scale.ap if isinstance(scale, V) else scale
        kw = {}
        wr = list(out.keys)
        if accum is not None:
            kw['accum_out'] = accum.ap
            wr += accum.keys
        self.S.add('act', lambda e: e.activation(o, i, func, bias=b, scale=s, **kw),
                   self._rk(in_, bias, scale), wr)

    def ts(self, eng, out, in0, s1, s2, op0, op1=None, accum=None):
        o, i = out.ap, in0.ap
        a = s1.ap if isinstance(s1, V) else s1
        b = s2.ap if isinstance(s2, V) else s2
        kw = {}
        wr = list(out.keys)
        if accum is not None:
            kw['accum_out'] = accum.ap
            wr += accum.keys
        if op1 is None:
            self.S.add(eng, lambda e: e.tensor_scalar(o, i, a, None, op0, **kw), self._rk(in0, s1), wr)
        else:
            self.S.add(eng, lambda e: e.tensor_scalar(o, i, a, b, op0, op1, **kw), self._rk(in0, s1, s2), wr)

    def tt(self, eng, out, in0, in1, op):
        o, a, b = out.ap, in0.ap, in1.ap
        self.S.add(eng, lambda e: e.tensor_tensor(o, a, b, op), self._rk(in0, in1), out.keys)

    def stt(self, eng, out, in0, scalar, in1, op0, op1):
        o, a, b = out.ap, in0.ap, in1.ap
        s = scalar.ap if isinstance(scalar, V) else scalar
        eng = 'dve'
        self.S.add(eng, lambda e: e.scalar_tensor_tensor(o, a, s, b, op0, op1), self._rk(in0, scalar, in1), out.keys)

    def copy(self, eng, out, in_):
        o, i = out.ap, in_.ap
        if eng == 'act':
            self.S.add(eng, lambda e: e.copy(o, i), in_.keys, out.keys)
        else:
            self.S.add(eng, lambda e: e.tensor_copy(o, i), in_.keys, out.keys)

    def memset(self, eng, out, val):
        o = out.ap
        self.S.add(eng, lambda e: e.memset(o, val), [], out.keys)

    def recip(self, out, in_):
        o, i = out.ap, in_.ap
        self.S.add('dve', lambda e: e.reciprocal(o, i), in_.keys, out.keys)

    def vmax8(self, out, in_):
        o, i = out.ap, in_.ap
        self.S.add('dve', lambda e: e.max(o, i), in_.keys, out.keys)

    def dma(self, q, out, in_, is_out=False):
        o, i = out.ap, in_.ap
        self.S.add(q, lambda e: e.dma_start(out=o, in_=i), in_.keys, out.keys, dma=True, is_out=is_out)

    def emit(self):
        nc, S = self.nc, self.S
        fw = []
        for (sem, val, e, d) in S.out_tokens:
            fw.append((sem, val))
        S.ops['sp'].append((None, fw, None))
        names = sorted(S.semmax.keys())
        with ExitStack() as st:
            sems = {n: st.enter_context(nc.semaphore(n)) for n in names}
            block = st.enter_context(nc.Block())

            def run(engname):
                def body(eng):
                    for (fn, waits, inc) in S.ops[engname]:
                        for (sn, v) in waits:
                            eng.wait_ge(sems[sn], v)
                        if fn is not None:
                            ins = fn(eng)
                            ins.then_inc(sems[inc[0]], inc[1])
                return body

            block.tensor(run('pe'))
            block.scalar(run('act'))
            block.vector(run('dve'))
            block.gpsimd(run('pool'))
            block.sync(run('sp'))


VC = {}
_o = 0
for _n, _w in [('ada_b', 96), ('n1g', 8), ('n2g', 8), ('conv_w', 62), ('conv_b', 2), ('cln_g', 2), ('cln_b', 2),
               ('cq_g', 2), ('ckv_g', 1), ('mqn_g', 1), ('mkn_g', 1), ('dqn_g', 1), ('dkn_g', 1),
               ('nqn_g', 1), ('nkn_g', 1), ('gate_b', 32), ('bg', 512), ('bu', 512), ('sub_g', 1)]:
    VC[_n] = (_o, _w)
    _o += _w
NVC = _o
CC = {}
_o = 0
for _n, _w in [('ones', 128), ('bd32', 128), ('bd64', 128), ('bd96', 128), ('identf', 128)]:
    CC[_n] = (_o, _w)
    _o += _w
NCC = _o
CB = {'ident': (0, 128), 'RM': (128, 128), 'RD': (256, 128)}
NCB = 384

TILES = [(0, 256)] + [(256 + 512 * i, 512) for i in range(8)]


def rope_tables():
    t = np.arange(SEQ)
    rows = (t // 64).astype(np.float32)
    cols = (t % 64).astype(np.float32)
    inv = (10000.0 ** (-np.arange(0, 16, 2, dtype=np.float32) / 16)).astype(np.float32)
    theta = np.concatenate([rows[:, None] * inv, cols[:, None] * inv], axis=-1).astype(np.float32)
    cos = np.cos(theta).astype(np.float32).T
    sin = np.sin(theta).astype(np.float32).T
    c32 = np.ones((32, TOK), np.float32)
    s32 = np.zeros((32, TOK), np.float32)
    c32[:16, CTX:] = cos
    c32[16:, CTX:] = cos
    s32[:16, CTX:] = sin
    s32[16:, CTX:] = sin
    cosM = np.ones((128, TOK), np.float32)
    sinM = np.zeros((128, TOK), np.float32)
    cosM[64:96] = c32
    sinM[64:96] = s32
    cosD = np.tile(c32, (4, 1))
    sinD = np.tile(s32, (4, 1))
    return np.stack([cosM, sinM, cosD, sinD], 0)


def rot_lhsT(n):
    m = np.zeros((128, 128), np.float32)
    for blk in n:
        for i in range(16):
            m[blk + i + 16, blk + i] = -1.0
            m[blk + i, blk + i + 16] = 1.0
    return m


def make_consts():
    c = np.zeros((128, NCC), np.float32)
    c[:, 0:128] = 1.0
    for b in range(4):
        c[b * 32:(b + 1) * 32, 128 + b * 32:128 + (b + 1) * 32] = 1.0
    for b in range(2):
        c[b * 64:(b + 1) * 64, 256 + b * 64:256 + (b + 1) * 64] = 1.0
    c[0:96, 384:384 + 96] = 1.0
    c[:, 512:640] = np.eye(128, dtype=np.float32)
    cb = np.zeros((128, NCB), np.float32)
    cb[:, 0:128] = np.eye(128)
    cb[:, 128:256] = rot_lhsT([64])
    cb[:, 256:384] = rot_lhsT([0, 32, 64, 96])
    return c, cb.astype(ml_dtypes.bfloat16)


def na_slots(i):
    rows = [2 * i, 2 * i + 1]
    need = set()
    for r in rows:
        rs = min(max(r - 4, 0), 56)
        for rr in range(rs, rs + 8):
            need.add(rr // 2)
    js = sorted(need)
    while len(js) < 5:
        js.append(js[0] if i >= 2 else js[-1])
    return js


def na_case(i):
    return {0: 0, 1: 1, 30: 3, 31: 4}.get(i, 2)


def na_tables():
    rep = {0: 0, 1: 1, 2: 5, 3: 30, 4: 31}
    idx_r = np.zeros((5, 128, 5, 128), np.int64)
    idx_c = np.zeros((5, 128, 5, 128), np.int64)
    mask = np.full((5, 128, 5, 128), -1e30, np.float32)
    for cs, i in rep.items():
        js = na_slots(i)
        seen = set()
        for sl, j in enumerate(js):
            dummy = j in seen
            seen.add(j)
            for kk in range(128):
                kr, kc = 2 * j + kk // 64, kk % 64
                for qq in range(128):
                    qr, qc = 2 * i + qq // 64, qq % 64
                    rs = min(max(qr - 4, 0), 56)
                    ws = min(max(qc - 8, 0), 48)
                    ok = (not dummy) and (rs <= kr < rs + 8) and (ws <= kc < ws + 16)
                    if ok:
                        idx_r[cs, kk, sl, qq] = kr - qr + 7
                        idx_c[cs, kk, sl, qq] = kc - qc + 15
                        mask[cs, kk, sl, qq] = 0.0
    return idx_r, idx_c, mask


_NA_CACHE = {}


def na_tables_cached():
    if 'v' not in _NA_CACHE:
        _NA_CACHE['v'] = na_tables_fast()
    return _NA_CACHE['v']


def na_tables_fast():
    rep = {0: 0, 1: 1, 2: 5, 3: 30, 4: 31}
    idx_r = np.zeros((5, 128, 5, 128), np.int64)
    idx_c = np.zeros((5, 128, 5, 128), np.int64)
    mask = np.full((5, 128, 5, 128), -1e30, np.float32)
    kk = np.arange(128)[:, None]
    qq = np.arange(128)[None, :]
    for cs, i in rep.items():
        js = na_slots(i)
        seen = set()
        for sl, j in enumerate(js):
            dummy = j in seen
            seen.add(j)
            if dummy:
                continue
            kr, kc = 2 * j + kk // 64, kk % 64
            qr, qc = 2 * i + qq // 64, qq % 64
            rs = np.clip(qr - 4, 0, 56)
            ws = np.clip(qc - 8, 0, 48)
            ok = (rs <= kr) & (kr < rs + 8) & (ws <= kc) & (kc < ws + 16)
            ir = np.where(ok, kr - qr + 7, 0)
            ic = np.where(ok, kc - qc + 15, 0)
            idx_r[cs, :, sl, :] = ir
            idx_c[cs, :, sl, :] = ic
            mask[cs, :, sl, :] = np.where(ok, 0.0, -1e30)
    return idx_r, idx_c, mask


def build(dbg=False, layers=DEPTH, stop_after=None, lvl=9, ntl=99):
    nc = bass.Bass("TRN2", target_bir_lowering=False)
    K = Ctx(nc)
    S = K.S
    xT = K.dram_in('xT', [D, TOK])
    cT = K.dram_in('cT', [128, 16])
    consts_d = K.dram_in('consts', [128, NCC])
    constb_d = K.dram_in('constb', [128, NCB], BF16)
    rope_d = K.dram_in('rope', [4, 128, TOK])
    namask_d = K.dram_in('namask', [5, 128, 640])
    W = []
    for l in range(layers):
        w = {}
        w['ada_w'] = K.dram_in('ada_w%d' % l, [D, 6 * D])
        w['vecs'] = K.dram_in('vecs%d' % l, [128, NVC])
        w['rows'] = K.dram_in('rows%d' % l, [1, 64 + 32 + 128])
        w['w_in'] = K.dram_in('w_in%d' % l, [D, 2528])
        w['w_uq'] = K.dram_in('w_uq%d' % l, [192, 384])
        w['w_ukv'] = K.dram_in('w_ukv%d' % l, [128, 640])
        w['bouts'] = K.dram_in('bouts%d' % l, [4, 256, D])
        w['gate_w'] = K.dram_in('gate_w%d' % l, [D, 4 * D])
        w['w_o'] = K.dram_in('w_o%d' % l, [D, D])
        w['router_w'] = K.dram_in('router_w%d' % l, [D, NE])
        w['w_gu'] = K.dram_in('w_gu%d' % l, [NE, D, 2 * DFF])
        w['w_dn'] = K.dram_in('w_dn%d' % l, [NE, DFF, D])
        w['b_dn'] = K.dram_in('b_dn%d' % l, [NE, D])
        w['nab'] = K.dram_in('nab%d' % l, [5, 4, 128, 640])
        W.append(w)
    outT = K.dram_out('outT', [D, SEQ])
    hT = K.dram('hT', [D, TOK], BF16)
    uT = K.dram('uT', [256, TOK], F32)
    QT = {'mla': K.dram('QTm', [4, 128, TOK], BF16), 'diff': K.dram('QTd', [2, 128, TOK], BF16),
          'na': K.dram('QTn', [2, 128, TOK], BF16)}
    KT = {'mla': K.dram('KTm', [4, 128, TOK], BF16), 'diff': K.dram('KTd', [2, 128, TOK], BF16),
          'na': K.dram('KTn', [2, 128, TOK], BF16)}
    VA = {'mla': K.dram('VAm', [NKT, 128, 260], BF16), 'diff': K.dram('VAd', [NKT, 128, 260], BF16),
          'na': K.dram('VAn', [NKT, 128, 260], BF16)}
    OT = {n: K.dram('OT' + n, [256, TOK], BF16) for n in ['conv', 'mla', 'diff', 'na']}
    res1 = K.dram('res1', [D, TOK], F32)
    res2 = K.dram('res2', [D, TOK], F32)
    h2T = K.dram('h2T', [D, TOK], BF16)
    gT = K.dram('gT', [NE, TOK], F32)
    dbg_out = {}
    if dbg:
        for n, shp in [('d_mod', [128, 96]), ('d_hT', [D, TOK]), ('d_uT', [256, TOK]), ('d_QTm', [4, 128, TOK]),
                       ('d_KTm', [4, 128, TOK]), ('d_QTd', [2, 128, TOK]), ('d_KTd', [2, 128, TOK]),
                       ('d_QTn', [2, 128, TOK]), ('d_KTn', [2, 128, TOK]), ('d_VAm', [NKT, 128, 260]),
                       ('d_OTconv', [256, TOK]), ('d_OTmla', [256, TOK]), ('d_OTdiff', [256, TOK]),
                       ('d_OTna', [256, TOK]), ('d_res1', [D, TOK]), ('d_h2T', [D, TOK]), ('d_gT', [NE, TOK]),
                       ('d_res2', [D, TOK])]:
            dbg_out[n] = K.dram_out(n, shp, F32 if n in ('d_mod', 'd_uT', 'd_res1', 'd_gT', 'd_res2') else BF16)

    def phase():
        st = ExitStack()
        K.stack = st
        return st

    def end_phase(st):
        S.barrier()
        st.close()

    for l in range(layers):
        w = W[l]
        last = (l == DEPTH - 1)
        lam_init = 0.8 - 0.6 * math.exp(-0.3 * l)
        res_in = xT if l == 0 else res2
        tiles = TILES
        lst = ExitStack()
        K.stack = lst
        vecs = K.sb('vecs%d' % l, [128, NVC], F32)
        cst = K.sb('cst%d' % l, [128, NCC], F32)
        cstb = K.sb('cstb%d' % l, [128, NCB], BF16)
        mod = K.sb('mod%d' % l, [128, 96], F32)
        A1 = K.sb('A1_%d' % l, [128, 16], F32)
        A2 = K.sb('A2_%d' % l, [128, 16], F32)
        lamt = K.sb('lamt%d' % l, [128, 4], F32)
        rowsb = K.sb('rowsb%d' % l, [128, 224], F32)
        vecs_eps2 = K.sb('vecs_eps2_%d' % l, [128, 1], F32)
        K.memset('pool', vecs_eps2[:, :], EPS)
        K.dma('sp', vecs[:, :], w['vecs'][:, :])
        K.dma('sp', cst[:, :], consts_d[:, :])
        K.dma('sp', cstb[:, :], constb_d[:, :])
        K.dma('sp', rowsb[:, :], V(w['rows'].apx[0:1, :].partition_broadcast(128), w['rows'][:, :].keys))

        def vec(name, col=0, rows=128, base=0):
            o, _ = VC[name]
            return vecs[base:base + rows, o + col:o + col + 1]

        def cc(name, r0=0, r1=128, c0=0, c1=128):
            o, _ = CC[name]
            return cst[r0:r1, o + c0:o + c1]

        def cb(name, r0=0, r1=128, c0=0, c1=128):
            o, _ = CB[name]
            return cstb[r0:r1, o + c0:o + c1]

        def modv(j, col):
            return mod[:, 2 * j + col:2 * j + col + 1]

        st = phase()
        cin = K.sb('cin', [128, 16], F32)
        sig = K.sb('sig', [128, 16], F32)
        scb = K.sb('scb', [128, 16], BF16)
        K.dma('sp', cin[:, :], cT[:, :])
        K.act(sig[:, :], cin[:, :], AF.Sigmoid)
        K.tt('dve', scb[:, :], cin[:, :], sig[:, :], ALU.mult)
        pmod = K.ps('pmod', [128, 96], F32)
        for blk in range(4):
            awb = K.sb('awb%d' % blk, [128, 8, 1536], BF16)
            K.dma('pool', awb[:, :, :], V(w['ada_w'].apx[:, blk * 1536:(blk + 1) * 1536].rearrange('(k p) n -> p k n', p=128),
                                         w['ada_w'][:, :].keys))
            for jj in range(12):
                j = blk * 12 + jj
                for k in range(8):
                    K.mm(pmod[:, 2 * j:2 * j + 2], awb[:, k, jj * 128:(jj + 1) * 128], scb[:, 2 * k:2 * k + 2],
                         start=(k == 0), stop=(k == 7), skip_group_check=True)
        o_ab, _ = VC['ada_b']
        K.tt('dve', mod[:, :], pmod[:, :], vecs[:, o_ab:o_ab + 96], ALU.add)
        for (A, gname, j0) in [(A1, 'n1g', 8), (A2, 'n2g', 32)]:
            for k in range(8):
                K.ts('dve', A[:, 2 * k:2 * k + 2], mod[:, 2 * (j0 + k):2 * (j0 + k) + 2], 1.0, vec(gname, k), ALU.add, ALU.mult)
        lt = K.sb('lt', [128, 64], F32)
        ls = K.sb('ls', [128, 4], F32)
        K.tt('dve', lt[:, 0:32], rowsb[:, 96:128], rowsb[:, 128:160], ALU.mult)
        K.tt('dve', lt[:, 32:64], rowsb[:, 160:192], rowsb[:, 192:224], ALU.mult)
        K.S.add('dve', (lambda o, i: (lambda e: e.tensor_reduce(o, i, AX.X, ALU.add)))(ls[:, 0:2].ap, lt[:, :].ap.rearrange('p (a b) -> p a b', a=2)),
                lt[:, :].keys, ls[:, :].keys)
        K.act(ls[:, 2:4], ls[:, 0:2], AF.Exp)
        K.tt('dve', lamt[:, 1:2], ls[:, 3:4], ls[:, 2:3], ALU.subtract)
        K.ts('dve', lamt[:, 0:1], lamt[:, 1:2], -lam_init, None, ALU.add)
        if dbg and l == 0:
            K.dma('sp', dbg_out['d_mod'][:, :], mod[:, :], is_out=True)
        end_phase(st)
        if stop_after == 'A':
            break

        st = phase()
        win = K.sb('win', [128, 8, 2528], BF16)
        K.dma('pool', win[:, :, :], V(w['w_in'].apx.rearrange('(k p) n -> p k n', p=128), w['w_in'][:, :].keys))
        wuq = K.sb('wuq', [128, 2, 384], BF16)
        K.dma('pool', wuq[:, 0, :], w['w_uq'][0:128, :])
        K.dma('pool', wuq[0:64, 1, :], w['w_uq'][128:192, :])
        wukv = K.sb('wukv', [128, 640], BF16)
        K.dma('pool', wukv[:, :], w['w_ukv'][:, :])
        NB = 2
        xt = [K.sb('xt%d' % i, [128, 8, 512], F32) for i in range(NB)]
        ht = [K.sb('ht%d' % i, [128, 8, 512], BF16) for i in range(NB)]
        rp = [K.sb('rp%d' % i, [128, 4, 512], F32) for i in range(NB)]
        rstd = K.sb('rstd', [128, 512], F32)
        tmp = [K.sb('tmp%d' % i, [128, 512], F32) for i in range(4)]
        tb = [K.sb('tb%d' % i, [128, 512], BF16) for i in range(4)]
        cqn = K.sb('cqn', [128, 2, 512], BF16)
        ckvn = K.sb('ckvn', [128, 512], BF16)
        ut = [K.sb('ut%d' % i, [128, 2, 512], F32) for i in range(NB)]
        qo = {n: [K.sb('qo%s%d' % (n, i), [128, c, 512], BF16) for i in range(NB)] for n, c in [('mla', 4), ('diff', 2), ('na', 2)]}
        ko = {n: [K.sb('ko%s%d' % (n, i), [128, c, 512], BF16) for i in range(NB)] for n, c in [('mla', 4), ('diff', 2), ('na', 2)]}
        va = {n: [K.sb('va%s%d' % (n, i), [128, 4, 65], BF16) for i in range(NB)] for n in ['mla', 'diff', 'na']}
        for n in va:
            for i in range(NB):
                K.memset('pool', va[n][i][:, :, :], 1.0)
        pb = [K.ps('pb%d' % i, [128, 512], F32) for i in range(8)]
        pbi = [0]

        def nextp():
            p = pb[pbi[0] % 8]
            pbi[0] += 1
            return p

        tmi = [0]

        def nt():
            t = tmp[tmi[0] % 4]
            tmi[0] += 1
            return t

        tbi = [0]

        def ntb():
            t = tb[tbi[0] % 4]
            tbi[0] += 1
            return t

        def rms_feat(src_list, bdname, nfeat, N):
            pst = nextp()
            for ii, (src, rows) in enumerate(src_list):
                s_ = nt()
                K.act(s_[0:rows, 0:N], src, AF.Square)
                K.mm(pst[:, 0:N], cc(bdname, 0, rows), s_[0:rows, 0:N], start=(ii == 0), stop=(ii == len(src_list) - 1))
            r_ = nt()
            K.act(r_[:, 0:N], pst[:, 0:N], AF.Sqrt, bias=vecs_eps[:, 0:1], scale=1.0 / nfeat)
            r2 = nt()
            K.recip(r2[:, 0:N], r_[:, 0:N])
            return r2

        vecs_eps = K.sb('vecs_eps', [128, 1], F32)
        K.memset('pool', vecs_eps[:, :], EPS)

        def rope_qk(src, rows, bdname, nfeat, gvec, rot, cosv, sinv, dst, N):
            r2 = rms_feat([(src, rows)], bdname, nfeat, N)
            if rot is None:
                K.stt('dve', dst, src, gvec, r2[0:rows, 0:N], ALU.mult, ALU.mult)
                return
            qn = ntb()
            K.stt('dve', qn[0:rows, 0:N], src, gvec, r2[0:rows, 0:N], ALU.mult, ALU.mult)
            pr = nextp()
            K.mm(pr[0:rows, 0:N], rot, qn[0:rows, 0:N])
            t1 = nt()
            K.tt('pool' if POOLP1 else 'dve', t1[0:rows, 0:N], qn[0:rows, 0:N], cosv, ALU.mult)
            t2 = nt()
            K.tt('dve', t2[0:rows, 0:N], pr[0:rows, 0:N], sinv, ALU.mult)
            K.tt('pool' if POOLP1 else 'dve', dst, t1[0:rows, 0:N], t2[0:rows, 0:N], ALU.add)

        cA = [K.sb('cA%d' % i, [128, 512], F32) for i in range(4)]
        cB = [K.sb('cB%d' % i, [128, 512], F32) for i in range(4)]
        cQ = [K.sb('cQ%d' % i, [128, 512], BF16) for i in range(4)]

        def chains(items, N):
            n = len(items)
            srcs = []
            for i, it in enumerate(items):
                srcs.append(it[0](pb[i]))
            for i, it in enumerate(items):
                rows = it[1]
                K.act(cA[i][0:rows, 0:N], srcs[i], AF.Square)
            for i, it in enumerate(items):
                rows = it[1]
                K.mm(pb[4 + i][:, 0:N], cc(it[2], 0, rows), cA[i][0:rows, 0:N])
            for i, it in enumerate(items):
                K.act(cA[i][:, 0:N], pb[4 + i][:, 0:N], AF.Sqrt, bias=vecs_eps[:, 0:1], scale=1.0 / it[3])
            for i, it in enumerate(items):
                K.recip(cA[i][:, 0:N], cA[i][:, 0:N])
            for i, it in enumerate(items):
                rows, gvec, rot, dst = it[1], it[4], it[5], it[8]
                if rot is None:
                    K.stt('dve', dst, srcs[i], gvec, cA[i][0:rows, 0:N], ALU.mult, ALU.mult)
                else:
                    K.stt('dve', cQ[i][0:rows, 0:N], srcs[i], gvec, cA[i][0:rows, 0:N], ALU.mult, ALU.mult)
            for i, it in enumerate(items):
                rows, rot = it[1], it[5]
                if rot is not None:
                    K.mm(pb[4 + i][0:rows, 0:N], rot, cQ[i][0:rows, 0:N])
            for i, it in enumerate(items):
                rows, rot, cosv = it[1], it[5], it[6]
                if rot is not None:
                    K.tt('dve', cA[i][0:rows, 0:N], cQ[i][0:rows, 0:N], cosv, ALU.mult)
            for i, it in enumerate(items):
                rows, rot, sinv = it[1], it[5], it[7]
                if rot is not None:
                    K.tt('dve', cB[i][0:rows, 0:N], pb[4 + i][0:rows, 0:N], sinv, ALU.mult)
            for i, it in enumerate(items):
                rows, rot, dst = it[1], it[5], it[8]
                if rot is not None:
                    K.tt('dve', dst, cA[i][0:rows, 0:N], cB[i][0:rows, 0:N], ALU.add)

        for ti, (c0, N) in enumerate(tiles[:ntl]):
            col = 1 if ti == 0 else 0
            b = ti % NB
            X, H, RP, U = xt[b], ht[b], rp[b], ut[b]
            K.dma('sp', X[:, :, 0:N], V(res_in.apx[:, c0:c0 + N].rearrange('(k p) n -> p k n', p=128), res_in[:, :].keys))
            K.dma('sp', RP[:, :, 0:N], V(rope_d.apx[:, :, c0:c0 + N].rearrange('f p n -> p f n'), rope_d[:, :, :].keys))
            pst = nextp()
            for k in range(8):
                sq_ = nt()
                K.act(sq_[:, 0:N], X[:, k, 0:N], AF.Square)
                K.mm(pst[:, 0:N], cc('ones'), sq_[:, 0:N], start=(k == 0), stop=(k == 7))
            r_ = nt()
            K.act(r_[:, 0:N], pst[:, 0:N], AF.Sqrt, bias=vecs_eps[:, 0:1], scale=1.0 / D)
            K.recip(rstd[:, 0:N], r_[:, 0:N])
            for k in range(8):
                t_ = nt()
                K.stt('dve' if k % 2 == 0 else 'pool', t_[:, 0:N], X[:, k, 0:N], A1[:, 2 * k + col:2 * k + col + 1], rstd[:, 0:N], ALU.mult, ALU.mult)
                K.act(H[:, k, 0:N], t_[:, 0:N], AF.Identity, bias=modv(k, col), scale=1.0)
            K.dma('sp', V(hT.apx[:, c0:c0 + N].rearrange('(k p) n -> p k n', p=128), [('hT', ti)]), H[:, :, 0:N])

            def proj(chunk, rows=128):
                p = nextp()
                for k in range(8):
                    K.mm(p[0:rows, 0:N], win[:, k, chunk * 128:chunk * 128 + rows], H[:, k, 0:N], start=(k == 0), stop=(k == 7))
                return p
            if lvl < 1:
                continue
            for c in range(2):
                pa = proj(c)
                pbb = proj(2 + c)
                sg = nt()
                K.act(sg[:, 0:N], pbb[:, 0:N], AF.Sigmoid)
                K.tt('dve', U[:, c, 0:N], pa[:, 0:N], sg[:, 0:N], ALU.mult)
            K.dma('sp', V(uT.apx[:, c0:c0 + N].rearrange('(k p) n -> p k n', p=128), [('uT', ti)]), U[:, :, 0:N])
            if lvl < 2:
                continue
            pq0 = proj(4)
            pq1 = proj(5, 64)
            r2 = rms_feat([(pq0[:, 0:N], 128), (pq1[0:64, 0:N], 64)], 'ones', 192, N)
            K.stt('dve', cqn[:, 0, 0:N], pq0[:, 0:N], vec('cq_g', 0), r2[:, 0:N], ALU.mult, ALU.mult)
            K.stt('dve', cqn[0:64, 1, 0:N], pq1[0:64, 0:N], vec('cq_g', 1, 64), r2[0:64, 0:N], ALU.mult, ALU.mult)
            pkv = proj(6)
            r2 = rms_feat([(pkv[:, 0:N], 128)], 'ones', 128, N)
            K.stt('dve', ckvn[:, 0:N], pkv[:, 0:N], vec('ckv_g', 0), r2[:, 0:N], ALU.mult, ALU.mult)
            QO, KO = qo['mla'][b], ko['mla'][b]
            def mk_q(h):
                def f(p):
                    K.mm(p[0:96, 0:N], wuq[:, 0, h * 96:(h + 1) * 96], cqn[:, 0, 0:N], start=True, stop=False)
                    K.mm(p[0:96, 0:N], wuq[0:64, 1, h * 96:(h + 1) * 96], cqn[0:64, 1, 0:N], start=False, stop=True)
                    return p[0:96, 0:N]
                return f

            def mk_k(h):
                def f(p):
                    K.mm(p[0:96, 0:N], wukv[:, h * 96:(h + 1) * 96], ckvn[:, 0:N], start=True, stop=False)
                    for k in range(8):
                        K.mm(p[0:96, 0:N], win[:, k, 1920:2016], H[:, k, 0:N], start=False, stop=(k == 7))
                    return p[0:96, 0:N]
                return f
            for hp in range(2):
                items = []
                for h in (2 * hp, 2 * hp + 1):
                    items.append((mk_q(h), 96, 'bd96', 96, vec('mqn_g', 0, 96), cb('RM', 0, 96, 0, 96), RP[0:96, 0, 0:N], RP[0:96, 1, 0:N], QO[0:96, h, 0:N]))
                    items.append((mk_k(h), 96, 'bd96', 96, vec('mkn_g', 0, 96), cb('RM', 0, 96, 0, 96), RP[0:96, 0, 0:N], RP[0:96, 1, 0:N], KO[0:96, h, 0:N]))
                chains(items, N)
            K.dma('sp', V(QT['mla'].apx[:, 0:96, c0:c0 + N].rearrange('h p n -> p h n'), [('QTm', ti)]), QO[0:96, :, 0:N])
            K.dma('sp', V(KT['mla'].apx[:, 0:96, c0:c0 + N].rearrange('h p n -> p h n'), [('KTm', ti)]), KO[0:96, :, 0:N])
            if lvl < 3:
                continue
            for (nm, ch0, bd, nf, gq, gk, rot, ci) in [('diff', 7, 'bd32', 32, 'dqn_g', 'dkn_g', cb('RD'), 2), ('na', 11, 'bd64', 64, 'nqn_g', 'nkn_g', None, None)]:
                if 'p1' not in NOBAR:
                    S.barrier()
                QO, KO = qo[nm][b], ko[nm][b]

                def mk_p(chunk):
                    def f(p):
                        for k in range(8):
                            K.mm(p[:, 0:N], win[:, k, chunk * 128:chunk * 128 + 128], H[:, k, 0:N], start=(k == 0), stop=(k == 7))
                        return p[:, 0:N]
                    return f
                items = []
                for c in range(2):
                    for (dst, cho, g) in [(QO, 0, gq), (KO, 2, gk)]:
                        items.append((mk_p(ch0 + cho + c), 128, bd, nf, vec(g, 0), rot,
                                      None if ci is None else RP[:, ci, 0:N], None if ci is None else RP[:, ci + 1, 0:N], dst[:, c, 0:N]))
                chains(items, N)
                sfx = 'd' if nm == 'diff' else 'n'
                K.dma('sp', V(QT[nm].apx[:, :, c0:c0 + N].rearrange('h p n -> p h n'), [('QT' + sfx, ti)]), QO[:, :, 0:N])
                K.dma('sp', V(KT[nm].apx[:, :, c0:c0 + N].rearrange('h p n -> p h n'), [('KT' + sfx, ti)]), KO[:, :, 0:N])
            if lvl < 4:
                continue
            nsub = N // 128
            for s_ in range(nsub):
                kt = c0 // 128 + s_
                VAm, VAd, VAn = va['mla'][kt % NB], va['diff'][kt % NB], va['na'][kt % NB]
                p = nextp()
                K.mm(p[:, 0:256], ckvn[:, s_ * 128:(s_ + 1) * 128], wukv[:, 384:640])
                for h4 in range(4):
                    K.copy('dve', VAm[:, h4, 0:64], p[:, h4 * 64:(h4 + 1) * 64])
                p = nextp()
                for k in range(8):
                    K.mm(p[:, 0:512], H[:, k, s_ * 128:(s_ + 1) * 128], win[:, k, 2016:2528], start=(k == 0), stop=(k == 7))
                for h4 in range(4):
                    K.copy('dve', VAd[:, h4, 0:64], p[:, h4 * 64:(h4 + 1) * 64])
                    K.copy('dve', VAn[:, h4, 0:64], p[:, 256 + h4 * 64:256 + (h4 + 1) * 64])
                for nm, t_ in [('mla', VAm), ('diff', VAd), ('na', VAn)]:
                    K.dma('sp', V(VA[nm].apx[kt, :, :].rearrange('p (h d) -> p h d', h=4), [('VA' + nm, kt)]), t_[:, :, :])
        if dbg and l == 0:
            S.barrier()
            for (dn, src) in [('d_hT', hT), ('d_uT', uT), ('d_QTm', QT['mla']), ('d_KTm', KT['mla']), ('d_QTd', QT['diff']),
                              ('d_KTd', KT['diff']), ('d_QTn', QT['na']), ('d_KTn', KT['na']), ('d_VAm', VA['mla'])]:
                K.dma('sp', V(dbg_out[dn].apx, [dn]), V(src.apx, [src.name + '_all']), is_out=True)
        end_phase(st)
        if stop_after == 'P1':
            break

        st = phase()
        accs = [K.sb('cacc%d' % c, [128, TOK], F32) for c in range(2)]
        o_cw, _ = VC['conv_w']
        Ucb = [K.sb('Ucb%d' % c, [128, 286], BF16) for c in range(2)]
        Ulb = [K.sb('Ulb%d' % c, [128, 4126], BF16) for c in range(2)]
        Dg = [K.sb('Dg%d' % c, [128, 31, 128], BF16) for c in range(2)]
        cvp = [K.ps('cvp%d' % i, [128, 512], F32) for i in range(4)]
        for c in range(2):
            K.memset('pool', Ucb[c][:, :], 0.0)
            K.memset('pool', Ulb[c][:, :], 0.0)
            K.dma('pool', Ucb[c][:, 15:271], V(uT.apx[c * 128:(c + 1) * 128, 0:256], ['uT_all']))
            K.dma('pool', Ulb[c][:, 15:4111], V(uT.apx[c * 128:(c + 1) * 128, 256:TOK], ['uT_all']))
            for wv in range(31):
                K.ts('dve', Dg[c][:, wv, :], cc('identf'), vecs[:, o_cw + c * 31 + wv:o_cw + c * 31 + wv + 1], None, ALU.mult)
        for ti, (c0, N) in enumerate(tiles):
            for c in range(2):
                src, off = (Ucb[c], 0) if ti == 0 else (Ulb[c], c0 - 256)
                pc = cvp[(ti * 2 + c) % 4]
                for wv in range(31):
                    K.mm(pc[:, 0:N], Dg[c][:, wv, :], src[:, off + wv:off + wv + N], start=(wv == 0), stop=(wv == 30))
                K.act(accs[c][:, c0:c0 + N], pc[:, 0:N], AF.Identity, bias=vec('conv_b', c), scale=1.0)
        ceps = K.sb('ceps', [128, 1], F32)
        K.memset('pool', ceps[:, :], EPS)
        ctmp = [K.sb('ctmp%d' % i, [128, 512], F32) for i in range(6)]
        zt = K.sb('zt', [128, 2, 512], BF16)
        cp1 = K.ps('cp1', [128, 512], F32)
        cp2 = K.ps('cp2', [128, 512], F32)
        for ti, (c0, N) in enumerate(tiles):
            for c in range(2):
                K.mm(cp1[:, 0:N], cc('ones'), accs[c][:, c0:c0 + N], start=(c == 0), stop=(c == 1))
            for c in range(2):
                K.act(ctmp[c][:, 0:N], accs[c][:, c0:c0 + N], AF.Square)
                K.mm(cp2[:, 0:N], cc('ones'), ctmp[c][:, 0:N], start=(c == 0), stop=(c == 1))
            mean, msq, var, sd, rs = ctmp[2], ctmp[3], ctmp[4], ctmp[5], ctmp[0]
            K.act(mean[:, 0:N], cp1[:, 0:N], AF.Copy, scale=1.0 / 256)
            K.tt('dve', msq[:, 0:N], mean[:, 0:N], mean[:, 0:N], ALU.mult)
            K.stt('dve', var[:, 0:N], cp2[:, 0:N], 1.0 / 256, msq[:, 0:N], ALU.mult, ALU.subtract)
            K.act(sd[:, 0:N], var[:, 0:N], AF.Sqrt, bias=ceps[:, 0:1], scale=1.0)
            K.recip(rs[:, 0:N], sd[:, 0:N])
            for c in range(2):
                t1 = ctmp[1]
                K.tt('dve', t1[:, 0:N], accs[c][:, c0:c0 + N], mean[:, 0:N], ALU.subtract)
                K.tt('dve', t1[:, 0:N], t1[:, 0:N], rs[:, 0:N], ALU.mult)
                K.act(zt[:, c, 0:N], t1[:, 0:N], AF.Silu, bias=vec('cln_b', c), scale=vec('cln_g', c))
            K.dma('sp', V(OT['conv'].apx[:, c0:c0 + N].rearrange('(k p) n -> p k n', p=128), [('OTconv', ti)]), zt[:, :, 0:N])
            if 'cv' not in NOBAR:
                S.barrier()
        end_phase(st)

        def attention(kind):
            st = phase()
            nshift = K.sb('nshift', [128, 1], F32)
            NPO = 4 if kind == 'na' else 2
            NPS = 7 - NPO
            NPT = 6
            psS = [K.ps('psS%d' % i, [128, 512], F32) for i in range(NPS)]
            psO = [K.ps('psO%d' % i, [128, 512], F32) for i in range(NPO)]
            psB = K.ps('psB', [128, 512], F32)
            Pt = [K.sb('Pt%d' % i, [128, 1024 if kind == 'na' else 512], BF16) for i in range(2 if kind == 'na' else NPT)]
            rcp = [K.sb('rcp%d' % i, [128, 512], F32) for i in range(2)]
            bcs = [K.sb('bcs%d' % i, [64, 512], F32) for i in range(2)]
            resb = [K.sb('resb%d' % i, [64, 512], BF16) for i in range(2)]
            VAs = K.sb('VAs', [128, NKT, 260], BF16)
            K.dma('sp', VAs[:, :, :], V(VA[kind].apx.rearrange('k p f -> p k f'), ['VA%s_all' % kind]))
            Qs = [K.sb('Qs%d' % i, [128, 4 if kind == 'mla' else 2, 512], BF16) for i in range(2)]
            if kind == 'mla':
                d = 96
                KTs = K.sb('KTs', [128, 4, TOK], BF16)
                K.dma('sp', KTs[:, :, :], V(KT['mla'].apx.rearrange('h p n -> p h n'), ['KTm_all']))
            elif kind == 'na':
                d = 64
                KTs = K.sb('KTs', [128, 2, TOK], BF16)
                K.dma('sp', KTs[:, :, :], V(KT['na'].apx.rearrange('h p n -> p h n'), ['KTn_all']))
                Bt = K.sb('Bt', [128, 20, 640], F32)
                Mt = K.sb('Mt', [128, 5, 640], F32)
                K.dma('sp', Bt[:, :, :], V(w['nab'].apx.rearrange('c h p n -> p (c h) n'), ['nab']))
                K.dma('sp', Mt[:, :, :], V(namask_d.apx.rearrange('c p n -> p c n'), ['namask']))
                for cs in range(5):
                    for h in range(4):
                        K.tt('dve', Bt[:, cs * 4 + h, :], Bt[:, cs * 4 + h, :], Mt[:, cs, :], ALU.add)
                sbs = [K.sb('sbs%d' % i, [128, 640], F32) for i in range(2)]
            else:
                d = 32
                KPs = K.sb('KPs', [128, 4, TOK], BF16)
                K.memset('pool', KPs[:, :, :], 0.0)
                for c in range(2):
                    for hh in range(2):
                        for m in range(2):
                            r0 = 64 * hh + 32 * m
                            K.dma('sp', KPs[r0:r0 + 32, c * 2 + m, :], V(KT['diff'].apx[c, r0:r0 + 32, :], ['KTd_all']))
                sub_g = K.sb('sub_g', [64, 1], F32)
                K.ts('dve', sub_g[:, :], vec('sub_g', 0, 64), 1.0 - lam_init, None, ALU.mult)
                dt0 = K.sb('dt0', [64, 512], F32)
                dt1 = K.sb('dt1', [64, 512], F32)
                dsq = K.sb('dsq', [64, 512], F32)
            scale = float(d) ** -0.5
            K.memset('pool', nshift[:, :], -math.sqrt(d))
            S.barrier()

            def qk_ops(h, m, Q):
                if kind == 'mla':
                    return (lambda kt: KTs[0:96, h, kt * 128:(kt + 1) * 128]), (lambda a, b_: Q[0:96, h, a:b_])
                c, hh = divmod(h, 2)
                r0 = 64 * hh
                if kind == 'na':
                    return (lambda kt: KTs[r0:r0 + 64, c, kt * 128:(kt + 1) * 128]), (lambda a, b_: Q[r0:r0 + 64, c, a:b_])
                return (lambda kt: KPs[r0:r0 + 64, c * 2 + m, kt * 128:(kt + 1) * 128]), (lambda a, b_: Q[r0:r0 + 64, c, a:b_])

            def dense_map(h, m, Q, N, kts, Ops):
                kf, qf = qk_ops(h, m, Q)
                LA = min(NPS - 2, len(kts) - 1)
                for a_ in range(LA + 1):
                    K.mm(psS[(sct[0] + a_) % NPS][:, 0:N], kf(kts[a_]), qf(0, N))
                for ii, kt in enumerate(kts):
                    ps = psS[(sct[0] + ii) % NPS]
                    if ii + LA + 1 < len(kts):
                        K.mm(psS[(sct[0] + ii + LA + 1) % NPS][:, 0:N], kf(kts[ii + LA + 1]), qf(0, N))
                    P = Pt[(pct[0] + ii) % len(Pt)]
                    K.act(P[:, 0:N], ps[:, 0:N], AF.Exp, bias=nshift[:, 0:1], scale=scale)
                    K.mm(Ops[0:65, 0:N], VAs[:, kt, h * 65:(h + 1) * 65], P[:, 0:N], start=(ii == 0), stop=(ii == len(kts) - 1))
                    if ii == min(2, len(kts) - 1) and apend:
                        apend.pop(0)()
                sct[0] += len(kts)
                pct[0] += len(kts)

            bci = [0]
            apend = []
            sct = [0]
            pct = [0]

            def bcast_recip(Ops, N, mult=None):
                i = bci[0] % 2
                bci[0] += 1
                r = rcp[i]
                K.recip(r[64:65, 0:N], Ops[64:65, 0:N])
                if mult is not None:
                    K.ts('dve', r[64:65, 0:N], r[64:65, 0:N], mult, None, ALU.mult)
                K.mm(psB[0:64, 0:N], cc('ones', 64, 65, 0, 64), r[64:65, 0:N])
                K.copy('act', bcs[i][:, 0:N], psB[0:64, 0:N])
                return bcs[i]

            ri = [0]

            def store_head(h, c0, N, resv, bi):
                K.dma('sp', V(OT[kind].apx[h * 64:(h + 1) * 64, c0:c0 + N], [('OT' + kind, bi, h)]), resv)

            blocks = ([(0, 256, [0, 1])] if not last else []) + [(256 + 512 * i, 512, list(range(NKT))) for i in range(8)]
            for bi, (c0, N, kts) in enumerate(blocks):
                Q = Qs[bi % 2]
                K.dma('sp', Q[:, :, 0:N], V(QT[kind].apx[:, :, c0:c0 + N].rearrange('h p n -> p h n'), ['QT%s_all' % kind]))
                if kind == 'na' and N == 512:
                    while apend:
                        apend.pop(0)()
                    for sb_ in range(4):
                        i = (c0 - 256) // 128 + sb_
                        js = na_slots(i)
                        cs = na_case(i)
                        qa, qb = sb_ * 128, (sb_ + 1) * 128
                        for h in range(4):
                            kf, qf = qk_ops(h, 0, Q)
                            P = Pt[h % 2]
                            SB = sbs[h % 2]
                            for ii, kt in enumerate([0, 1]):
                                K.mm(psS[0][:, ii * 128:(ii + 1) * 128], kf(kt), qf(qa, qb))
                            for ii, j in enumerate(js):
                                pp = psS[1] if ii < 4 else psS[2]
                                K.mm(pp[:, (ii % 4) * 128:(ii % 4 + 1) * 128], kf(2 + j), qf(qa, qb))
                            K.stt('dve', SB[:, 0:512], psS[1][:, 0:512], scale, Bt[:, cs * 4 + h, 0:512], ALU.mult, ALU.add)
                            K.stt('dve', SB[:, 512:640], psS[2][:, 0:128], scale, Bt[:, cs * 4 + h, 512:640], ALU.mult, ALU.add)
                            K.act(P[:, 0:256], psS[0][:, 0:256], AF.Exp, bias=nshift[:, 0:1], scale=scale)
                            K.act(P[:, 256:896], SB[:, 0:640], AF.Exp, bias=nshift[:, 0:1], scale=1.0)
                            ktl = [0, 1] + [2 + j for j in js]
                            for ii, kt in enumerate(ktl):
                                K.mm(psO[h][0:65, qa:qb], VAs[:, kt, h * 65:(h + 1) * 65], P[:, ii * 128:(ii + 1) * 128],
                                     start=(ii == 0), stop=(ii == 6), skip_group_check=True)
                    for h in range(4):
                        bc = bcast_recip(psO[h], N)
                        res = resb[h % 2]
                        K.tt('dve', res[:, 0:N], psO[h][0:64, 0:N], bc[:, 0:N], ALU.mult)
                        store_head(h, c0, N, res[:, 0:N], bi)
                    continue
                for h in range(4):
                    if kind == 'diff':
                        O0, O1 = psO[0], psO[1]
                        dense_map(h, 0, Q, N, kts, O0)
                        dense_map(h, 1, Q, N, kts, O1)

                        def fin(h=h, O0=O0, O1=O1, N=N, c0=c0, bi=bi):
                            res = resb[h % 2]
                            bc0 = bcast_recip(O0, N)
                            bc1 = bcast_recip(O1, N, mult=lamt[64:65, 0:1])
                            K.tt('dve', dt0[:, 0:N], O0[0:64, 0:N], bc0[:, 0:N], ALU.mult)
                            K.tt('dve', dt1[:, 0:N], O1[0:64, 0:N], bc1[:, 0:N], ALU.mult)
                            K.tt('dve', dt0[:, 0:N], dt0[:, 0:N], dt1[:, 0:N], ALU.add)
                            K.act(dsq[:, 0:N], dt0[:, 0:N], AF.Square)
                            K.mm(psB[0:64, 0:N], cc('ones', 0, 64, 0, 64), dsq[:, 0:N])
                            K.act(dt1[:, 0:N], psB[0:64, 0:N], AF.Sqrt, bias=vecs_eps2[0:64, 0:1], scale=1.0 / 64)
                            K.recip(dt1[:, 0:N], dt1[:, 0:N])
                            K.stt('dve', res[:, 0:N], dt0[:, 0:N], sub_g[:, 0:1], dt1[:, 0:N], ALU.mult, ALU.mult)
                            store_head(h, c0, N, res[:, 0:N], bi)
                    else:
                        Ops = psO[h % 2]
                        dense_map(h, 0, Q, N, kts, Ops)

                        def fin(h=h, Ops=Ops, N=N, c0=c0, bi=bi):
                            res = resb[h % 2]
                            bc = bcast_recip(Ops, N)
                            K.tt('dve', res[:, 0:N], Ops[0:64, 0:N], bc[:, 0:N], ALU.mult)
                            store_head(h, c0, N, res[:, 0:N], bi)
                    if kind == 'diff':
                        fin()
                    else:
                        apend.append(fin)
            while apend:
                apend.pop(0)()
            end_phase(st)

        for kind in ['mla', 'diff', 'na']:
            if stop_after == 'C':
                break
            attention(kind)
        if dbg and l == 0:
            S.barrier()
            for (dn, src) in [('d_OTconv', OT['conv']), ('d_OTmla', OT['mla']), ('d_OTdiff', OT['diff']), ('d_OTna', OT['na'])]:
                K.dma('sp', V(dbg_out[dn].apx, [dn]), V(src.apx, [src.name + '_all']), is_out=True)
            S.barrier()
        if stop_after in ('C', 'ATT'):
            break

        st = phase()
        bo = K.sb('bo', [128, 8, 1024], BF16)
        K.dma('pool', bo[:, :, :], V(w['bouts'].apx.rearrange('b (k p) n -> p (b k) n', p=128), ['bouts']))
        gwm = [K.sb('gwm%d' % m, [128, 8, 512], BF16) for m in range(8)]
        for m in range(8):
            K.dma('pool', gwm[m][:, :, :], V(w['gate_w'].apx[:, m * 512:(m + 1) * 512].rearrange('(k p) n -> p k n', p=128), ['gate_w']))
        wo = K.sb('wo', [128, 8, 1024], BF16)
        K.dma('pool', wo[:, :, :], V(w['w_o'].apx.rearrange('(k p) n -> p k n', p=128), ['w_o']))
        rw = K.sb('rw', [128, 8, 32], F32)
        K.dma('sp', rw[:, :, :], V(w['router_w'].apx.rearrange('(k p) n -> p k n', p=128), ['router_w']))
        Hm = K.sb('Hm', [128, 8, 512], BF16)
        B4 = K.sb('B4', [128, 8, 512], BF16)
        Xm = K.sb('Xm', [128, 8, 512], F32)
        X1 = K.sb('X1', [128, 8, 512], F32)
        ym = K.sb('ym', [128, 8, 512], BF16)
        H2f = K.sb('H2f', [128, 8, 512], F32)
        H2b = K.sb('H2b', [128, 8, 512], BF16)
        mt = [K.sb('mt%d' % i, [128, 512], F32) for i in range(6)]
        macc = K.sb('macc', [128, 512], F32)
        rstd2_t = K.sb('rstd2_t', [128, 512], F32)
        mpend = []
        meps = K.sb('meps', [128, 1], F32)
        K.memset('pool', meps[:, :], EPS)
        gTt = K.sb('gTt', [32, 512], F32)
        rlg = [K.sb('rlg%d' % i, [128, 32], F32) for i in range(4)]
        rmsk = [K.sb('rmsk%d' % i, [128, 32], F32) for i in range(4)]
        rex = [K.sb('rex%d' % i, [128, 32], F32) for i in range(4)]
        rgt = [K.sb('rgt%d' % i, [128, 32], F32) for i in range(4)]
        rr8 = [K.sb('rr8%d' % i, [128, 8], F32) for i in range(4)]
        rrs = [K.sb('rrs%d' % i, [128, 4], F32) for i in range(4)]
        mp = [K.ps('mp%d' % i, [128, 512], F32) for i in range(8)]
        mpi = [0]

        def mnext():
            p = mp[mpi[0] % 8]
            mpi[0] += 1
            return p
        mti = [0]

        def mnt():
            t = mt[mti[0] % 6]
            mti[0] += 1
            return t
        S.barrier(skip_pool_dma=True)
        for ti, (c0, N) in enumerate(tiles):
            if last and ti == 0:
                continue
            col = 1 if ti == 0 else 0
            K.dma('sp', Hm[:, :, 0:N], V(hT.apx[:, c0:c0 + N].rearrange('(k p) n -> p k n', p=128), ['hT_all']))
            for bi_, nm in enumerate(['conv', 'mla', 'diff', 'na']):
                K.dma('sp', B4[:, bi_ * 2:bi_ * 2 + 2, 0:N], V(OT[nm].apx[:, c0:c0 + N].rearrange('(k p) n -> p k n', p=128), ['OT_all' + nm]))
            K.dma('sp', Xm[:, :, 0:N], V(res_in.apx[:, c0:c0 + N].rearrange('(k p) n -> p k n', p=128), res_in[:, :].keys))
            for m in range(8):
                if m == 1 and mpend:
                    mpend.pop(0)()
                tms = []
                for b_ in range(4):
                    pg = mnext()
                    for k in range(8):
                        K.mm(pg[:, 0:N], gwm[m][:, k, b_ * 128:(b_ + 1) * 128], Hm[:, k, 0:N], start=(k == 0), stop=(k == 7))
                    sg = mnt()
                    K.act(sg[:, 0:N], pg[:, 0:N], AF.Sigmoid, bias=vec('gate_b', b_ * 8 + m), scale=1.0)
                    py = mnext()
                    for k in range(2):
                        K.mm(py[:, 0:N], bo[:, b_ * 2 + k, m * 128:(m + 1) * 128], B4[:, b_ * 2 + k, 0:N], start=(k == 0), stop=(k == 1))
                    K.tt('dve', sg[:, 0:N], sg[:, 0:N], py[:, 0:N], ALU.mult)
                    tms.append(sg)
                K.tt('dve', tms[0][:, 0:N], tms[0][:, 0:N], tms[1][:, 0:N], ALU.add)
                K.tt('dve', tms[2][:, 0:N], tms[2][:, 0:N], tms[3][:, 0:N], ALU.add)
                K.tt('dve', ym[:, m, 0:N], tms[0][:, 0:N], tms[2][:, 0:N], ALU.add)
            if 'mg' not in NOBAR:
                S.barrier()
            for m2 in range(8):
                po = mnext()
                for m in range(8):
                    K.mm(po[:, 0:N], wo[:, m, m2 * 128:(m2 + 1) * 128], ym[:, m, 0:N], start=(m == 0), stop=(m == 7))
                K.stt('dve', X1[:, m2, 0:N], po[:, 0:N], modv(16 + m2, col), Xm[:, m2, 0:N], ALU.mult, ALU.add)
            K.dma('sp', V(res1.apx[:, c0:c0 + N].rearrange('(k p) n -> p k n', p=128), [('res1', ti)]), X1[:, :, 0:N])
            def post(ti=ti, c0=c0, N=N, col=col):
                pst = mnext()
                for k in range(8):
                    sq_ = mnt()
                    K.act(sq_[:, 0:N], X1[:, k, 0:N], AF.Square)
                    K.mm(pst[:, 0:N], cc('ones'), sq_[:, 0:N], start=(k == 0), stop=(k == 7))
                r_ = mnt()
                K.act(r_[:, 0:N], pst[:, 0:N], AF.Sqrt, bias=meps[:, 0:1], scale=1.0 / D)
                rstd2 = rstd2_t
                K.recip(rstd2[:, 0:N], r_[:, 0:N])
                for k in range(8):
                    t_ = mnt()
                    K.stt('dve', t_[:, 0:N], X1[:, k, 0:N], A2[:, 2 * k + col:2 * k + col + 1], rstd2[:, 0:N], ALU.mult, ALU.mult)
                    K.act(H2f[:, k, 0:N], t_[:, 0:N], AF.Identity, bias=modv(24 + k, col), scale=1.0)
                    K.copy('dve', H2b[:, k, 0:N], H2f[:, k, 0:N])
                K.dma('sp', V(h2T.apx[:, c0:c0 + N].rearrange('(k p) n -> p k n', p=128), [('h2T', ti)]), H2b[:, :, 0:N])
                if 'mg' not in NOBAR:
                    S.barrier()
                nsb = N // 128
                prs = []
                for sb_ in range(nsb):
                    pr = mnext()
                    for k in range(8):
                        K.mm(pr[:, 0:32], H2f[:, k, sb_ * 128:(sb_ + 1) * 128], rw[:, k, :], start=(k == 0), stop=(k == 7))
                    prs.append(pr)
                for sb_ in range(nsb):
                    K.tt('dve', rlg[sb_][:, :], prs[sb_][:, 0:32], rowsb[:, 64:96], ALU.add)
                for sb_ in range(nsb):
                    K.vmax8(rr8[sb_][:, :], rlg[sb_][:, :])
                for sb_ in range(nsb):
                    K.ts('dve', rmsk[sb_][:, :], rlg[sb_][:, :], rr8[sb_][:, 3:4], None, ALU.is_ge)
                    K.ts('dve', rrs[sb_][:, 0:1], rr8[sb_][:, 0:1], -1.0, None, ALU.mult)
                for sb_ in range(nsb):
                    K.act(rex[sb_][:, :], rlg[sb_][:, :], AF.Exp, bias=rrs[sb_][:, 0:1], scale=1.0)
                for sb_ in range(nsb):
                    K.tt('dve', rex[sb_][:, :], rex[sb_][:, :], rmsk[sb_][:, :], ALU.mult)
                for sb_ in range(nsb):
                    K.S.add('dve', (lambda o, i: (lambda e: e.tensor_reduce(o, i, AX.X, ALU.add)))(rrs[sb_][:, 1:2].ap, rex[sb_][:, :].ap), rex[sb_][:, :].keys, rrs[sb_][:, :].keys)
                for sb_ in range(nsb):
                    K.recip(rrs[sb_][:, 2:3], rrs[sb_][:, 1:2])
                for sb_ in range(nsb):
                    K.ts('dve', rgt[sb_][:, :], rex[sb_][:, :], rrs[sb_][:, 2:3], None, ALU.mult)
                pts = []
                for sb_ in range(nsb):
                    pt_ = mnext()
                    K.tr(pt_[0:32, 0:128], rgt[sb_][:, :], cc('identf'))
                    pts.append(pt_)
                for sb_ in range(nsb):
                    K.copy('dve', gTt[:, sb_ * 128:(sb_ + 1) * 128], pts[sb_][0:32, 0:128])
                K.dma('sp', V(gT.apx[:, c0:c0 + N], [('gT', ti)]), gTt[:, 0:N])

            mpend.append(post)
            if 'mg' not in NOBAR:
                S.barrier()
        while mpend:
            mpend.pop(0)()
        S.barrier()
        if dbg and l == 0:
            for (dn, src) in [('d_res1', res1), ('d_h2T', h2T), ('d_gT', gT)]:
                K.dma('sp', V(dbg_out[dn].apx, [dn]), V(src.apx, [src.name + '_all']), is_out=True)
        end_phase(st)
        if stop_after == 'M':
            break

        st = phase()
        bd = K.sb('bd', [32, 1024], F32)
        K.dma('sp', bd[:, :], w['b_dn'][:, :])
        o_bg, _ = VC['bg']
        bup = K.sb('bup', [128, 512], F32)
        K.ts('dve', bup[:, :], vecs[:, o_bg:o_bg + 512], 1.0, None, ALU.add)
        EB = 1088
        H2 = K.sb('H2', [128, 8, EB], BF16)
        gts = K.sb('gts', [32, EB], F32)
        acc = K.sb('eacc', [128, 8, EB], F32)
        Wg = [K.sb('Wg%d' % i, [128, 8, 2048], BF16) for i in range(2)]
        Wd = [K.sb('Wd%d' % i, [128, 8, 1024], BF16) for i in range(2)]
        G = [K.sb('G%d' % i, [128, EB], F32) for i in range(2)]
        At = [K.sb('At%d' % i, [128, 8, 512], BF16) for i in range(2)]
        et = [K.sb('et%d' % i, [128, 512], F32) for i in range(6)]
        ep = [K.ps('ep%d' % i, [128, 512], F32) for i in range(8)]
        epi = [0]

        def enext():
            p = ep[epi[0] % 8]
            epi[0] += 1
            return p
        eti = [0]

        def ent():
            t = et[eti[0] % 6]
            eti[0] += 1
            return t
        S.barrier()
        eblocks = ([(256 + 1024 * i, 1024) for i in range(4)] if last else [(1088 * i, 1088) for i in range(4)])
        ai = 0
        for (c0, Nb) in eblocks:
            btiles = ([(0, 512), (512, 512)] if Nb == 1024 else ([(0, 256), (256, 416), (672, 416)] if c0 == 0 else [(0, 364), (364, 362), (726, 362)]))
            K.dma('sp', H2[:, :, 0:Nb], V(h2T.apx[:, c0:c0 + Nb].rearrange('(k p) n -> p k n', p=128), ['h2T_all']))
            K.dma('sp', gts[:, 0:Nb], V(gT.apx[:, c0:c0 + Nb], ['gT_all']))
            for m in range(8):
                for (t0, N) in btiles:
                    p = enext()
                    K.mm(p[:, 0:N], bd[0:32, m * 128:(m + 1) * 128], gts[0:32, t0:t0 + N])
                    K.copy('dve', acc[:, m, t0:t0 + N], p[:, 0:N])
            def load_w(e):
                K.dma('pool', Wg[e % 2][:, :, :], V(w['w_gu'].apx[e].rearrange('(k p) n -> p k n', p=128), ['w_gu']))
                K.dma('pool', Wd[e % 2][:, :, :], V(w['w_dn'].apx[e].rearrange('(k p) n -> p k n', p=128), ['w_dn']))
            load_w(0)
            pend = []
            for e in range(NE):
                WG, WD, GE = Wg[e % 2], Wd[e % 2], G[e % 2]
                K.dma('sp', GE[:, 0:Nb], V(gT.apx[e:e + 1, c0:c0 + Nb].partition_broadcast(128), ['gT_all']))
                K.act(GE[:, 0:Nb], GE[:, 0:Nb], AF.Copy, scale=1.0 / 1.702)
                for (t0, N) in btiles:
                    A = At[ai % 2]
                    ai += 1
                    for j in range(8):
                        pg = enext()
                        for k in range(8):
                            K.mm(pg[:, 0:N], WG[:, k, j * 128:(j + 1) * 128], H2[:, k, t0:t0 + N], start=(k == 0), stop=(k == 7))
                        pu = enext()
                        for k in range(8):
                            K.mm(pu[:, 0:N], WG[:, k, 1024 + j * 128:1024 + (j + 1) * 128], H2[:, k, t0:t0 + N], start=(k == 0), stop=(k == 7))
                        tg, sl, tu = ent(), ent(), ent()
                        tu2, p1 = tu, sl
                        K.ts('dve', tg[:, 0:N], pg[:, 0:N], vecs[:, o_bg + e * 16 + j:o_bg + e * 16 + j + 1], 7.0, ALU.add, ALU.min)
                        K.act(sl[:, 0:N], tg[:, 0:N], AF.Silu, scale=1.702)
                        K.act(tu[:, 0:N], pu[:, 0:N], AF.Identity, bias=bup[:, e * 16 + 8 + j:e * 16 + 8 + j + 1], scale=1.0)
                        K.ts('dve', tu2[:, 0:N], tu[:, 0:N], 8.0, -6.0, ALU.min, ALU.max)
                        K.tt('dve', p1[:, 0:N], sl[:, 0:N], tu2[:, 0:N], ALU.mult)
                        K.tt('dve', A[:, j, 0:N], p1[:, 0:N], GE[:, t0:t0 + N], ALU.mult)
                        if j == 1:
                            if pend:
                                pend.pop(0)()
                            if t0 == 0 and e + 1 < NE:
                                load_w(e + 1)
                    def down(A=A, WD=WD, t0=t0, N=N):
                        for m in range(8):
                            py = enext()
                            for j in range(8):
                                K.mm(py[:, 0:N], WD[:, j, m * 128:(m + 1) * 128], A[:, j, 0:N], start=(j == 0), stop=(j == 7))
                            K.tt('dve', acc[:, m, t0:t0 + N], acc[:, m, t0:t0 + N], py[:, 0:N], ALU.add)
                    pend.append(down)
                if e % 4 == 3 or e == NE - 1:
                    while pend:
                        pend.pop(0)()
                if e % 4 == 3:
                    S.barrier(skip_pool_dma=True)
            for m in range(8):
                for (t0, N) in btiles:
                    col = 1 if (c0 + t0) < 256 else 0
                    xo = ent()
                    K.dma('sp', xo[:, 0:N], V(res1.apx[m * 128:(m + 1) * 128, c0 + t0:c0 + t0 + N], ['res1_all']))
                    K.stt('dve', xo[:, 0:N], acc[:, m, t0:t0 + N], modv(40 + m, col), xo[:, 0:N], ALU.mult, ALU.add)
                    if last:
                        K.dma('sp', V(outT.apx[m * 128:(m + 1) * 128, c0 + t0 - 256:c0 + t0 - 256 + N], [('outT', m, c0, t0)]), xo[:, 0:N], is_out=True)
                    else:
                        K.dma('sp', V(res2.apx[m * 128:(m + 1) * 128, c0 + t0:c0 + t0 + N], [('res2', m, c0, t0)]), xo[:, 0:N])
            S.barrier()
        if dbg and l == 0:
            K.dma('sp', V(dbg_out['d_res2'].apx, ['d_res2']), V(res2.apx, ['res2_all']), is_out=True)
        end_phase(st)
        S.barrier()
        lst.close()
    K.emit()
    return nc


OFF_B, OFF_C, OFF_D = 512, 864, 1632


def _pk(v, n):
    return np.ascontiguousarray(np.asarray(v, np.float32).reshape(n, 128).T)


def prep_shared(inp, layers=DEPTH):
    sh = {}
    c, cb = make_consts()
    sh['consts'] = c
    sh['constb'] = cb
    sh['rope'] = rope_tables()
    idx_r, idx_c, mask = na_tables_cached()
    sh['namask'] = np.ascontiguousarray(mask.reshape(5, 128, 640))
    for l in range(layers):
        sh['ada_w%d' % l] = np.ascontiguousarray(inp['ada_w'][l])
        vec = np.zeros((128, NVC), np.float32)

        def put(name, arr):
            o, wd = VC[name]
            arr = np.asarray(arr, np.float32)
            vec[:arr.shape[0], o:o + arr.shape[1]] = arr
        ab = _pk(inp['ada_b'][l], 48)
        put('ada_b', np.repeat(ab, 2, axis=1))
        put('n1g', _pk(inp['norm1_g'][l], 8))
        put('n2g', _pk(inp['norm2_g'][l], 8))
        cw = inp['conv_w'][l]
        put('conv_w', np.concatenate([cw[:, 0:128].T, cw[:, 128:256].T], axis=1))
        put('conv_b', _pk(inp['conv_b'][l], 2))
        put('cln_g', _pk(inp['conv_ln_g'][l], 2))
        put('cln_b', _pk(inp['conv_ln_b'][l], 2))
        cq = np.zeros((128, 2), np.float32)
        cq[:, 0] = inp['mla_cq_g'][l][0:128]
        cq[0:64, 1] = inp['mla_cq_g'][l][128:192]
        put('cq_g', cq)
        put('ckv_g', inp['mla_ckv_g'][l][:, None])
        put('mqn_g', inp['mla_qn_g'][l][:, None])
        put('mkn_g', inp['mla_kn_g'][l][:, None])
        put('dqn_g', np.tile(inp['diff_qn_g'][l], 4)[:, None])
        put('dkn_g', np.tile(inp['diff_kn_g'][l], 4)[:, None])
        put('nqn_g', np.tile(inp['na_qn_g'][l], 2)[:, None])
        put('nkn_g', np.tile(inp['na_kn_g'][l], 2)[:, None])
        put('gate_b', _pk(inp['gate_b'][l], 32))
        put('sub_g', inp['diff_subln_g'][l][:, None])
        bgu = inp['exp_b_gu'][l]
        put('bg', np.ascontiguousarray(bgu.reshape(32, 16, 128).transpose(2, 0, 1).reshape(128, 512)))
        sh['vecs%d' % l] = vec
        sh['rows%d' % l] = np.concatenate([inp['diff_subln_g'][l], inp['router_b'][l], inp['diff_lam'][l].reshape(-1)])[None, :].astype(np.float32)
        wi = inp['w_in'][l]
        z64 = np.zeros((D, 64), np.float32)
        cols = [wi[:, 0:512], wi[:, 512:640], wi[:, 640:704], z64, wi[:, 704:832]]
        for off in (0, 64):
            for base in (OFF_C, OFF_D):
                for hp in range(2):
                    cols.append(np.concatenate([wi[:, base + h * 192 + off: base + h * 192 + off + 64] for h in (2 * hp, 2 * hp + 1)], axis=1))
        c5 = cols[5:]
        cols = cols[:5] + [c5[0], c5[1], c5[4], c5[5], c5[2], c5[3], c5[6], c5[7]]
        cols += [z64, wi[:, 832:864]]
        for base in (OFF_C, OFF_D):
            cols.append(np.concatenate([wi[:, base + h * 192 + 128: base + h * 192 + 192] for h in range(4)], axis=1))
        wr = np.ascontiguousarray(np.concatenate(cols, axis=1))
        assert wr.shape == (D, 2528), wr.shape
        sh['w_in%d' % l] = wr
        sh['w_uq%d' % l] = np.ascontiguousarray(inp['mla_w_uq'][l])
        wk = inp['mla_w_ukv'][l]
        z32 = np.zeros((128, 32), np.float32)
        kc = []
        for h in range(4):
            kc += [wk[:, h * 128:h * 128 + 64], z32]
        vc = [wk[:, h * 128 + 64:h * 128 + 128] for h in range(4)]
        sh['w_ukv%d' % l] = np.ascontiguousarray(np.concatenate(kc + vc, axis=1))
        sh['bouts%d' % l] = np.ascontiguousarray(np.stack([inp['conv_out'][l], inp['mla_out'][l], inp['diff_out'][l], inp['na_out'][l]], 0))
        sh['gate_w%d' % l] = np.ascontiguousarray(inp['gate_w'][l].reshape(D, 4, 8, 128).transpose(0, 2, 1, 3).reshape(D, 4 * D))
        sh['w_o%d' % l] = np.ascontiguousarray(inp['w_o'][l])
        sh['router_w%d' % l] = np.ascontiguousarray(inp['router_w'][l])
        sh['w_gu%d' % l] = np.ascontiguousarray(inp['exp_w_gu'][l])
        sh['w_dn%d' % l] = np.ascontiguousarray(inp['exp_w_down'][l])
        sh['b_dn%d' % l] = np.ascontiguousarray(inp['exp_b_down'][l])
        rpb = inp['na_rpb'][l]
        g = rpb[:, idx_r, idx_c]
        sh['nab%d' % l] = np.ascontiguousarray(g.transpose(1, 0, 2, 3, 4).reshape(5, 4, 128, 640)).astype(np.float32)
    return sh


def prep_core(inp, b):
    m = {}
    m['xT'] = np.ascontiguousarray(np.concatenate([inp['ctx'][b].T, inp['x'][b].T], axis=1)).astype(np.float32)
    cT = np.zeros((128, 16), np.float32)
    cT[:, 0::2] = _pk(inp['c'][b], 8)
    cT[:, 1::2] = _pk(inp['c_ctx'], 8)
    m['cT'] = cT
    return m


_NC_CACHE = {}


def kernel(**inputs):
    inp = {k: np.asarray(v) for k, v in inputs.items()}
    if 'nc' not in _NC_CACHE:
        _NC_CACHE['nc'] = build()
    nc = _NC_CACHE['nc']
    sh = prep_shared(inp)
    in_maps = []
    for b in range(8):
        m = dict(sh)
        m.update(prep_core(inp, b))
        in_maps.append(m)
    res = run_bass_kernel_spmd(nc, in_maps, core_ids=list(range(8)))
    out = np.stack([np.ascontiguousarray(res.results[b]['outT'].T) for b in range(8)], 0)
    return out.astype(np.float32)
```

```python
import math
from contextlib import ExitStack
import numpy as np
import ml_dtypes
import concourse.bass as bass
import concourse.mybir as mybir
from concourse.bass_utils import run_bass_kernel_spmd

F32 = mybir.dt.float32
BF16 = mybir.dt.bfloat16
AF = mybir.ActivationFunctionType
ALU = mybir.AluOpType
AX = mybir.AxisListType

D = 1024
SEQ = 4096
CTX = 256
TOK = SEQ + CTX
NE = 32
DFF = 1024
EPS = 1e-6
DEPTH = 2
NKT = TOK // 128

ENGS = ['pe', 'act', 'dve', 'pool', 'sp']
import os as _os
NOBAR = _os.environ.get('NOBAR', 'p1,cv,na,at,mg').split(',')
POOLP1 = _os.environ.get('POOLP1', '0') == '1'
NDSEM = 8


class V:
    __slots__ = ('ap', 'keys')

    def __init__(self, ap, keys):
        self.ap = ap
        self.keys = keys


class Buf:
    def __init__(self, name, ap, nsub=0):
        self.name = name
        self.apx = ap
        self.nsub = nsub

    def __getitem__(self, idx):
        keys = [self.name] if self.nsub == 0 else [(self.name, i) for i in range(self.nsub)]
        return V(self.apx[idx], keys)

    def s(self, i, idx=None):
        ap = self.apx if idx is None else self.apx[idx]
        return V(ap, [(self.name, i)])


class Sched:
    def __init__(self):
        self.ops = {e: [] for e in ENGS}
        self.ccnt = {e: 0 for e in ENGS}
        self.dcnt = {e: 0 for e in ENGS}
        self.last_w = {}
        self.readers = {}
        self.waited = {e: {} for e in ENGS}
        self.semmax = {}
        self.out_tokens = []

    def _need(self, eng, tok, waits):
        sem, val, teng, is_dma = tok
        if (not is_dma) and teng == eng and eng == 'pe':
            return
        if self.waited[eng].get(sem, 0) >= val:
            return
        self.waited[eng][sem] = val
        waits.append((sem, val))

    def add(self, eng, fn, reads, writes, dma=False, is_out=False):
        waits = []
        if dma:
            i = self.dcnt[eng]
            self.dcnt[eng] += 1
            sem = 'd_%s_%d' % (eng, i % NDSEM)
            val = 16 * (i // NDSEM + 1)
            if i >= NDSEM:
                self._need(eng, (sem, val - 16, eng, True), waits)
            tok = (sem, val, eng, True)
            inc = (sem, 16)
        else:
            self.ccnt[eng] += 1
            sem = 'c_' + eng
            tok = (sem, self.ccnt[eng], eng, False)
            inc = (sem, 1)
        self.semmax[sem] = tok[1]
        for k in reads:
            t = self.last_w.get(k)
            if t is not None:
                self._need(eng, t, waits)
        for k in writes:
            t = self.last_w.get(k)
            if t is not None:
                self._need(eng, t, waits)
            for (rs, (rv, re, rd)) in self.readers.get(k, {}).items():
                self._need(eng, (rs, rv, re, rd), waits)
        for k in writes:
            self.last_w[k] = tok
            self.readers[k] = {}
        for k in reads:
            r = self.readers.setdefault(k, {})
            r[tok[0]] = (tok[1], tok[2], tok[3])
        self.ops[eng].append((fn, waits, inc))
        if is_out:
            self.out_tokens.append(tok)

    def barrier(self, skip_pool_dma=False):
        for e in ENGS:
            waits = []
            for sem, val in self.semmax.items():
                if skip_pool_dma and sem.startswith('d_pool'):
                    continue
                if self.waited[e].get(sem, 0) < val:
                    self.waited[e][sem] = val
                    waits.append((sem, val))
            if waits:
                self.ops[e].append((None, waits, None))
        if not skip_pool_dma:
            self.last_w = {}
            self.readers = {}


class Ctx:
    def __init__(self, nc):
        self.nc = nc
        self.S = Sched()
        self.n_in = {}
        self.stack = None

    def dram_in(self, name, shape, dtype=F32, nsub=0):
        t = self.nc.dram_tensor(name, list(shape), dtype, kind="ExternalInput").ap()
        return Buf(name, t, nsub)

    def dram_out(self, name, shape, dtype=F32, nsub=0):
        t = self.nc.dram_tensor(name, list(shape), dtype, kind="ExternalOutput").ap()
        return Buf(name, t, nsub)

    def dram(self, name, shape, dtype, nsub=0):
        t = self.nc.dram_tensor(name, list(shape), dtype, kind="Internal").ap()
        return Buf(name, t, nsub)

    def sb(self, name, shape, dtype, nsub=0):
        self.uid = getattr(self, 'uid', 0) + 1
        name = 'sb%d_%s' % (self.uid, name)
        t = self.stack.enter_context(self.nc.sbuf_tensor(name, list(shape), dtype))
        return Buf(name, t, nsub)

    def ps(self, name, shape, dtype=F32, nsub=0):
        self.uid = getattr(self, 'uid', 0) + 1
        name = 'ps%d_%s' % (self.uid, name)
        t = self.stack.enter_context(self.nc.psum_tensor(name, list(shape), dtype))
        return Buf(name, t, nsub)

    def _rk(self, *vs):
        ks = []
        for v in vs:
            if isinstance(v, V):
                ks += v.keys
        return ks

    def mm(self, out, lhsT, rhs, start=True, stop=True, **kw):
        o, l, r = out.ap, lhsT.ap, rhs.ap
        rd = self._rk(lhsT, rhs) + ([] if start else out.keys)
        self.S.add('pe', lambda e: e.matmul(o, l, r, start=start, stop=stop, **kw), rd, out.keys)

    def tr(self, out, in_, ident):
        o, i, d = out.ap, in_.ap, ident.ap
        self.S.add('pe', lambda e: e.transpose(o, i, d), self._rk(in_, ident), out.keys)

    def act(self, out, in_, func, bias=0.0, scale=1.0, accum=None):
        o, i = out.ap, in_.ap
        b = bias.ap if isinstance(bias, V) else bias
        s = scale.ap if isinstance(scale, V) else scale
        kw = {}
        wr = list(out.keys)
        if accum is not None:
            kw['accum_out'] = accum.ap
            wr += accum.keys
        self.S.add('act', lambda e: e.activation(o, i, func, bias=b, scale=s, **kw),
                   self._rk(in_, bias, scale), wr)

    def ts(self, eng, out, in0, s1, s2, op0, op1=None, accum=None):
        o, i = out.ap, in0.ap
        a = s1.ap if isinstance(s1, V) else s1
        b = s2.ap if isinstance(s2, V) else s2
        kw = {}
        wr = list(out.keys)
        if accum is not None:
            kw['accum_out'] = accum.ap
            wr += accum.keys
        if op1 is None:
            self.S.add(eng, lambda e: e.tensor_scalar(o, i, a, None, op0, **kw), self._rk(in0, s1), wr)
        else:
            self.S.add(eng, lambda e: e.tensor_scalar(o, i, a, b, op0, op1, **kw), self._rk(in0, s1, s2), wr)

    def tt(self, eng, out, in0, in1, op):
        o, a, b = out.ap, in0.ap, in1.ap
        self.S.add(eng, lambda e: e.tensor_tensor(o, a, b, op), self._rk(in0, in1), out.keys)

    def stt(self, eng, out, in0, scalar, in1, op0, op1):
        o, a, b = out.ap, in0.ap, in1.ap
        s = scalar.ap if isinstance(scalar, V) else scalar
        eng = 'dve'
        self.S.add(eng, lambda e: e.scalar_tensor_tensor(o, a, s, b, op0, op1), self._rk(in0, scalar, in1), out.keys)

    def copy(self, eng, out, in_):
        o, i = out.ap, in_.ap
        if eng == 'act':
            self.S.add(eng, lambda e: e.copy(o, i), in_.keys, out.keys)
        else:
            self.S.add(eng, lambda e: e.tensor_copy(o, i), in_.keys, out.keys)

    def memset(self, eng, out, val):
        o = out.ap
        self.S.add(eng, lambda e: e.memset(o, val), [], out.keys)

    def recip(self, out, in_):
        o, i = out.ap, in_.ap
        self.S.add('dve', lambda e: e.reciprocal(o, i), in_.keys, out.keys)

    def vmax8(self, out, in_):
        o, i = out.ap, in_.ap
        self.S.add('dve', lambda e: e.max(o, i), in_.keys, out.keys)

    def dma(self, q, out, in_, is_out=False):
        o, i = out.ap, in_.ap
        self.S.add(q, lambda e: e.dma_start(out=o, in_=i), in_.keys, out.keys, dma=True, is_out=is_out)

    def emit(self):
        nc, S = self.nc, self.S
        fw = []
        for (sem, val, e, d) in S.out_tokens:
            fw.append((sem, val))
        S.ops['sp'].append((None, fw, None))
        names = sorted(S.semmax.keys())
        with ExitStack() as st:
            sems = {n: st.enter_context(nc.semaphore(n)) for n in names}
            block = st.enter_context(nc.Block())

            def run(engname):
                def body(eng):
                    for (fn, waits, inc) in S.ops[engname]:
                        for (sn, v) in waits:
                            eng.wait_ge(sems[sn], v)
                        if fn is not None:
                            ins = fn(eng)
                            ins.then_inc(sems[inc[0]], inc[1])
                return body

            block.tensor(run('pe'))
            block.scalar(run('act'))
            block.vector(run('dve'))
            block.gpsimd(run('pool'))
            block.sync(run('sp'))


VC = {}
_o = 0
for _n, _w in [('ada_b', 96), ('n1g', 8), ('n2g', 8), ('conv_w', 62), ('conv_b', 2), ('cln_g', 2), ('cln_b', 2),
               ('cq_g', 2), ('ckv_g', 1), ('mqn_g', 1), ('mkn_g', 1), ('dqn_g', 1), ('dkn_g', 1),
               ('nqn_g', 1), ('nkn_g', 1), ('gate_b', 32), ('bg', 512), ('bu', 512), ('sub_g', 1)]:
    VC[_n] = (_o, _w)
    _o += _w
NVC = _o
CC = {}
_o = 0
for _n, _w in [('ones', 128), ('bd32', 128), ('bd64', 128), ('bd96', 128), ('identf', 128)]:
    CC[_n] = (_o, _w)
    _o += _w
NCC = _o
CB = {'ident': (0, 128), 'RM': (128, 128), 'RD': (256, 128)}
NCB = 384

TILES = [(0, 256)] + [(256 + 512 * i, 512) for i in range(8)]


def rope_tables():
    t = np.arange(SEQ)
    rows = (t // 64).astype(np.float32)
    cols = (t % 64).astype(np.float32)
    inv = (10000.0 ** (-np.arange(0, 16, 2, dtype=np.float32) / 16)).astype(np.float32)
    theta = np.concatenate([rows[:, None] * inv, cols[:, None] * inv], axis=-1).astype(np.float32)
    cos = np.cos(theta).astype(np.float32).T
    sin = np.sin(theta).astype(np.float32).T
    c32 = np.ones((32, TOK), np.float32)
    s32 = np.zeros((32, TOK), np.float32)
    c32[:16, CTX:] = cos
    c32[16:, CTX:] = cos
    s32[:16, CTX:] = sin
    s32[16:, CTX:] = sin
    cosM = np.ones((128, TOK), np.float32)
    sinM = np.zeros((128, TOK), np.float32)
    cosM[64:96] = c32
    sinM[64:96] = s32
    cosD = np.tile(c32, (4, 1))
    sinD = np.tile(s32, (4, 1))
    return np.stack([cosM, sinM, cosD, sinD], 0)


def rot_lhsT(n):
    m = np.zeros((128, 128), np.float32)
    for blk in n:
        for i in range(16):
            m[blk + i + 16, blk + i] = -1.0
            m[blk + i, blk + i + 16] = 1.0
    return m


def make_consts():
    c = np.zeros((128, NCC), np.float32)
    c[:, 0:128] = 1.0
    for b in range(4):
        c[b * 32:(b + 1) * 32, 128 + b * 32:128 + (b + 1) * 32] = 1.0
    for b in range(2):
        c[b * 64:(b + 1) * 64, 256 + b * 64:256 + (b + 1) * 64] = 1.0
    c[0:96, 384:384 + 96] = 1.0
    c[:, 512:640] = np.eye(128, dtype=np.float32)
    cb = np.zeros((128, NCB), np.float32)
    cb[:, 0:128] = np.eye(128)
    cb[:, 128:256] = rot_lhsT([64])
    cb[:, 256:384] = rot_lhsT([0, 32, 64, 96])
    return c, cb.astype(ml_dtypes.bfloat16)


def na_slots(i):
    rows = [2 * i, 2 * i + 1]
    need = set()
    for r in rows:
        rs = min(max(r - 4, 0), 56)
        for rr in range(rs, rs + 8):
            need.add(rr // 2)
    js = sorted(need)
    while len(js) < 5:
        js.append(js[0] if i >= 2 else js[-1])
    return js


def na_case(i):
    return {0: 0, 1: 1, 30: 3, 31: 4}.get(i, 2)


def na_tables():
    rep = {0: 0, 1: 1, 2: 5, 3: 30, 4: 31}
    idx_r = np.zeros((5, 128, 5, 128), np.int64)
    idx_c = np.zeros((5, 128, 5, 128), np.int64)
    mask = np.full((5, 128, 5, 128), -1e30, np.float32)
    for cs, i in rep.items():
        js = na_slots(i)
        seen = set()
        for sl, j in enumerate(js):
            dummy = j in seen
            seen.add(j)
            for kk in range(128):
                kr, kc = 2 * j + kk // 64, kk % 64
                for qq in range(128):
                    qr, qc = 2 * i + qq // 64, qq % 64
                    rs = min(max(qr - 4, 0), 56)
                    ws = min(max(qc - 8, 0), 48)
                    ok = (not dummy) and (rs <= kr < rs + 8) and (ws <= kc < ws + 16)
                    if ok:
                        idx_r[cs, kk, sl, qq] = kr - qr + 7
                        idx_c[cs, kk, sl, qq] = kc - qc + 15
                        mask[cs, kk, sl, qq] = 0.0
    return idx_r, idx_c, mask


_NA_CACHE = {}


def na_tables_cached():
    if 'v' not in _NA_CACHE:
        _NA_CACHE['v'] = na_tables_fast()
    return _NA_CACHE['v']


def na_tables_fast():
    rep = {0: 0, 1: 1, 2: 5, 3: 30, 4: 31}
    idx_r = np.zeros((5, 128, 5, 128), np.int64)
    idx_c = np.zeros((5, 128, 5, 128), np.int64)
    mask = np.full((5, 128, 5, 128), -1e30, np.float32)
    kk = np.arange(128)[:, None]
    qq = np.arange(128)[None, :]
    for cs, i in rep.items():
        js = na_slots(i)
        seen = set()
        for sl, j in enumerate(js):
            dummy = j in seen
            seen.add(j)
            if dummy:
                continue
            kr, kc = 2 * j + kk // 64, kk % 64
            qr, qc = 2 * i + qq // 64, qq % 64
            rs = np.clip(qr - 4, 0, 56)
            ws = np.clip(qc - 8, 0, 48)
            ok = (rs <= kr) & (kr < rs + 8) & (ws <= kc) & (kc < ws + 16)
            ir = np.where(ok, kr - qr + 7, 0)
            ic = np.where(ok, kc - qc + 15, 0)
            idx_r[cs, :, sl, :] = ir
            idx_c[cs, :, sl, :] = ic
            mask[cs, :, sl, :] = np.where(ok, 0.0, -1e30)
    return idx_r, idx_c, mask


def build(dbg=False, layers=DEPTH, stop_after=None, lvl=9, ntl=99):
    nc = bass.Bass("TRN2", target_bir_lowering=False)
    K = Ctx(nc)
    S = K.S
    xT = K.dram_in('xT', [D, TOK])
    cT = K.dram_in('cT', [128, 16])
    consts_d = K.dram_in('consts', [128, NCC])
    constb_d = K.dram_in('constb', [128, NCB], BF16)
    rope_d = K.dram_in('rope', [4, 128, TOK])
    namask_d = K.dram_in('namask', [5, 128, 640])
    W = []
    for l in range(layers):
        w = {}
        w['ada_w'] = K.dram_in('ada_w%d' % l, [D, 6 * D])
        w['vecs'] = K.dram_in('vecs%d' % l, [128, NVC])
        w['rows'] = K.dram_in('rows%d' % l, [1, 64 + 32 + 128])
        w['w_in'] = K.dram_in('w_in%d' % l, [D, 2528])
        w['w_uq'] = K.dram_in('w_uq%d' % l, [192, 384])
        w['w_ukv'] = K.dram_in('w_ukv%d' % l, [128, 640])
        w['bouts'] = K.dram_in('bouts%d' % l, [4, 256, D])
        w['gate_w'] = K.dram_in('gate_w%d' % l, [D, 4 * D])
        w['w_o'] = K.dram_in('w_o%d' % l, [D, D])
        w['router_w'] = K.dram_in('router_w%d' % l, [D, NE])
        w['w_gu'] = K.dram_in('w_gu%d' % l, [NE, D, 2 * DFF])
        w['w_dn'] = K.dram_in('w_dn%d' % l, [NE, DFF, D])
        w['b_dn'] = K.dram_in('b_dn%d' % l, [NE, D])
        w['nab'] = K.dram_in('nab%d' % l, [5, 4, 128, 640])
        W.append(w)
    outT = K.dram_out('outT', [D, SEQ])
    hT = K.dram('hT', [D, TOK], BF16)
    uT = K.dram('uT', [256, TOK], F32)
    QT = {'mla': K.dram('QTm', [4, 128, TOK], BF16), 'diff': K.dram('QTd', [2, 128, TOK], BF16),
          'na': K.dram('QTn', [2, 128, TOK], BF16)}
    KT = {'mla': K.dram('KTm', [4, 128, TOK], BF16), 'diff': K.dram('KTd', [2, 128, TOK], BF16),
          'na': K.dram('KTn', [2, 128, TOK], BF16)}
    VA = {'mla': K.dram('VAm', [NKT, 128, 260], BF16), 'diff': K.dram('VAd', [NKT, 128, 260], BF16),
          'na': K.dram('VAn', [NKT, 128, 260], BF16)}
    OT = {n: K.dram('OT' + n, [256, TOK], BF16) for n in ['conv', 'mla', 'diff', 'na']}
    res1 = K.dram('res1', [D, TOK], F32)
    res2 = K.dram('res2', [D, TOK], F32)
    h2T = K.dram('h2T', [D, TOK], BF16)
    gT = K.dram('gT', [NE, TOK], F32)
    dbg_out = {}
    if dbg:
        for n, shp in [('d_mod', [128, 96]), ('d_hT', [D, TOK]), ('d_uT', [256, TOK]), ('d_QTm', [4, 128, TOK]),
                       ('d_KTm', [4, 128, TOK]), ('d_QTd', [2, 128, TOK]), ('d_KTd', [2, 128, TOK]),
                       ('d_QTn', [2, 128, TOK]), ('d_KTn', [2, 128, TOK]), ('d_VAm', [NKT, 128, 260]),
                       ('d_OTconv', [256, TOK]), ('d_OTmla', [256, TOK]), ('d_OTdiff', [256, TOK]),
                       ('d_OTna', [256, TOK]), ('d_res1', [D, TOK]), ('d_h2T', [D, TOK]), ('d_gT', [NE, TOK]),
                       ('d_res2', [D, TOK])]:
            dbg_out[n] = K.dram_out(n, shp, F32 if n in ('d_mod', 'd_uT', 'd_res1', 'd_gT', 'd_res2') else BF16)

    def phase():
        st = ExitStack()
        K.stack = st
        return st

    def end_phase(st):
        S.barrier()
        st.close()

    for l in range(layers):
        w = W[l]
        last = (l == DEPTH - 1)
        lam_init = 0.8 - 0.6 * math.exp(-0.3 * l)
        res_in = xT if l == 0 else res2
        tiles = TILES
        lst = ExitStack()
        K.stack = lst
        vecs = K.sb('vecs%d' % l, [128, NVC], F32)
        cst = K.sb('cst%d' % l, [128, NCC], F32)
        cstb = K.sb('cstb%d' % l, [128, NCB], BF16)
        mod = K.sb('mod%d' % l, [128, 96], F32)
        A1 = K.sb('A1_%d' % l, [128, 16], F32)
        A2 = K.sb('A2_%d' % l, [128, 16], F32)
        lamt = K.sb('lamt%d' % l, [128, 4], F32)
        rowsb = K.sb('rowsb%d' % l, [128, 224], F32)
        vecs_eps2 = K.sb('vecs_eps2_%d' % l, [128, 1], F32)
        K.memset('pool', vecs_eps2[:, :], EPS)
        K.dma('sp', vecs[:, :], w['vecs'][:, :])
        K.dma('sp', cst[:, :], consts_d[:, :])
        K.dma('sp', cstb[:, :], constb_d[:, :])
        K.dma('sp', rowsb[:, :], V(w['rows'].apx[0:1, :].partition_broadcast(128), w['rows'][:, :].keys))

        def vec(name, col=0, rows=128, base=0):
            o, _ = VC[name]
            return vecs[base:base + rows, o + col:o + col + 1]

        def cc(name, r0=0, r1=128, c0=0, c1=128):
            o, _ = CC[name]
            return cst[r0:r1, o + c0:o + c1]

        def cb(name, r0=0, r1=128, c0=0, c1=128):
            o, _ = CB[name]
            return cstb[r0:r1, o + c0:o + c1]

        def modv(j, col):
            return mod[:, 2 * j + col:2 * j + col + 1]

        st = phase()
        cin = K.sb('cin', [128, 16], F32)
        sig = K.sb('sig', [128, 16], F32)
        scb = K.sb('scb', [128, 16], BF16)
        K.dma('sp', cin[:, :], cT[:, :])
        K.act(sig[:, :], cin[:, :], AF.Sigmoid)
        K.tt('dve', scb[:, :], cin[:, :], sig[:, :], ALU.mult)
        pmod = K.ps('pmod', [128, 96], F32)
        for blk in range(4):
            awb = K.sb('awb%d' % blk, [128, 8, 1536], BF16)
            K.dma('pool', awb[:, :, :], V(w['ada_w'].apx[:, blk * 1536:(blk + 1) * 1536].rearrange('(k p) n -> p k n', p=128),
                                         w['ada_w'][:, :].keys))
            for jj in range(12):
                j = blk * 12 + jj
                for k in range(8):
                    K.mm(pmod[:, 2 * j:2 * j + 2], awb[:, k, jj * 128:(jj + 1) * 128], scb[:, 2 * k:2 * k + 2],
                         start=(k == 0), stop=(k == 7), skip_group_check=True)
        o_ab, _ = VC['ada_b']
        K.tt('dve', mod[:, :], pmod[:, :], vecs[:, o_ab:o_ab + 96], ALU.add)
        for (A, gname, j0) in [(A1, 'n1g', 8), (A2, 'n2g', 32)]:
            for k in range(8):
                K.ts('dve', A[:, 2 * k:2 * k + 2], mod[:, 2 * (j0 + k):2 * (j0 + k) + 2], 1.0, vec(gname, k), ALU.add, ALU.mult)
        lt = K.sb('lt', [128, 64], F32)
        ls = K.sb('ls', [128, 4], F32)
        K.tt('dve', lt[:, 0:32], rowsb[:, 96:128], rowsb[:, 128:160], ALU.mult)
        K.tt('dve', lt[:, 32:64], rowsb[:, 160:192], rowsb[:, 192:224], ALU.mult)
        K.S.add('dve', (lambda o, i: (lambda e: e.tensor_reduce(o, i, AX.X, ALU.add)))(ls[:, 0:2].ap, lt[:, :].ap.rearrange('p (a b) -> p a b', a=2)),
                lt[:, :].keys, ls[:, :].keys)
        K.act(ls[:, 2:4], ls[:, 0:2], AF.Exp)
        K.tt('dve', lamt[:, 1:2], ls[:, 3:4], ls[:, 2:3], ALU.subtract)
        K.ts('dve', lamt[:, 0:1], lamt[:, 1:2], -lam_init, None, ALU.add)
        if dbg and l == 0:
            K.dma('sp', dbg_out['d_mod'][:, :], mod[:, :], is_out=True)
        end_phase(st)
        if stop_after == 'A':
            break

        st = phase()
        win = K.sb('win', [128, 8, 2528], BF16)
        K.dma('pool', win[:, :, :], V(w['w_in'].apx.rearrange('(k p) n -> p k n', p=128), w['w_in'][:, :].keys))
        wuq = K.sb('wuq', [128, 2, 384], BF16)
        K.dma('pool', wuq[:, 0, :], w['w_uq'][0:128, :])
        K.dma('pool', wuq[0:64, 1, :], w['w_uq'][128:192, :])
        wukv = K.sb('wukv', [128, 640], BF16)
        K.dma('pool', wukv[:, :], w['w_ukv'][:, :])
        NB = 2
        xt = [K.sb('xt%d' % i, [128, 8, 512], F32) for i in range(NB)]
        ht = [K.sb('ht%d' % i, [128, 8, 512], BF16) for i in range(NB)]
        rp = [K.sb('rp%d' % i, [128, 4, 512], F32) for i in range(NB)]
        rstd = K.sb('rstd', [128, 512], F32)
        tmp = [K.sb('tmp%d' % i, [128, 512], F32) for i in range(4)]
        tb = [K.sb('tb%d' % i, [128, 512], BF16) for i in range(4)]
        cqn = K.sb('cqn', [128, 2, 512], BF16)
        ckvn = K.sb('ckvn', [128, 512], BF16)
        ut = [K.sb('ut%d' % i, [128, 2, 512], F32) for i in range(NB)]
        qo = {n: [K.sb('qo%s%d' % (n, i), [128, c, 512], BF16) for i in range(NB)] for n, c in [('mla', 4), ('diff', 2), ('na', 2)]}
        ko = {n: [K.sb('ko%s%d' % (n, i), [128, c, 512], BF16) for i in range(NB)] for n, c in [('mla', 4), ('diff', 2), ('na', 2)]}
        va = {n: [K.sb('va%s%d' % (n, i), [128, 4, 65], BF16) for i in range(NB)] for n in ['mla', 'diff', 'na']}
        for n in va:
            for i in range(NB):
                K.memset('pool', va[n][i][:, :, :], 1.0)
        pb = [K.ps('pb%d' % i, [128, 512], F32) for i in range(8)]
        pbi = [0]

        def nextp():
            p = pb[pbi[0] % 8]
            pbi[0] += 1
            return p

        tmi = [0]

        def nt():
            t = tmp[tmi[0] % 4]
            tmi[0] += 1
            return t

        tbi = [0]

        def ntb():
            t = tb[tbi[0] % 4]
            tbi[0] += 1
            return t

        def rms_feat(src_list, bdname, nfeat, N):
            pst = nextp()
            for ii, (src, rows) in enumerate(src_list):
                s_ = nt()
                K.act(s_[0:rows, 0:N], src, AF.Square)
                K.mm(pst[:, 0:N], cc(bdname, 0, rows), s_[0:rows, 0:N], start=(ii == 0), stop=(ii == len(src_list) - 1))
            r_ = nt()
            K.act(r_[:, 0:N], pst[:, 0:N], AF.Sqrt, bias=vecs_eps[:, 0:1], scale=1.0 / nfeat)
            r2 = nt()
            K.recip(r2[:, 0:N], r_[:, 0:N])
            return r2

        vecs_eps = K.sb('vecs_eps', [128, 1], F32)
        K.memset('pool', vecs_eps[:, :], EPS)

        def rope_qk(src, rows, bdname, nfeat, gvec, rot, cosv, sinv, dst, N):
            r2 = rms_feat([(src, rows)], bdname, nfeat, N)
            if rot is None:
                K.stt('dve', dst, src, gvec, r2[0:rows, 0:N], ALU.mult, ALU.mult)
                return
            qn = ntb()
            K.stt('dve', qn[0:rows, 0:N], src, gvec, r2[0:rows, 0:N], ALU.mult, ALU.mult)
            pr = nextp()
            K.mm(pr[0:rows, 0:N], rot, qn[0:rows, 0:N])
            t1 = nt()
            K.tt('pool' if POOLP1 else 'dve', t1[0:rows, 0:N], qn[0:rows, 0:N], cosv, ALU.mult)
            t2 = nt()
            K.tt('dve', t2[0:rows, 0:N], pr[0:rows, 0:N], sinv, ALU.mult)
            K.tt('pool' if POOLP1 else 'dve', dst, t1[0:rows, 0:N], t2[0:rows, 0:N], ALU.add)

        cA = [K.sb('cA%d' % i, [128, 512], F32) for i in range(4)]
        cB = [K.sb('cB%d' % i, [128, 512], F32) for i in range(4)]
        cQ = [K.sb('cQ%d' % i, [128, 512], BF16) for i in range(4)]

        def chains(items, N):
            n = len(items)
            srcs = []
            for i, it in enumerate(items):
                srcs.append(it[0](pb[i]))
            for i, it in enumerate(items):
                rows = it[1]
                K.act(cA[i][0:rows, 0:N], srcs[i], AF.Square)
            for i, it in enumerate(items):
                rows = it[1]
                K.mm(pb[4 + i][:, 0:N], cc(it[2], 0, rows), cA[i][0:rows, 0:N])
            for i, it in enumerate(items):
                K.act(cA[i][:, 0:N], pb[4 + i][:, 0:N], AF.Sqrt, bias=vecs_eps[:, 0:1], scale=1.0 / it[3])
            for i, it in enumerate(items):
                K.recip(cA[i][:, 0:N], cA[i][:, 0:N])
            for i, it in enumerate(items):
                rows, gvec, rot, dst = it[1], it[4], it[5], it[8]
                if rot is None:
                    K.stt('dve', dst, srcs[i], gvec, cA[i][0:rows, 0:N], ALU.mult, ALU.mult)
                else:
                    K.stt('dve', cQ[i][0:rows, 0:N], srcs[i], gvec, cA[i][0:rows, 0:N], ALU.mult, ALU.mult)
            for i, it in enumerate(items):
                rows, rot = it[1], it[5]
                if rot is not None:
                    K.mm(pb[4 + i][0:rows, 0:N], rot, cQ[i][0:rows, 0:N])
            for i, it in enumerate(items):
                rows, rot, cosv = it[1], it[5], it[6]
                if rot is not None:
                    K.tt('dve', cA[i][0:rows, 0:N], cQ[i][0:rows, 0:N], cosv, ALU.mult)
            for i, it in enumerate(items):
                rows, rot, sinv = it[1], it[5], it[7]
                if rot is not None:
                    K.tt('dve', cB[i][0:rows, 0:N], pb[4 + i][0:rows, 0:N], sinv, ALU.mult)
            for i, it in enumerate(items):
                rows, rot, dst = it[1], it[5], it[8]
                if rot is not None:
                    K.tt('dve', dst, cA[i][0:rows, 0:N], cB[i][0:rows, 0:N], ALU.add)

        for ti, (c0, N) in enumerate(tiles[:ntl]):
            col = 1 if ti == 0 else 0
            b = ti % NB
            X, H, RP, U = xt[b], ht[b], rp[b], ut[b]
            K.dma('sp', X[:, :, 0:N], V(res_in.apx[:, c0:c0 + N].rearrange('(k p) n -> p k n', p=128), res_in[:, :].keys))
            K.dma('sp', RP[:, :, 0:N], V(rope_d.apx[:, :, c0:c0 + N].rearrange('f p n -> p f n'), rope_d[:, :, :].keys))
            pst = nextp()
            for k in range(8):
                sq_ = nt()
                K.act(sq_[:, 0:N], X[:, k, 0:N], AF.Square)
                K.mm(pst[:, 0:N], cc('ones'), sq_[:, 0:N], start=(k == 0), stop=(k == 7))
            r_ = nt()
            K.act(r_[:, 0:N], pst[:, 0:N], AF.Sqrt, bias=vecs_eps[:, 0:1], scale=1.0 / D)
            K.recip(rstd[:, 0:N], r_[:, 0:N])
            for k in range(8):
                t_ = nt()
                K.stt('dve' if k % 2 == 0 else 'pool', t_[:, 0:N], X[:, k, 0:N], A1[:, 2 * k + col:2 * k + col + 1], rstd[:, 0:N], ALU.mult, ALU.mult)
                K.act(H[:, k, 0:N], t_[:, 0:N], AF.Identity, bias=modv(k, col), scale=1.0)
            K.dma('sp', V(hT.apx[:, c0:c0 + N].rearrange('(k p) n -> p k n', p=128), [('hT', ti)]), H[:, :, 0:N])

            def proj(chunk, rows=128):
                p = nextp()
                for k in range(8):
                    K.mm(p[0:rows, 0:N], win[:, k, chunk * 128:chunk * 128 + rows], H[:, k, 0:N], start=(k == 0), stop=(k == 7))
                return p
            if lvl < 1:
                continue
            for c in range(2):
                pa = proj(c)
                pbb = proj(2 + c)
                sg = nt()
                K.act(sg[:, 0:N], pbb[:, 0:N], AF.Sigmoid)
                K.tt('dve', U[:, c, 0:N], pa[:, 0:N], sg[:, 0:N], ALU.mult)
            K.dma('sp', V(uT.apx[:, c0:c0 + N].rearrange('(k p) n -> p k n', p=128), [('uT', ti)]), U[:, :, 0:N])
            if lvl < 2:
                continue
            pq0 = proj(4)
            pq1 = proj(5, 64)
            r2 = rms_feat([(pq0[:, 0:N], 128), (pq1[0:64, 0:N], 64)], 'ones', 192, N)
            K.stt('dve', cqn[:, 0, 0:N], pq0[:, 0:N], vec('cq_g', 0), r2[:, 0:N], ALU.mult, ALU.mult)
            K.stt('dve', cqn[0:64, 1, 0:N], pq1[0:64, 0:N], vec('cq_g', 1, 64), r2[0:64, 0:N], ALU.mult, ALU.mult)
            pkv = proj(6)
            r2 = rms_feat([(pkv[:, 0:N], 128)], 'ones', 128, N)
            K.stt('dve', ckvn[:, 0:N], pkv[:, 0:N], vec('ckv_g', 0), r2[:, 0:N], ALU.mult, ALU.mult)
            QO, KO = qo['mla'][b], ko['mla'][b]
            def mk_q(h):
                def f(p):
                    K.mm(p[0:96, 0:N], wuq[:, 0, h * 96:(h + 1) * 96], cqn[:, 0, 0:N], start=True, stop=False)
                    K.mm(p[0:96, 0:N], wuq[0:64, 1, h * 96:(h + 1) * 96], cqn[0:64, 1, 0:N], start=False, stop=True)
                    return p[0:96, 0:N]
                return f

            def mk_k(h):
                def f(p):
                    K.mm(p[0:96, 0:N], wukv[:, h * 96:(h + 1) * 96], ckvn[:, 0:N], start=True, stop=False)
                    for k in range(8):
                        K.mm(p[0:96, 0:N], win[:, k, 1920:2016], H[:, k, 0:N], start=False, stop=(k == 7))
                    return p[0:96, 0:N]
                return f
            for hp in range(2):
                items = []
                for h in (2 * hp, 2 * hp + 1):
                    items.append((mk_q(h), 96, 'bd96', 96, vec('mqn_g', 0, 96), cb('RM', 0, 96, 0, 96), RP[0:96, 0, 0:N], RP[0:96, 1, 0:N], QO[0:96, h, 0:N]))
                    items.append((mk_k(h), 96, 'bd96', 96, vec('mkn_g', 0, 96), cb('RM', 0, 96, 0, 96), RP[0:96, 0, 0:N], RP[0:96, 1, 0:N], KO[0:96, h, 0:N]))
                chains(items, N)
            K.dma('sp', V(QT['mla'].apx[:, 0:96, c0:c0 + N].rearrange('h p n -> p h n'), [('QTm', ti)]), QO[0:96, :, 0:N])
            K.dma('sp', V(KT['mla'].apx[:, 0:96, c0:c0 + N].rearrange('h p n -> p h n'), [('KTm', ti)]), KO[0:96, :, 0:N])
            if lvl < 3:
                continue
            for (nm, ch0, bd, nf, gq, gk, rot, ci) in [('diff', 7, 'bd32', 32, 'dqn_g', 'dkn_g', cb('RD'), 2), ('na', 11, 'bd64', 64, 'nqn_g', 'nkn_g', None, None)]:
                if 'p1' not in NOBAR:
                    S.barrier()
                QO, KO = qo[nm][b], ko[nm][b]

                def mk_p(chunk):
                    def f(p):
                        for k in range(8):
                            K.mm(p[:, 0:N], win[:, k, chunk * 128:chunk * 128 + 128], H[:, k, 0:N], start=(k == 0), stop=(k == 7))
                        return p[:, 0:N]
                    return f
                items = []
                for c in range(2):
                    for (dst, cho, g) in [(QO, 0, gq), (KO, 2, gk)]:
                        items.append((mk_p(ch0 + cho + c), 128, bd, nf, vec(g, 0), rot,
                                      None if ci is None else RP[:, ci, 0:N], None if ci is None else RP[:, ci + 1, 0:N], dst[:, c, 0:N]))
                chains(items, N)
                sfx = 'd' if nm == 'diff' else 'n'
                K.dma('sp', V(QT[nm].apx[:, :, c0:c0 + N].rearrange('h p n -> p h n'), [('QT' + sfx, ti)]), QO[:, :, 0:N])
                K.dma('sp', V(KT[nm].apx[:, :, c0:c0 + N].rearrange('h p n -> p h n'), [('KT' + sfx, ti)]), KO[:, :, 0:N])
            if lvl < 4:
                continue
            nsub = N // 128
            for s_ in range(nsub):
                kt = c0 // 128 + s_
                VAm, VAd, VAn = va['mla'][kt % NB], va['diff'][kt % NB], va['na'][kt % NB]
                p = nextp()
                K.mm(p[:, 0:256], ckvn[:, s_ * 128:(s_ + 1) * 128], wukv[:, 384:640])
                for h4 in range(4):
                    K.copy('dve', VAm[:, h4, 0:64], p[:, h4 * 64:(h4 + 1) * 64])
                p = nextp()
                for k in range(8):
                    K.mm(p[:, 0:512], H[:, k, s_ * 128:(s_ + 1) * 128], win[:, k, 2016:2528], start=(k == 0), stop=(k == 7))
                for h4 in range(4):
                    K.copy('dve', VAd[:, h4, 0:64], p[:, h4 * 64:(h4 + 1) * 64])
                    K.copy('dve', VAn[:, h4, 0:64], p[:, 256 + h4 * 64:256 + (h4 + 1) * 64])
                for nm, t_ in [('mla', VAm), ('diff', VAd), ('na', VAn)]:
                    K.dma('sp', V(VA[nm].apx[kt, :, :].rearrange('p (h d) -> p h d', h=4), [('VA' + nm, kt)]), t_[:, :, :])
        if dbg and l == 0:
            S.barrier()
            for (dn, src) in [('d_hT', hT), ('d_uT', uT), ('d_QTm', QT['mla']), ('d_KTm', KT['mla']), ('d_QTd', QT['diff']),
                              ('d_KTd', KT['diff']), ('d_QTn', QT['na']), ('d_KTn', KT['na']), ('d_VAm', VA['mla'])]:
                K.dma('sp', V(dbg_out[dn].apx, [dn]), V(src.apx, [src.name + '_all']), is_out=True)
        end_phase(st)
        if stop_after == 'P1':
            break

        st = phase()
        accs = [K.sb('cacc%d' % c, [128, TOK], F32) for c in range(2)]
        o_cw, _ = VC['conv_w']
        Ucb = [K.sb('Ucb%d' % c, [128, 286], BF16) for c in range(2)]
        Ulb = [K.sb('Ulb%d' % c, [128, 4126], BF16) for c in range(2)]
        Dg = [K.sb('Dg%d' % c, [128, 31, 128], BF16) for c in range(2)]
        cvp = [K.ps('cvp%d' % i, [128, 512], F32) for i in range(4)]
        for c in range(2):
            K.memset('pool', Ucb[c][:, :], 0.0)
            K.memset('pool', Ulb[c][:, :], 0.0)
            K.dma('pool', Ucb[c][:, 15:271], V(uT.apx[c * 128:(c + 1) * 128, 0:256], ['uT_all']))
            K.dma('pool', Ulb[c][:, 15:4111], V(uT.apx[c * 128:(c + 1) * 128, 256:TOK], ['uT_all']))
            for wv in range(31):
                K.ts('dve', Dg[c][:, wv, :], cc('identf'), vecs[:, o_cw + c * 31 + wv:o_cw + c * 31 + wv + 1], None, ALU.mult)
        for ti, (c0, N) in enumerate(tiles):
            for c in range(2):
                src, off = (Ucb[c], 0) if ti == 0 else (Ulb[c], c0 - 256)
                pc = cvp[(ti * 2 + c) % 4]
                for wv in range(31):
                    K.mm(pc[:, 0:N], Dg[c][:, wv, :], src[:, off + wv:off + wv + N], start=(wv == 0), stop=(wv == 30))
                K.act(accs[c][:, c0:c0 + N], pc[:, 0:N], AF.Identity, bias=vec('conv_b', c), scale=1.0)
        ceps = K.sb('ceps', [128, 1], F32)
        K.memset('pool', ceps[:, :], EPS)
        ctmp = [K.sb('ctmp%d' % i, [128, 512], F32) for i in range(6)]
        zt = K.sb('zt', [128, 2, 512], BF16)
        cp1 = K.ps('cp1', [128, 512], F32)
        cp2 = K.ps('cp2', [128, 512], F32)
        for ti, (c0, N) in enumerate(tiles):
            for c in range(2):
                K.mm(cp1[:, 0:N], cc('ones'), accs[c][:, c0:c0 + N], start=(c == 0), stop=(c == 1))
            for c in range(2):
                K.act(ctmp[c][:, 0:N], accs[c][:, c0:c0 + N], AF.Square)
                K.mm(cp2[:, 0:N], cc('ones'), ctmp[c][:, 0:N], start=(c == 0), stop=(c == 1))
            mean, msq, var, sd, rs = ctmp[2], ctmp[3], ctmp[4], ctmp[5], ctmp[0]
            K.act(mean[:, 0:N], cp1[:, 0:N], AF.Copy, scale=1.0 / 256)
            K.tt('dve', msq[:, 0:N], mean[:, 0:N], mean[:, 0:N], ALU.mult)
            K.stt('dve', var[:, 0:N], cp2[:, 0:N], 1.0 / 256, msq[:, 0:N], ALU.mult, ALU.subtract)
            K.act(sd[:, 0:N], var[:, 0:N], AF.Sqrt, bias=ceps[:, 0:1], scale=1.0)
            K.recip(rs[:, 0:N], sd[:, 0:N])
            for c in range(2):
                t1 = ctmp[1]
                K.tt('dve', t1[:, 0:N], accs[c][:, c0:c0 + N], mean[:, 0:N], ALU.subtract)
                K.tt('dve', t1[:, 0:N], t1[:, 0:N], rs[:, 0:N], ALU.mult)
                K.act(zt[:, c, 0:N], t1[:, 0:N], AF.Silu, bias=vec('cln_b', c), scale=vec('cln_g', c))
            K.dma('sp', V(OT['conv'].apx[:, c0:c0 + N].rearrange('(k p) n -> p k n', p=128), [('OTconv', ti)]), zt[:, :, 0:N])
            if 'cv' not in NOBAR:
                S.barrier()
        end_phase(st)

        def attention(kind):
            st = phase()
            nshift = K.sb('nshift', [128, 1], F32)
            NPO = 4 if kind == 'na' else 2
            NPS = 7 - NPO
            NPT = 6
            psS = [K.ps('psS%d' % i, [128, 512], F32) for i in range(NPS)]
            psO = [K.ps('psO%d' % i, [128, 512], F32) for i in range(NPO)]
            psB = K.ps('psB', [128, 512], F32)
            Pt = [K.sb('Pt%d' % i, [128, 1024 if kind == 'na' else 512], BF16) for i in range(2 if kind == 'na' else NPT)]
            rcp = [K.sb('rcp%d' % i, [128, 512], F32) for i in range(2)]
            bcs = [K.sb('bcs%d' % i, [64, 512], F32) for i in range(2)]
            resb = [K.sb('resb%d' % i, [64, 512], BF16) for i in range(2)]
            VAs = K.sb('VAs', [128, NKT, 260], BF16)
            K.dma('sp', VAs[:, :, :], V(VA[kind].apx.rearrange('k p f -> p k f'), ['VA%s_all' % kind]))
            Qs = [K.sb('Qs%d' % i, [128, 4 if kind == 'mla' else 2, 512], BF16) for i in range(2)]
            if kind == 'mla':
                d = 96
                KTs = K.sb('KTs', [128, 4, TOK], BF16)
                K.dma('sp', KTs[:, :, :], V(KT['mla'].apx.rearrange('h p n -> p h n'), ['KTm_all']))
            elif kind == 'na':
                d = 64
                KTs = K.sb('KTs', [128, 4, TOK], BF16)
                K.memset('pool', KTs[:, :, :], 0.0)
                for h_ in range(4):
                    r0_ = 64 * (h_ % 2)
                    K.dma('sp', KTs[r0_:r0_ + 64, h_, :], V(KT['na'].apx[h_ // 2, r0_:r0_ + 64, :], ['KTn_all']))
                Bt = K.sb('Bt', [128, 20, 640], F32)
                Mt = K.sb('Mt', [128, 5, 640], F32)
                K.dma('sp', Bt[:, :, :], V(w['nab'].apx.rearrange('c h p n -> p (c h) n'), ['nab']))
                K.dma('sp', Mt[:, :, :], V(namask_d.apx.rearrange('c p n -> p c n'), ['namask']))
                for cs in range(5):
                    for h in range(4):
                        K.tt('dve', Bt[:, cs * 4 + h, :], Bt[:, cs * 4 + h, :], Mt[:, cs, :], ALU.add)
                sbs = [K.sb('sbs%d' % i, [128, 640], F32) for i in range(2)]
            else:
                d = 32
                KPs = K.sb('KPs', [128, 8, TOK], BF16)
                K.memset('pool', KPs[:, :, :], 0.0)
                for c in range(2):
                    for hh in range(2):
                        for m in range(2):
                            r0 = 64 * hh + 32 * m
                            K.dma('sp', KPs[r0:r0 + 32, (2 * c + hh) * 2 + m, :], V(KT['diff'].apx[c, r0:r0 + 32, :], ['KTd_all']))
                sub_g = K.sb('sub_g', [64, 1], F32)
                K.ts('dve', sub_g[:, :], vec('sub_g', 0, 64), 1.0 - lam_init, None, ALU.mult)
                dt0 = K.sb('dt0', [64, 512], F32)
                dt1 = K.sb('dt1', [64, 512], F32)
                dsq = K.sb('dsq', [64, 512], F32)
            scale = float(d) ** -0.5
            K.memset('pool', nshift[:, :], -math.sqrt(d))
            S.barrier()

            def qk_ops(h, m, Q):
                if kind == 'mla':
                    return (lambda kt: KTs[0:96, h, kt * 128:(kt + 1) * 128]), (lambda a, b_: Q[0:96, h, a:b_])
                c, hh = divmod(h, 2)
                r0 = 64 * hh
                if kind == 'na':
                    return (lambda kt: KTs[:, h, kt * 128:(kt + 1) * 128]), (lambda a, b_: Q[:, c, a:b_])
                return (lambda kt: KPs[:, h * 2 + m, kt * 128:(kt + 1) * 128]), (lambda a, b_: Q[:, c, a:b_])

            def dense_map(h, m, Q, N, kts, Ops):
                kf, qf = qk_ops(h, m, Q)
                LA = min(NPS - 2, len(kts) - 1)
                for a_ in range(LA + 1):
                    K.mm(psS[(sct[0] + a_) % NPS][:, 0:N], kf(kts[a_]), qf(0, N))
                for ii, kt in enumerate(kts):
                    ps = psS[(sct[0] + ii) % NPS]
                    if ii + LA + 1 < len(kts):
                        K.mm(psS[(sct[0] + ii + LA + 1) % NPS][:, 0:N], kf(kts[ii + LA + 1]), qf(0, N))
                    P = Pt[(pct[0] + ii) % len(Pt)]
                    K.act(P[:, 0:N], ps[:, 0:N], AF.Exp, bias=nshift[:, 0:1], scale=scale)
                    K.mm(Ops[0:65, 0:N], VAs[:, kt, h * 65:(h + 1) * 65], P[:, 0:N], start=(ii == 0), stop=(ii == len(kts) - 1))
                    if ii == min(2, len(kts) - 1) and apend:
                        apend.pop(0)()
                sct[0] += len(kts)
                pct[0] += len(kts)

            bci = [0]
            apend = []
            sct = [0]
            pct = [0]

            def bcast_recip(Ops, N, mult=None):
                i = bci[0] % 2
                bci[0] += 1
                r = rcp[i]
                K.recip(r[64:65, 0:N], Ops[64:65, 0:N])
                if mult is not None:
                    K.ts('dve', r[64:65, 0:N], r[64:65, 0:N], mult, None, ALU.mult)
                K.mm(psB[0:64, 0:N], cc('ones', 64, 65, 0, 64), r[64:65, 0:N])
                K.copy('act', bcs[i][:, 0:N], psB[0:64, 0:N])
                return bcs[i]

            ri = [0]

            def store_head(h, c0, N, resv, bi):
                K.dma('sp', V(OT[kind].apx[h * 64:(h + 1) * 64, c0:c0 + N], [('OT' + kind, bi, h)]), resv)

            blocks = ([(0, 256, [0, 1])] if not last else []) + [(256 + 512 * i, 512, list(range(NKT))) for i in range(8)]
            for bi, (c0, N, kts) in enumerate(blocks):
                Q = Qs[bi % 2]
                K.dma('sp', Q[:, :, 0:N], V(QT[kind].apx[:, :, c0:c0 + N].rearrange('h p n -> p h n'), ['QT%s_all' % kind]))
                if kind == 'na' and N == 512:
                    while apend:
                        apend.pop(0)()
                    for sb_ in range(4):
                        i = (c0 - 256) // 128 + sb_
                        js = na_slots(i)
                        cs = na_case(i)
                        qa, qb = sb_ * 128, (sb_ + 1) * 128
                        for h in range(4):
                            kf, qf = qk_ops(h, 0, Q)
                            P = Pt[h % 2]
                            SB = sbs[h % 2]
                            for ii, kt in enumerate([0, 1]):
                                K.mm(psS[0][:, ii * 128:(ii + 1) * 128], kf(kt), qf(qa, qb))
                            for ii, j in enumerate(js):
                                pp = psS[1] if ii < 4 else psS[2]
                                K.mm(pp[:, (ii % 4) * 128:(ii % 4 + 1) * 128], kf(2 + j), qf(qa, qb))
                            K.stt('dve', SB[:, 0:512], psS[1][:, 0:512], scale, Bt[:, cs * 4 + h, 0:512], ALU.mult, ALU.add)
                            K.stt('dve', SB[:, 512:640], psS[2][:, 0:128], scale, Bt[:, cs * 4 + h, 512:640], ALU.mult, ALU.add)
                            K.act(P[:, 0:256], psS[0][:, 0:256], AF.Exp, bias=nshift[:, 0:1], scale=scale)
                            K.act(P[:, 256:896], SB[:, 0:640], AF.Exp, bias=nshift[:, 0:1], scale=1.0)
                            ktl = [0, 1] + [2 + j for j in js]
                            for ii, kt in enumerate(ktl):
                                K.mm(psO[h][0:65, qa:qb], VAs[:, kt, h * 65:(h + 1) * 65], P[:, ii * 128:(ii + 1) * 128],
                                     start=(ii == 0), stop=(ii == 6), skip_group_check=True)
                    for h in range(4):
                        bc = bcast_recip(psO[h], N)
                        res = resb[h % 2]
                        K.tt('dve', res[:, 0:N], psO[h][0:64, 0:N], bc[:, 0:N], ALU.mult)
                        store_head(h, c0, N, res[:, 0:N], bi)
                    continue
                for h in range(4):
                    if kind == 'diff':
                        O0, O1 = psO[0], psO[1]
                        dense_map(h, 0, Q, N, kts, O0)
                        dense_map(h, 1, Q, N, kts, O1)

                        def fin(h=h, O0=O0, O1=O1, N=N, c0=c0, bi=bi):
                            res = resb[h % 2]
                            bc0 = bcast_recip(O0, N)
                            bc1 = bcast_recip(O1, N, mult=lamt[64:65, 0:1])
                            K.tt('dve', dt0[:, 0:N], O0[0:64, 0:N], bc0[:, 0:N], ALU.mult)
                            K.tt('dve', dt1[:, 0:N], O1[0:64, 0:N], bc1[:, 0:N], ALU.mult)
                            K.tt('dve', dt0[:, 0:N], dt0[:, 0:N], dt1[:, 0:N], ALU.add)
                            K.act(dsq[:, 0:N], dt0[:, 0:N], AF.Square)
                            K.mm(psB[0:64, 0:N], cc('ones', 0, 64, 0, 64), dsq[:, 0:N])
                            K.act(dt1[:, 0:N], psB[0:64, 0:N], AF.Sqrt, bias=vecs_eps2[0:64, 0:1], scale=1.0 / 64)
                            K.recip(dt1[:, 0:N], dt1[:, 0:N])
                            K.stt('dve', res[:, 0:N], dt0[:, 0:N], sub_g[:, 0:1], dt1[:, 0:N], ALU.mult, ALU.mult)
                            store_head(h, c0, N, res[:, 0:N], bi)
                    else:
                        Ops = psO[h % 2]
                        dense_map(h, 0, Q, N, kts, Ops)

                        def fin(h=h, Ops=Ops, N=N, c0=c0, bi=bi):
                            res = resb[h % 2]
                            bc = bcast_recip(Ops, N)
                            K.tt('dve', res[:, 0:N], Ops[0:64, 0:N], bc[:, 0:N], ALU.mult)
                            store_head(h, c0, N, res[:, 0:N], bi)
                    if kind == 'diff':
                        fin()
                    else:
                        apend.append(fin)
            while apend:
                apend.pop(0)()
            end_phase(st)

        for kind in ['mla', 'diff', 'na']:
            if stop_after == 'C':
                break
            attention(kind)
        if dbg and l == 0:
            S.barrier()
            for (dn, src) in [('d_OTconv', OT['conv']), ('d_OTmla', OT['mla']), ('d_OTdiff', OT['diff']), ('d_OTna', OT['na'])]:
                K.dma('sp', V(dbg_out[dn].apx, [dn]), V(src.apx, [src.name + '_all']), is_out=True)
            S.barrier()
        if stop_after in ('C', 'ATT'):
            break

        st = phase()
        bo = K.sb('bo', [128, 8, 1024], BF16)
        K.dma('pool', bo[:, :, :], V(w['bouts'].apx.rearrange('b (k p) n -> p (b k) n', p=128), ['bouts']))
        gwm = [K.sb('gwm%d' % m, [128, 8, 512], BF16) for m in range(8)]
        for m in range(8):
            K.dma('pool', gwm[m][:, :, :], V(w['gate_w'].apx[:, m * 512:(m + 1) * 512].rearrange('(k p) n -> p k n', p=128), ['gate_w']))
        wo = K.sb('wo', [128, 8, 1024], BF16)
        K.dma('pool', wo[:, :, :], V(w['w_o'].apx.rearrange('(k p) n -> p k n', p=128), ['w_o']))
        rw = K.sb('rw', [128, 8, 32], F32)
        K.dma('sp', rw[:, :, :], V(w['router_w'].apx.rearrange('(k p) n -> p k n', p=128), ['router_w']))
        Hm = K.sb('Hm', [128, 8, 512], BF16)
        B4 = K.sb('B4', [128, 8, 512], BF16)
        Xm = K.sb('Xm', [128, 8, 512], F32)
        X1 = K.sb('X1', [128, 8, 512], F32)
        ym = K.sb('ym', [128, 8, 512], BF16)
        H2f = K.sb('H2f', [128, 8, 512], F32)
        H2b = K.sb('H2b', [128, 8, 512], BF16)
        mt = [K.sb('mt%d' % i, [128, 512], F32) for i in range(6)]
        macc = K.sb('macc', [128, 512], F32)
        rstd2_t = K.sb('rstd2_t', [128, 512], F32)
        mpend = []
        meps = K.sb('meps', [128, 1], F32)
        K.memset('pool', meps[:, :], EPS)
        gTt = K.sb('gTt', [32, 512], F32)
        rlg = [K.sb('rlg%d' % i, [128, 32], F32) for i in range(4)]
        rmsk = [K.sb('rmsk%d' % i, [128, 32], F32) for i in range(4)]
        rex = [K.sb('rex%d' % i, [128, 32], F32) for i in range(4)]
        rgt = [K.sb('rgt%d' % i, [128, 32], F32) for i in range(4)]
        rr8 = [K.sb('rr8%d' % i, [128, 8], F32) for i in range(4)]
        rrs = [K.sb('rrs%d' % i, [128, 4], F32) for i in range(4)]
        mp = [K.ps('mp%d' % i, [128, 512], F32) for i in range(8)]
        mpi = [0]

        def mnext():
            p = mp[mpi[0] % 8]
            mpi[0] += 1
            return p
        mti = [0]

        def mnt():
            t = mt[mti[0] % 6]
            mti[0] += 1
            return t
        S.barrier(skip_pool_dma=True)
        for ti, (c0, N) in enumerate(tiles):
            if last and ti == 0:
                continue
            col = 1 if ti == 0 else 0
            K.dma('sp', Hm[:, :, 0:N], V(hT.apx[:, c0:c0 + N].rearrange('(k p) n -> p k n', p=128), ['hT_all']))
            for bi_, nm in enumerate(['conv', 'mla', 'diff', 'na']):
                K.dma('sp', B4[:, bi_ * 2:bi_ * 2 + 2, 0:N], V(OT[nm].apx[:, c0:c0 + N].rearrange('(k p) n -> p k n', p=128), ['OT_all' + nm]))
            K.dma('sp', Xm[:, :, 0:N], V(res_in.apx[:, c0:c0 + N].rearrange('(k p) n -> p k n', p=128), res_in[:, :].keys))
            for m in range(8):
                if m == 1 and mpend:
                    mpend.pop(0)()
                tms = []
                for b_ in range(4):
                    pg = mnext()
                    for k in range(8):
                        K.mm(pg[:, 0:N], gwm[m][:, k, b_ * 128:(b_ + 1) * 128], Hm[:, k, 0:N], start=(k == 0), stop=(k == 7))
                    sg = mnt()
                    K.act(sg[:, 0:N], pg[:, 0:N], AF.Sigmoid, bias=vec('gate_b', b_ * 8 + m), scale=1.0)
                    py = mnext()
                    for k in range(2):
                        K.mm(py[:, 0:N], bo[:, b_ * 2 + k, m * 128:(m + 1) * 128], B4[:, b_ * 2 + k, 0:N], start=(k == 0), stop=(k == 1))
                    K.tt('dve', sg[:, 0:N], sg[:, 0:N], py[:, 0:N], ALU.mult)
                    tms.append(sg)
                K.tt('dve', tms[0][:, 0:N], tms[0][:, 0:N], tms[1][:, 0:N], ALU.add)
                K.tt('dve', tms[2][:, 0:N], tms[2][:, 0:N], tms[3][:, 0:N], ALU.add)
                K.tt('dve', ym[:, m, 0:N], tms[0][:, 0:N], tms[2][:, 0:N], ALU.add)
            if 'mg' not in NOBAR:
                S.barrier()
            for m2 in range(8):
                po = mnext()
                for m in range(8):
                    K.mm(po[:, 0:N], wo[:, m, m2 * 128:(m2 + 1) * 128], ym[:, m, 0:N], start=(m == 0), stop=(m == 7))
                K.stt('dve', X1[:, m2, 0:N], po[:, 0:N], modv(16 + m2, col), Xm[:, m2, 0:N], ALU.mult, ALU.add)
            K.dma('sp', V(res1.apx[:, c0:c0 + N].rearrange('(k p) n -> p k n', p=128), [('res1', ti)]), X1[:, :, 0:N])
            def post(ti=ti, c0=c0, N=N, col=col):
                pst = mnext()
                for k in range(8):
                    sq_ = mnt()
                    K.act(sq_[:, 0:N], X1[:, k, 0:N], AF.Square)
                    K.mm(pst[:, 0:N], cc('ones'), sq_[:, 0:N], start=(k == 0), stop=(k == 7))
                r_ = mnt()
                K.act(r_[:, 0:N], pst[:, 0:N], AF.Sqrt, bias=meps[:, 0:1], scale=1.0 / D)
                rstd2 = rstd2_t
                K.recip(rstd2[:, 0:N], r_[:, 0:N])
                for k in range(8):
                    t_ = mnt()
                    K.stt('dve', t_[:, 0:N], X1[:, k, 0:N], A2[:, 2 * k + col:2 * k + col + 1], rstd2[:, 0:N], ALU.mult, ALU.mult)
                    K.act(H2f[:, k, 0:N], t_[:, 0:N], AF.Identity, bias=modv(24 + k, col), scale=1.0)
                    K.copy('dve', H2b[:, k, 0:N], H2f[:, k, 0:N])
                K.dma('sp', V(h2T.apx[:, c0:c0 + N].rearrange('(k p) n -> p k n', p=128), [('h2T', ti)]), H2b[:, :, 0:N])
                if 'mg' not in NOBAR:
                    S.barrier()
                nsb = N // 128
                prs = []
                for sb_ in range(nsb):
                    pr = mnext()
                    for k in range(8):
                        K.mm(pr[:, 0:32], H2f[:, k, sb_ * 128:(sb_ + 1) * 128], rw[:, k, :], start=(k == 0), stop=(k == 7))
                    prs.append(pr)
                for sb_ in range(nsb):
                    K.tt('dve', rlg[sb_][:, :], prs[sb_][:, 0:32], rowsb[:, 64:96], ALU.add)
                for sb_ in range(nsb):
                    K.vmax8(rr8[sb_][:, :], rlg[sb_][:, :])
                for sb_ in range(nsb):
                    K.ts('dve', rmsk[sb_][:, :], rlg[sb_][:, :], rr8[sb_][:, 3:4], None, ALU.is_ge)
                    K.ts('dve', rrs[sb_][:, 0:1], rr8[sb_][:, 0:1], -1.0, None, ALU.mult)
                for sb_ in range(nsb):
                    K.act(rex[sb_][:, :], rlg[sb_][:, :], AF.Exp, bias=rrs[sb_][:, 0:1], scale=1.0)
                for sb_ in range(nsb):
                    K.tt('dve', rex[sb_][:, :], rex[sb_][:, :], rmsk[sb_][:, :], ALU.mult)
                for sb_ in range(nsb):
                    K.S.add('dve', (lambda o, i: (lambda e: e.tensor_reduce(o, i, AX.X, ALU.add)))(rrs[sb_][:, 1:2].ap, rex[sb_][:, :].ap), rex[sb_][:, :].keys, rrs[sb_][:, :].keys)
                for sb_ in range(nsb):
                    K.recip(rrs[sb_][:, 2:3], rrs[sb_][:, 1:2])
                for sb_ in range(nsb):
                    K.ts('dve', rgt[sb_][:, :], rex[sb_][:, :], rrs[sb_][:, 2:3], None, ALU.mult)
                pts = []
                for sb_ in range(nsb):
                    pt_ = mnext()
                    K.tr(pt_[0:32, 0:128], rgt[sb_][:, :], cc('identf'))
                    pts.append(pt_)
                for sb_ in range(nsb):
                    K.copy('dve', gTt[:, sb_ * 128:(sb_ + 1) * 128], pts[sb_][0:32, 0:128])
                K.dma('sp', V(gT.apx[:, c0:c0 + N], [('gT', ti)]), gTt[:, 0:N])

            mpend.append(post)
            if 'mg' not in NOBAR:
                S.barrier()
        while mpend:
            mpend.pop(0)()
        S.barrier()
        if dbg and l == 0:
            for (dn, src) in [('d_res1', res1), ('d_h2T', h2T), ('d_gT', gT)]:
                K.dma('sp', V(dbg_out[dn].apx, [dn]), V(src.apx, [src.name + '_all']), is_out=True)
        end_phase(st)
        if stop_after == 'M':
            break

        st = phase()
        bd = K.sb('bd', [32, 1024], F32)
        K.dma('sp', bd[:, :], w['b_dn'][:, :])
        o_bg, _ = VC['bg']
        bup = K.sb('bup', [128, 512], F32)
        K.ts('dve', bup[:, :], vecs[:, o_bg:o_bg + 512], 1.0, None, ALU.add)
        EB = 1088
        H2 = K.sb('H2', [128, 8, EB], BF16)
        gts = K.sb('gts', [32, EB], F32)
        acc = K.sb('eacc', [128, 8, EB], F32)
        Wg = [K.sb('Wg%d' % i, [128, 8, 2048], BF16) for i in range(2)]
        Wd = [K.sb('Wd%d' % i, [128, 8, 1024], BF16) for i in range(2)]
        G = [K.sb('G%d' % i, [128, EB], F32) for i in range(2)]
        At = [K.sb('At%d' % i, [128, 8, 512], BF16) for i in range(2)]
        et = [K.sb('et%d' % i, [128, 512], F32) for i in range(6)]
        ep = [K.ps('ep%d' % i, [128, 512], F32) for i in range(8)]
        epi = [0]

        def enext():
            p = ep[epi[0] % 8]
            epi[0] += 1
            return p
        eti = [0]

        def ent():
            t = et[eti[0] % 6]
            eti[0] += 1
            return t
        S.barrier()
        eblocks = ([(256 + 1024 * i, 1024) for i in range(4)] if last else [(1088 * i, 1088) for i in range(4)])
        ai = 0
        for (c0, Nb) in eblocks:
            btiles = ([(0, 512), (512, 512)] if Nb == 1024 else ([(0, 256), (256, 416), (672, 416)] if c0 == 0 else [(0, 364), (364, 362), (726, 362)]))
            K.dma('sp', H2[:, :, 0:Nb], V(h2T.apx[:, c0:c0 + Nb].rearrange('(k p) n -> p k n', p=128), ['h2T_all']))
            K.dma('sp', gts[:, 0:Nb], V(gT.apx[:, c0:c0 + Nb], ['gT_all']))
            for m in range(8):
                for (t0, N) in btiles:
                    p = enext()
                    K.mm(p[:, 0:N], bd[0:32, m * 128:(m + 1) * 128], gts[0:32, t0:t0 + N])
                    K.copy('dve', acc[:, m, t0:t0 + N], p[:, 0:N])
            def load_w(e):
                K.dma('pool', Wg[e % 2][:, :, :], V(w['w_gu'].apx[e].rearrange('(k p) n -> p k n', p=128), ['w_gu']))
                K.dma('pool', Wd[e % 2][:, :, :], V(w['w_dn'].apx[e].rearrange('(k p) n -> p k n', p=128), ['w_dn']))
            load_w(0)
            pend = []
            for e in range(NE):
                WG, WD, GE = Wg[e % 2], Wd[e % 2], G[e % 2]
                K.dma('sp', GE[:, 0:Nb], V(gT.apx[e:e + 1, c0:c0 + Nb].partition_broadcast(128), ['gT_all']))
                K.act(GE[:, 0:Nb], GE[:, 0:Nb], AF.Copy, scale=1.0 / 1.702)
                for (t0, N) in btiles:
                    A = At[ai % 2]
                    ai += 1
                    for j in range(8):
                        pg = enext()
                        for k in range(8):
                            K.mm(pg[:, 0:N], WG[:, k, j * 128:(j + 1) * 128], H2[:, k, t0:t0 + N], start=(k == 0), stop=(k == 7))
                        pu = enext()
                        for k in range(8):
                            K.mm(pu[:, 0:N], WG[:, k, 1024 + j * 128:1024 + (j + 1) * 128], H2[:, k, t0:t0 + N], start=(k == 0), stop=(k == 7))
                        tg, sl, tu = ent(), ent(), ent()
                        tu2, p1 = tu, sl
                        K.ts('dve', tg[:, 0:N], pg[:, 0:N], vecs[:, o_bg + e * 16 + j:o_bg + e * 16 + j + 1], 7.0, ALU.add, ALU.min)
                        K.act(sl[:, 0:N], tg[:, 0:N], AF.Silu, scale=1.702)
                        K.act(tu[:, 0:N], pu[:, 0:N], AF.Identity, bias=bup[:, e * 16 + 8 + j:e * 16 + 8 + j + 1], scale=1.0)
                        K.ts('dve', tu2[:, 0:N], tu[:, 0:N], 8.0, -6.0, ALU.min, ALU.max)
                        K.tt('dve', p1[:, 0:N], sl[:, 0:N], tu2[:, 0:N], ALU.mult)
                        K.tt('dve', A[:, j, 0:N], p1[:, 0:N], GE[:, t0:t0 + N], ALU.mult)
                        if j == 1:
                            if pend:
                                pend.pop(0)()
                            if t0 == 0 and e + 1 < NE:
                                load_w(e + 1)
                    def down(A=A, WD=WD, t0=t0, N=N):
                        for m in range(8):
                            py = enext()
                            for j in range(8):
                                K.mm(py[:, 0:N], WD[:, j, m * 128:(m + 1) * 128], A[:, j, 0:N], start=(j == 0), stop=(j == 7))
                            K.tt('dve', acc[:, m, t0:t0 + N], acc[:, m, t0:t0 + N], py[:, 0:N], ALU.add)
                    pend.append(down)
                if e % 4 == 3 or e == NE - 1:
                    while pend:
                        pend.pop(0)()
                if e % 4 == 3:
                    S.barrier(skip_pool_dma=True)
            for m in range(8):
                for (t0, N) in btiles:
                    col = 1 if (c0 + t0) < 256 else 0
                    xo = ent()
                    K.dma('sp', xo[:, 0:N], V(res1.apx[m * 128:(m + 1) * 128, c0 + t0:c0 + t0 + N], ['res1_all']))
                    K.stt('dve', xo[:, 0:N], acc[:, m, t0:t0 + N], modv(40 + m, col), xo[:, 0:N], ALU.mult, ALU.add)
                    if last:
                        K.dma('sp', V(outT.apx[m * 128:(m + 1) * 128, c0 + t0 - 256:c0 + t0 - 256 + N], [('outT', m, c0, t0)]), xo[:, 0:N], is_out=True)
                    else:
                        K.dma('sp', V(res2.apx[m * 128:(m + 1) * 128, c0 + t0:c0 + t0 + N], [('res2', m, c0, t0)]), xo[:, 0:N])
            S.barrier()
        if dbg and l == 0:
            K.dma('sp', V(dbg_out['d_res2'].apx, ['d_res2']), V(res2.apx, ['res2_all']), is_out=True)
        end_phase(st)
        S.barrier()
        lst.close()
    K.emit()
    return nc


OFF_B, OFF_C, OFF_D = 512, 864, 1632


def _pk(v, n):
    return np.ascontiguousarray(np.asarray(v, np.float32).reshape(n, 128).T)


def prep_shared(inp, layers=DEPTH):
    sh = {}
    c, cb = make_consts()
    sh['consts'] = c
    sh['constb'] = cb
    sh['rope'] = rope_tables()
    idx_r, idx_c, mask = na_tables_cached()
    sh['namask'] = np.ascontiguousarray(mask.reshape(5, 128, 640))
    for l in range(layers):
        sh['ada_w%d' % l] = np.ascontiguousarray(inp['ada_w'][l])
        vec = np.zeros((128, NVC), np.float32)

        def put(name, arr):
            o, wd = VC[name]
            arr = np.asarray(arr, np.float32)
            vec[:arr.shape[0], o:o + arr.shape[1]] = arr
        ab = _pk(inp['ada_b'][l], 48)
        put('ada_b', np.repeat(ab, 2, axis=1))
        put('n1g', _pk(inp['norm1_g'][l], 8))
        put('n2g', _pk(inp['norm2_g'][l], 8))
        cw = inp['conv_w'][l]
        put('conv_w', np.concatenate([cw[:, 0:128].T, cw[:, 128:256].T], axis=1))
        put('conv_b', _pk(inp['conv_b'][l], 2))
        put('cln_g', _pk(inp['conv_ln_g'][l], 2))
        put('cln_b', _pk(inp['conv_ln_b'][l], 2))
        cq = np.zeros((128, 2), np.float32)
        cq[:, 0] = inp['mla_cq_g'][l][0:128]
        cq[0:64, 1] = inp['mla_cq_g'][l][128:192]
        put('cq_g', cq)
        put('ckv_g', inp['mla_ckv_g'][l][:, None])
        put('mqn_g', inp['mla_qn_g'][l][:, None])
        put('mkn_g', inp['mla_kn_g'][l][:, None])
        put('dqn_g', np.tile(inp['diff_qn_g'][l], 4)[:, None])
        put('dkn_g', np.tile(inp['diff_kn_g'][l], 4)[:, None])
        put('nqn_g', np.tile(inp['na_qn_g'][l], 2)[:, None])
        put('nkn_g', np.tile(inp['na_kn_g'][l], 2)[:, None])
        put('gate_b', _pk(inp['gate_b'][l], 32))
        put('sub_g', inp['diff_subln_g'][l][:, None])
        bgu = inp['exp_b_gu'][l]
        put('bg', np.ascontiguousarray(bgu.reshape(32, 16, 128).transpose(2, 0, 1).reshape(128, 512)))
        sh['vecs%d' % l] = vec
        sh['rows%d' % l] = np.concatenate([inp['diff_subln_g'][l], inp['router_b'][l], inp['diff_lam'][l].reshape(-1)])[None, :].astype(np.float32)
        wi = inp['w_in'][l]
        z64 = np.zeros((D, 64), np.float32)
        cols = [wi[:, 0:512], wi[:, 512:640], wi[:, 640:704], z64, wi[:, 704:832]]
        for off in (0, 64):
            for base in (OFF_C, OFF_D):
                for hp in range(2):
                    cols.append(np.concatenate([wi[:, base + h * 192 + off: base + h * 192 + off + 64] for h in (2 * hp, 2 * hp + 1)], axis=1))
        c5 = cols[5:]
        cols = cols[:5] + [c5[0], c5[1], c5[4], c5[5], c5[2], c5[3], c5[6], c5[7]]
        cols += [z64, wi[:, 832:864]]
        for base in (OFF_C, OFF_D):
            cols.append(np.concatenate([wi[:, base + h * 192 + 128: base + h * 192 + 192] for h in range(4)], axis=1))
        wr = np.ascontiguousarray(np.concatenate(cols, axis=1))
        assert wr.shape == (D, 2528), wr.shape
        sh['w_in%d' % l] = wr
        sh['w_uq%d' % l] = np.ascontiguousarray(inp['mla_w_uq'][l])
        wk = inp['mla_w_ukv'][l]
        z32 = np.zeros((128, 32), np.float32)
        kc = []
        for h in range(4):
            kc += [wk[:, h * 128:h * 128 + 64], z32]
        vc = [wk[:, h * 128 + 64:h * 128 + 128] for h in range(4)]
        sh['w_ukv%d' % l] = np.ascontiguousarray(np.concatenate(kc + vc, axis=1))
        sh['bouts%d' % l] = np.ascontiguousarray(np.stack([inp['conv_out'][l], inp['mla_out'][l], inp['diff_out'][l], inp['na_out'][l]], 0))
        sh['gate_w%d' % l] = np.ascontiguousarray(inp['gate_w'][l].reshape(D, 4, 8, 128).transpose(0, 2, 1, 3).reshape(D, 4 * D))
        sh['w_o%d' % l] = np.ascontiguousarray(inp['w_o'][l])
        sh['router_w%d' % l] = np.ascontiguousarray(inp['router_w'][l])
        sh['w_gu%d' % l] = np.ascontiguousarray(inp['exp_w_gu'][l])
        sh['w_dn%d' % l] = np.ascontiguousarray(inp['exp_w_down'][l])
        sh['b_dn%d' % l] = np.ascontiguousarray(inp['exp_b_down'][l])
        rpb = inp['na_rpb'][l]
        g = rpb[:, idx_r, idx_c]
        sh['nab%d' % l] = np.ascontiguousarray(g.transpose(1, 0, 2, 3, 4).reshape(5, 4, 128, 640)).astype(np.float32)
    return sh


def prep_core(inp, b):
    m = {}
    m['xT'] = np.ascontiguousarray(np.concatenate([inp['ctx'][b].T, inp['x'][b].T], axis=1)).astype(np.float32)
    cT = np.zeros((128, 16), np.float32)
    cT[:, 0::2] = _pk(inp['c'][b], 8)
    cT[:, 1::2] = _pk(inp['c_ctx'], 8)
    m['cT'] = cT
    return m


_NC_CACHE = {}


def kernel(**inputs):
    inp = {k: np.asarray(v) for k, v in inputs.items()}
    if 'nc' not in _NC_CACHE:
        _NC_CACHE['nc'] = build()
    nc = _NC_CACHE['nc']
    sh = prep_shared(inp)
    in_maps = []
    for b in range(8):
        m = dict(sh)
        m.update(prep_core(inp, b))
        in_maps.append(m)
    res = run_bass_kernel_spmd(nc, in_maps, core_ids=list(range(8)))
    out = np.stack([np.ascontiguousarray(res.results[b]['outT'].T) for b in range(8)], 0)
    return out.astype(np.float32)
```

```python
import math
from contextlib import ExitStack
import numpy as np
import ml_dtypes
import concourse.bass as bass
import concourse.mybir as mybir
from concourse.bass_utils import run_bass_kernel_spmd

F32 = mybir.dt.float32
BF16 = mybir.dt.bfloat16
AF = mybir.ActivationFunctionType
ALU = mybir.AluOpType
AX = mybir.AxisListType

D = 1024
SEQ = 4096
CTX = 256
TOK = SEQ + CTX
NE = 32
DFF = 1024
EPS = 1e-6
DEPTH = 2
NKT = TOK // 128

ENGS = ['pe', 'act', 'dve', 'pool', 'sp']
import os as _os
NOBAR = _os.environ.get('NOBAR', 'p1,cv,na,at,mg').split(',')
POOLP1 = _os.environ.get('POOLP1', '0') == '1'
NDSEM = 8


class V:
    __slots__ = ('ap', 'keys')

    def __init__(self, ap, keys):
        self.ap = ap
        self.keys = keys


class Buf:
    def __init__(self, name, ap, nsub=0):
        self.name = name
        self.apx = ap
        self.nsub = nsub

    def __getitem__(self, idx):
        keys = [self.name] if self.nsub == 0 else [(self.name, i) for i in range(self.nsub)]
        return V(self.apx[idx], keys)

    def s(self, i, idx=None):
        ap = self.apx if idx is None else self.apx[idx]
        return V(ap, [(self.name, i)])


class Sched:
    def __init__(self):
        self.ops = {e: [] for e in ENGS}
        self.ccnt = {e: 0 for e in ENGS}
        self.dcnt = {e: 0 for e in ENGS}
        self.last_w = {}
        self.readers = {}
        self.waited = {e: {} for e in ENGS}
        self.semmax = {}
        self.out_tokens = []

    def _need(self, eng, tok, waits):
        sem, val, teng, is_dma = tok
        if (not is_dma) and teng == eng and eng == 'pe':
            return
        if self.waited[eng].get(sem, 0) >= val:
            return
        self.waited[eng][sem] = val
        waits.append((sem, val))

    def add(self, eng, fn, reads, writes, dma=False, is_out=False):
        waits = []
        if dma:
            i = self.dcnt[eng]
            self.dcnt[eng] += 1
            sem = 'd_%s_%d' % (eng, i % NDSEM)
            val = 16 * (i // NDSEM + 1)
            if i >= NDSEM:
                self._need(eng, (sem, val - 16, eng, True), waits)
            tok = (sem, val, eng, True)
            inc = (sem, 16)
        else:
            self.ccnt[eng] += 1
            sem = 'c_' + eng
            tok = (sem, self.ccnt[eng], eng, False)
            inc = (sem, 1)
        self.semmax[sem] = tok[1]
        for k in reads:
            t = self.last_w.get(k)
            if t is not None:
                self._need(eng, t, waits)
        for k in writes:
            t = self.last_w.get(k)
            if t is not None:
                self._need(eng, t, waits)
            for (rs, (rv, re, rd)) in self.readers.get(k, {}).items():
                self._need(eng, (rs, rv, re, rd), waits)
        for k in writes:
            self.last_w[k] = tok
            self.readers[k] = {}
        for k in reads:
            r = self.readers.setdefault(k, {})
            r[tok[0]] = (tok[1], tok[2], tok[3])
        self.ops[eng].append((fn, waits, inc))
        if is_out:
            self.out_tokens.append(tok)

    def barrier(self, skip_pool_dma=False):
        for e in ENGS:
            waits = []
            for sem, val in self.semmax.items():
                if skip_pool_dma and sem.startswith('d_pool'):
                    continue
                if self.waited[e].get(sem, 0) < val:
                    self.waited[e][sem] = val
                    waits.append((sem, val))
            if waits:
                self.ops[e].append((None, waits, None))
        if not skip_pool_dma:
            self.last_w = {}
            self.readers = {}


class Ctx:
    def __init__(self, nc):
        self.nc = nc
        self.S = Sched()
        self.n_in = {}
        self.stack = None

    def dram_in(self, name, shape, dtype=F32, nsub=0):
        t = self.nc.dram_tensor(name, list(shape), dtype, kind="ExternalInput").ap()
        return Buf(name, t, nsub)

    def dram_out(self, name, shape, dtype=F32, nsub=0):
        t = self.nc.dram_tensor(name, list(shape), dtype, kind="ExternalOutput").ap()
        return Buf(name, t, nsub)

    def dram(self, name, shape, dtype, nsub=0):
        t = self.nc.dram_tensor(name, list(shape), dtype, kind="Internal").ap()
        return Buf(name, t, nsub)

    def sb(self, name, shape, dtype, nsub=0):
        self.uid = getattr(self, 'uid', 0) + 1
        name = 'sb%d_%s' % (self.uid, name)
        t = self.stack.enter_context(self.nc.sbuf_tensor(name, list(shape), dtype))
        return Buf(name, t, nsub)

    def ps(self, name, shape, dtype=F32, nsub=0):
        self.uid = getattr(self, 'uid', 0) + 1
        name = 'ps%d_%s' % (self.uid, name)
        t = self.stack.enter_context(self.nc.psum_tensor(name, list(shape), dtype))
        return Buf(name, t, nsub)

    def _rk(self, *vs):
        ks = []
        for v in vs:
            if isinstance(v, V):
                ks += v.keys
        return ks

    def mm(self, out, lhsT, rhs, start=True, stop=True, **kw):
        o, l, r = out.ap, lhsT.ap, rhs.ap
        rd = self._rk(lhsT, rhs) + ([] if start else out.keys)
        self.S.add('pe', lambda e: e.matmul(o, l, r, start=start, stop=stop, **kw), rd, out.keys)

    def tr(self, out, in_, ident):
        o, i, d = out.ap, in_.ap, ident.ap
        self.S.add('pe', lambda e: e.transpose(o, i, d), self._rk(in_, ident), out.keys)

    def act(self, out, in_, func, bias=0.0, scale=1.0, accum=None):
        o, i = out.ap, in_.ap
        b = bias.ap if isinstance(bias, V) else bias
        s = scale.ap if isinstance(scale, V) else scale
        kw = {}
        wr = list(out.keys)
        if accum is not None:
            kw['accum_out'] = accum.ap
            wr += accum.keys
        self.S.add('act', lambda e: e.activation(o, i, func, bias=b, scale=s, **kw),
                   self._rk(in_, bias, scale), wr)

    def ts(self, eng, out, in0, s1, s2, op0, op1=None, accum=None):
        o, i = out.ap, in0.ap
        a = s1.ap if isinstance(s1, V) else s1
        b = s2.ap if isinstance(s2, V) else s2
        kw = {}
        wr = list(out.keys)
        if accum is not None:
            kw['accum_out'] = accum.ap
            wr += accum.keys
        if op1 is None:
            self.S.add(eng, lambda e: e.tensor_scalar(o, i, a, None, op0, **kw), self._rk(in0, s1), wr)
        else:
            self.S.add(eng, lambda e: e.tensor_scalar(o, i, a, b, op0, op1, **kw), self._rk(in0, s1, s2), wr)

    def tt(self, eng, out, in0, in1, op):
        o, a, b = out.ap, in0.ap, in1.ap
        self.S.add(eng, lambda e: e.tensor_tensor(o, a, b, op), self._rk(in0, in1), out.keys)

    def stt(self, eng, out, in0, scalar, in1, op0, op1):
        o, a, b = out.ap, in0.ap, in1.ap
        s = scalar.ap if isinstance(scalar, V) else scalar
        eng = 'dve'
        self.S.add(eng, lambda e: e.scalar_tensor_tensor(o, a, s, b, op0, op1), self._rk(in0, scalar, in1), out.keys)

    def copy(self, eng, out, in_):
        o, i = out.ap, in_.ap
        if eng == 'act':
            self.S.add(eng, lambda e: e.copy(o, i), in_.keys, out.keys)
        else:
            self.S.add(eng, lambda e: e.tensor_copy(o, i), in_.keys, out.keys)

    def memset(self, eng, out, val):
        o = out.ap
        self.S.add(eng, lambda e: e.memset(o, val), [], out.keys)

    def recip(self, out, in_):
        o, i = out.ap, in_.ap
        self.S.add('dve', lambda e: e.reciprocal(o, i), in_.keys, out.keys)

    def vmax8(self, out, in_):
        o, i = out.ap, in_.ap
        self.S.add('dve', lambda e: e.max(o, i), in_.keys, out.keys)

    def dma(self, q, out, in_, is_out=False):
        o, i = out.ap, in_.ap
        self.S.add(q, lambda e: e.dma_start(out=o, in_=i), in_.keys, out.keys, dma=True, is_out=is_out)

    def emit(self):
        nc, S = self.nc, self.S
        fw = []
        for (sem, val, e, d) in S.out_tokens:
            fw.append((sem, val))
        S.ops['sp'].append((None, fw, None))
        names = sorted(S.semmax.keys())
        with ExitStack() as st:
            sems = {n: st.enter_context(nc.semaphore(n)) for n in names}
            block = st.enter_context(nc.Block())

            def run(engname):
                def body(eng):
                    for (fn, waits, inc) in S.ops[engname]:
                        for (sn, v) in waits:
                            eng.wait_ge(sems[sn], v)
                        if fn is not None:
                            ins = fn(eng)
                            ins.then_inc(sems[inc[0]], inc[1])
                return body

            block.tensor(run('pe'))
            block.scalar(run('act'))
            block.vector(run('dve'))
            block.gpsimd(run('pool'))
            block.sync(run('sp'))


VC = {}
_o = 0
for _n, _w in [('ada_b', 96), ('n1g', 8), ('n2g', 8), ('conv_w', 62), ('conv_b', 2), ('cln_g', 2), ('cln_b', 2),
               ('cq_g', 2), ('ckv_g', 1), ('mqn_g', 1), ('mkn_g', 1), ('dqn_g', 1), ('dkn_g', 1),
               ('nqn_g', 1), ('nkn_g', 1), ('gate_b', 32), ('bg', 512), ('bu', 512), ('sub_g', 1)]:
    VC[_n] = (_o, _w)
    _o += _w
NVC = _o
CC = {}
_o = 0
for _n, _w in [('ones', 128), ('bd32', 128), ('bd64', 128), ('bd96', 128), ('identf', 128)]:
    CC[_n] = (_o, _w)
    _o += _w
NCC = _o
CB = {'ident': (0, 128), 'RM': (128, 128), 'RD': (256, 128)}
NCB = 384

TILES = [(0, 256)] + [(256 + 512 * i, 512) for i in range(8)]


def rope_tables():
    t = np.arange(SEQ)
    rows = (t // 64).astype(np.float32)
    cols = (t % 64).astype(np.float32)
    inv = (10000.0 ** (-np.arange(0, 16, 2, dtype=np.float32) / 16)).astype(np.float32)
    theta = np.concatenate([rows[:, None] * inv, cols[:, None] * inv], axis=-1).astype(np.float32)
    cos = np.cos(theta).astype(np.float32).T
    sin = np.sin(theta).astype(np.float32).T
    c32 = np.ones((32, TOK), np.float32)
    s32 = np.zeros((32, TOK), np.float32)
    c32[:16, CTX:] = cos
    c32[16:, CTX:] = cos
    s32[:16, CTX:] = sin
    s32[16:, CTX:] = sin
    cosM = np.ones((128, TOK), np.float32)
    sinM = np.zeros((128, TOK), np.float32)
    cosM[64:96] = c32
    sinM[64:96] = s32
    cosD = np.tile(c32, (4, 1))
    sinD = np.tile(s32, (4, 1))
    return np.stack([cosM, sinM, cosD, sinD], 0)


def rot_lhsT(n):
    m = np.zeros((128, 128), np.float32)
    for blk in n:
        for i in range(16):
            m[blk + i + 16, blk + i] = -1.0
            m[blk + i, blk + i + 16] = 1.0
    return m


def make_consts():
    c = np.zeros((128, NCC), np.float32)
    c[:, 0:128] = 1.0
    for b in range(4):
        c[b * 32:(b + 1) * 32, 128 + b * 32:128 + (b + 1) * 32] = 1.0
    for b in range(2):
        c[b * 64:(b + 1) * 64, 256 + b * 64:256 + (b + 1) * 64] = 1.0
    c[0:96, 384:384 + 96] = 1.0
    c[:, 512:640] = np.eye(128, dtype=np.float32)
    cb = np.zeros((128, NCB), np.float32)
    cb[:, 0:128] = np.eye(128)
    cb[:, 128:256] = rot_lhsT([64])
    cb[:, 256:384] = rot_lhsT([0, 32, 64, 96])
    return c, cb.astype(ml_dtypes.bfloat16)


def na_slots(i):
    rows = [2 * i, 2 * i + 1]
    need = set()
    for r in rows:
        rs = min(max(r - 4, 0), 56)
        for rr in range(rs, rs + 8):
            need.add(rr // 2)
    js = sorted(need)
    while len(js) < 5:
        js.append(js[0] if i >= 2 else js[-1])
    return js


def na_case(i):
    return {0: 0, 1: 1, 30: 3, 31: 4}.get(i, 2)


def na_tables():
    rep = {0: 0, 1: 1, 2: 5, 3: 30, 4: 31}
    idx_r = np.zeros((5, 128, 5, 128), np.int64)
    idx_c = np.zeros((5, 128, 5, 128), np.int64)
    mask = np.full((5, 128, 5, 128), -1e30, np.float32)
    for cs, i in rep.items():
        js = na_slots(i)
        seen = set()
        for sl, j in enumerate(js):
            dummy = j in seen
            seen.add(j)
            for kk in range(128):
                kr, kc = 2 * j + kk // 64, kk % 64
                for qq in range(128):
                    qr, qc = 2 * i + qq // 64, qq % 64
                    rs = min(max(qr - 4, 0), 56)
                    ws = min(max(qc - 8, 0), 48)
                    ok = (not dummy) and (rs <= kr < rs + 8) and (ws <= kc < ws + 16)
                    if ok:
                        idx_r[cs, kk, sl, qq] = kr - qr + 7
                        idx_c[cs, kk, sl, qq] = kc - qc + 15
                        mask[cs, kk, sl, qq] = 0.0
    return idx_r, idx_c, mask


_NA_CACHE = {}


def na_tables_cached():
    if 'v' not in _NA_CACHE:
        _NA_CACHE['v'] = na_tables_fast()
    return _NA_CACHE['v']


def na_tables_fast():
    rep = {0: 0, 1: 1, 2: 5, 3: 30, 4: 31}
    idx_r = np.zeros((5, 128, 5, 128), np.int64)
    idx_c = np.zeros((5, 128, 5, 128), np.int64)
    mask = np.full((5, 128, 5, 128), -1e30, np.float32)
    kk = np.arange(128)[:, None]
    qq = np.arange(128)[None, :]
    for cs, i in rep.items():
        js = na_slots(i)
        seen = set()
        for sl, j in enumerate(js):
            dummy = j in seen
            seen.add(j)
            if dummy:
                continue
            kr, kc = 2 * j + kk // 64, kk % 64
            qr, qc = 2 * i + qq // 64, qq % 64
            rs = np.clip(qr - 4, 0, 56)
            ws = np.clip(qc - 8, 0, 48)
            ok = (rs <= kr) & (kr < rs + 8) & (ws <= kc) & (kc < ws + 16)
            ir = np.where(ok, kr - qr + 7, 0)
            ic = np.where(ok, kc - qc + 15, 0)
            idx_r[cs, :, sl, :] = ir
            idx_c[cs, :, sl, :] = ic
            mask[cs, :, sl, :] = np.where(ok, 0.0, -1e30)
    return idx_r, idx_c, mask


def build(dbg=False, layers=DEPTH, stop_after=None, lvl=9, ntl=99):
    nc = bass.Bass("TRN2", target_bir_lowering=False)
    K = Ctx(nc)
    S = K.S
    xT = K.dram_in('xT', [D, TOK])
    cT = K.dram_in('cT', [128, 16])
    consts_d = K.dram_in('consts', [128, NCC])
    constb_d = K.dram_in('constb', [128, NCB], BF16)
    rope_d = K.dram_in('rope', [4, 128, TOK])
    namask_d = K.dram_in('namask', [5, 128, 640])
    W = []
    for l in range(layers):
        w = {}
        w['ada_w'] = K.dram_in('ada_w%d' % l, [D, 6 * D])
        w['vecs'] = K.dram_in('vecs%d' % l, [128, NVC])
        w['rows'] = K.dram_in('rows%d' % l, [1, 64 + 32 + 128])
        w['w_in'] = K.dram_in('w_in%d' % l, [D, 2528])
        w['w_uq'] = K.dram_in('w_uq%d' % l, [192, 384])
        w['w_ukv'] = K.dram_in('w_ukv%d' % l, [128, 640])
        w['bouts'] = K.dram_in('bouts%d' % l, [4, 256, D])
        w['gate_w'] = K.dram_in('gate_w%d' % l, [D, 4 * D])
        w['w_o'] = K.dram_in('w_o%d' % l, [D, D])
        w['router_w'] = K.dram_in('router_w%d' % l, [D, NE])
        w['w_gu'] = K.dram_in('w_gu%d' % l, [NE, D, 2 * DFF])
        w['w_dn'] = K.dram_in('w_dn%d' % l, [NE, DFF, D])
        w['b_dn'] = K.dram_in('b_dn%d' % l, [NE, D])
        w['nab'] = K.dram_in('nab%d' % l, [5, 4, 128, 640])
        W.append(w)
    outT = K.dram_out('outT', [D, SEQ])
    hT = K.dram('hT', [D, TOK], BF16)
    uT = K.dram('uT', [256, TOK], F32)
    QT = {'mla': K.dram('QTm', [4, 128, TOK], BF16), 'diff': K.dram('QTd', [2, 128, TOK], BF16),
          'na': K.dram('QTn', [2, 128, TOK], BF16)}
    KT = {'mla': K.dram('KTm', [4, 128, TOK], BF16), 'diff': K.dram('KTd', [2, 128, TOK], BF16),
          'na': K.dram('KTn', [2, 128, TOK], BF16)}
    VA = {'mla': K.dram('VAm', [NKT, 128, 260], BF16), 'diff': K.dram('VAd', [NKT, 128, 260], BF16),
          'na': K.dram('VAn', [NKT, 128, 260], BF16)}
    OT = {n: K.dram('OT' + n, [256, TOK], BF16) for n in ['conv', 'mla', 'diff', 'na']}
    res1 = K.dram('res1', [D, TOK], F32)
    res2 = K.dram('res2', [D, TOK], F32)
    h2T = K.dram('h2T', [D, TOK], BF16)
    gT = K.dram('gT', [NE, TOK], F32)
    dbg_out = {}
    if dbg:
        for n, shp in [('d_mod', [128, 96]), ('d_hT', [D, TOK]), ('d_uT', [256, TOK]), ('d_QTm', [4, 128, TOK]),
                       ('d_KTm', [4, 128, TOK]), ('d_QTd', [2, 128, TOK]), ('d_KTd', [2, 128, TOK]),
                       ('d_QTn', [2, 128, TOK]), ('d_KTn', [2, 128, TOK]), ('d_VAm', [NKT, 128, 260]),
                       ('d_OTconv', [256, TOK]), ('d_OTmla', [256, TOK]), ('d_OTdiff', [256, TOK]),
                       ('d_OTna', [256, TOK]), ('d_res1', [D, TOK]), ('d_h2T', [D, TOK]), ('d_gT', [NE, TOK]),
                       ('d_res2', [D, TOK])]:
            dbg_out[n] = K.dram_out(n, shp, F32 if n in ('d_mod', 'd_uT', 'd_res1', 'd_gT', 'd_res2') else BF16)

    def phase():
        st = ExitStack()
        K.stack = st
        return st

    def end_phase(st):
        S.barrier()
        st.close()

    for l in range(layers):
        w = W[l]
        last = (l == DEPTH - 1)
        lam_init = 0.8 - 0.6 * math.exp(-0.3 * l)
        res_in = xT if l == 0 else res2
        tiles = TILES
        lst = ExitStack()
        K.stack = lst
        vecs = K.sb('vecs%d' % l, [128, NVC], F32)
        cst = K.sb('cst%d' % l, [128, NCC], F32)
        cstb = K.sb('cstb%d' % l, [128, NCB], BF16)
        mod = K.sb('mod%d' % l, [128, 96], F32)
        A1 = K.sb('A1_%d' % l, [128, 16], F32)
        A2 = K.sb('A2_%d' % l, [128, 16], F32)
        lamt = K.sb('lamt%d' % l, [128, 4], F32)
        rowsb = K.sb('rowsb%d' % l, [128, 224], F32)
        vecs_eps2 = K.sb('vecs_eps2_%d' % l, [128, 1], F32)
        K.memset('pool', vecs_eps2[:, :], EPS)
        K.dma('sp', vecs[:, :], w['vecs'][:, :])
        K.dma('sp', cst[:, :], consts_d[:, :])
        K.dma('sp', cstb[:, :], constb_d[:, :])
        K.dma('sp', rowsb[:, :], V(w['rows'].apx[0:1, :].partition_broadcast(128), w['rows'][:, :].keys))

        def vec(name, col=0, rows=128, base=0):
            o, _ = VC[name]
            return vecs[base:base + rows, o + col:o + col + 1]

        def cc(name, r0=0, r1=128, c0=0, c1=128):
            o, _ = CC[name]
            return cst[r0:r1, o + c0:o + c1]

        def cb(name, r0=0, r1=128, c0=0, c1=128):
            o, _ = CB[name]
            return cstb[r0:r1, o + c0:o + c1]

        def modv(j, col):
            return mod[:, 2 * j + col:2 * j + col + 1]

        st = phase()
        cin = K.sb('cin', [128, 16], F32)
        sig = K.sb('sig', [128, 16], F32)
        scb = K.sb('scb', [128, 16], BF16)
        K.dma('sp', cin[:, :], cT[:, :])
        K.act(sig[:, :], cin[:, :], AF.Sigmoid)
        K.tt('dve', scb[:, :], cin[:, :], sig[:, :], ALU.mult)
        pmod = K.ps('pmod', [128, 96], F32)
        for blk in range(4):
            awb = K.sb('awb%d' % blk, [128, 8, 1536], BF16)
            K.dma('pool', awb[:, :, :], V(w['ada_w'].apx[:, blk * 1536:(blk + 1) * 1536].rearrange('(k p) n -> p k n', p=128),
                                         w['ada_w'][:, :].keys))
            for jj in range(12):
                j = blk * 12 + jj
                for k in range(8):
                    K.mm(pmod[:, 2 * j:2 * j + 2], awb[:, k, jj * 128:(jj + 1) * 128], scb[:, 2 * k:2 * k + 2],
                         start=(k == 0), stop=(k == 7), skip_group_check=True)
        o_ab, _ = VC['ada_b']
        K.tt('dve', mod[:, :], pmod[:, :], vecs[:, o_ab:o_ab + 96], ALU.add)
        for (A, gname, j0) in [(A1, 'n1g', 8), (A2, 'n2g', 32)]:
            for k in range(8):
                K.ts('dve', A[:, 2 * k:2 * k + 2], mod[:, 2 * (j0 + k):2 * (j0 + k) + 2], 1.0, vec(gname, k), ALU.add, ALU.mult)
        lt = K.sb('lt', [128, 64], F32)
        ls = K.sb('ls', [128, 4], F32)
        K.tt('dve', lt[:, 0:32], rowsb[:, 96:128], rowsb[:, 128:160], ALU.mult)
        K.tt('dve', lt[:, 32:64], rowsb[:, 160:192], rowsb[:, 192:224], ALU.mult)
        K.S.add('dve', (lambda o, i: (lambda e: e.tensor_reduce(o, i, AX.X, ALU.add)))(ls[:, 0:2].ap, lt[:, :].ap.rearrange('p (a b) -> p a b', a=2)),
                lt[:, :].keys, ls[:, :].keys)
        K.act(ls[:, 2:4], ls[:, 0:2], AF.Exp)
        K.tt('dve', lamt[:, 1:2], ls[:, 3:4], ls[:, 2:3], ALU.subtract)
        K.ts('dve', lamt[:, 0:1], lamt[:, 1:2], -lam_init, None, ALU.add)
        if dbg and l == 0:
            K.dma('sp', dbg_out['d_mod'][:, :], mod[:, :], is_out=True)
        end_phase(st)
        if stop_after == 'A':
            break

        st = phase()
        win = K.sb('win', [128, 8, 2528], BF16)
        K.dma('pool', win[:, :, :], V(w['w_in'].apx.rearrange('(k p) n -> p k n', p=128), w['w_in'][:, :].keys))
        wuq = K.sb('wuq', [128, 2, 384], BF16)
        K.dma('pool', wuq[:, 0, :], w['w_uq'][0:128, :])
        K.dma('pool', wuq[0:64, 1, :], w['w_uq'][128:192, :])
        wukv = K.sb('wukv', [128, 640], BF16)
        K.dma('pool', wukv[:, :], w['w_ukv'][:, :])
        NB = 2
        xt = [K.sb('xt%d' % i, [128, 8, 512], F32) for i in range(NB)]
        ht = [K.sb('ht%d' % i, [128, 8, 512], BF16) for i in range(NB)]
        rp = [K.sb('rp%d' % i, [128, 4, 512], F32) for i in range(NB)]
        rstd = K.sb('rstd', [128, 512], F32)
        tmp = [K.sb('tmp%d' % i, [128, 512], F32) for i in range(4)]
        tb = [K.sb('tb%d' % i, [128, 512], BF16) for i in range(4)]
        cqn = K.sb('cqn', [128, 2, 512], BF16)
        ckvn = K.sb('ckvn', [128, 512], BF16)
        ut = [K.sb('ut%d' % i, [128, 2, 512], F32) for i in range(NB)]
        qo = {n: [K.sb('qo%s%d' % (n, i), [128, c, 512], BF16) for i in range(NB)] for n, c in [('mla', 4), ('diff', 2), ('na', 2)]}
        ko = {n: [K.sb('ko%s%d' % (n, i), [128, c, 512], BF16) for i in range(NB)] for n, c in [('mla', 4), ('diff', 2), ('na', 2)]}
        va = {n: [K.sb('va%s%d' % (n, i), [128, 4, 65], BF16) for i in range(NB)] for n in ['mla', 'diff', 'na']}
        for n in va:
            for i in range(NB):
                K.memset('pool', va[n][i][:, :, :], 1.0)
        pb = [K.ps('pb%d' % i, [128, 512], F32) for i in range(8)]
        pbi = [0]

        def nextp():
            p = pb[pbi[0] % 8]
            pbi[0] += 1
            return p

        tmi = [0]

        def nt():
            t = tmp[tmi[0] % 4]
            tmi[0] += 1
            return t

        tbi = [0]

        def ntb():
            t = tb[tbi[0] % 4]
            tbi[0] += 1
            return t

        def rms_feat(src_list, bdname, nfeat, N):
            pst = nextp()
            for ii, (src, rows) in enumerate(src_list):
                s_ = nt()
                K.act(s_[0:rows, 0:N], src, AF.Square)
                K.mm(pst[:, 0:N], cc(bdname, 0, rows), s_[0:rows, 0:N], start=(ii == 0), stop=(ii == len(src_list) - 1))
            r_ = nt()
            K.act(r_[:, 0:N], pst[:, 0:N], AF.Ln, bias=vecs_eps[:, 0:1], scale=1.0 / nfeat)
            r2 = nt()
            K.act(r2[:, 0:N], r_[:, 0:N], AF.Exp, scale=-0.5)
            return r2

        vecs_eps = K.sb('vecs_eps', [128, 1], F32)
        K.memset('pool', vecs_eps[:, :], EPS)

        def rope_qk(src, rows, bdname, nfeat, gvec, rot, cosv, sinv, dst, N):
            r2 = rms_feat([(src, rows)], bdname, nfeat, N)
            if rot is None:
                K.stt('dve', dst, src, gvec, r2[0:rows, 0:N], ALU.mult, ALU.mult)
                return
            qn = ntb()
            K.stt('dve', qn[0:rows, 0:N], src, gvec, r2[0:rows, 0:N], ALU.mult, ALU.mult)
            pr = nextp()
            K.mm(pr[0:rows, 0:N], rot, qn[0:rows, 0:N])
            t1 = nt()
            K.tt('pool' if POOLP1 else 'dve', t1[0:rows, 0:N], qn[0:rows, 0:N], cosv, ALU.mult)
            t2 = nt()
            K.tt('dve', t2[0:rows, 0:N], pr[0:rows, 0:N], sinv, ALU.mult)
            K.tt('pool' if POOLP1 else 'dve', dst, t1[0:rows, 0:N], t2[0:rows, 0:N], ALU.add)

        cA = [K.sb('cA%d' % i, [128, 512], F32) for i in range(4)]
        cB = [K.sb('cB%d' % i, [128, 512], F32) for i in range(4)]
        cQ = [K.sb('cQ%d' % i, [128, 512], BF16) for i in range(4)]

        def chains(items, N):
            n = len(items)
            srcs = []
            for i, it in enumerate(items):
                srcs.append(it[0](pb[i]))
            for i, it in enumerate(items):
                rows = it[1]
                K.act(cA[i][0:rows, 0:N], srcs[i], AF.Square)
            for i, it in enumerate(items):
                rows = it[1]
                K.mm(pb[4 + i][:, 0:N], cc(it[2], 0, rows), cA[i][0:rows, 0:N])
            for i, it in enumerate(items):
                K.act(cA[i][:, 0:N], pb[4 + i][:, 0:N], AF.Ln, bias=vecs_eps[:, 0:1], scale=1.0 / it[3])
            for i, it in enumerate(items):
                K.act(cA[i][:, 0:N], cA[i][:, 0:N], AF.Exp, scale=-0.5)
            for i, it in enumerate(items):
                rows, gvec, rot, dst = it[1], it[4], it[5], it[8]
                if rot is None:
                    K.stt('dve', dst, srcs[i], gvec, cA[i][0:rows, 0:N], ALU.mult, ALU.mult)
                else:
                    K.stt('dve', cQ[i][0:rows, 0:N], srcs[i], gvec, cA[i][0:rows, 0:N], ALU.mult, ALU.mult)
            for i, it in enumerate(items):
                rows, rot = it[1], it[5]
                if rot is not None:
                    K.mm(pb[4 + i][0:rows, 0:N], rot, cQ[i][0:rows, 0:N])
            for i, it in enumerate(items):
                rows, rot, cosv = it[1], it[5], it[6]
                if rot is not None:
                    K.tt('dve', cA[i][0:rows, 0:N], cQ[i][0:rows, 0:N], cosv, ALU.mult)
            for i, it in enumerate(items):
                rows, rot, sinv = it[1], it[5], it[7]
                if rot is not None:
                    K.tt('dve', cB[i][0:rows, 0:N], pb[4 + i][0:rows, 0:N], sinv, ALU.mult)
            for i, it in enumerate(items):
                rows, rot, dst = it[1], it[5], it[8]
                if rot is not None:
                    K.tt('dve', dst, cA[i][0:rows, 0:N], cB[i][0:rows, 0:N], ALU.add)

        for ti, (c0, N) in enumerate(tiles[:ntl]):
            col = 1 if ti == 0 else 0
            b = ti % NB
            X, H, RP, U = xt[b], ht[b], rp[b], ut[b]
            K.dma('sp', X[:, :, 0:N], V(res_in.apx[:, c0:c0 + N].rearrange('(k p) n -> p k n', p=128), res_in[:, :].keys))
            K.dma('sp', RP[:, :, 0:N], V(rope_d.apx[:, :, c0:c0 + N].rearrange('f p n -> p f n'), rope_d[:, :, :].keys))
            pst = nextp()
            for k in range(8):
                sq_ = nt()
                K.act(sq_[:, 0:N], X[:, k, 0:N], AF.Square)
                K.mm(pst[:, 0:N], cc('ones'), sq_[:, 0:N], start=(k == 0), stop=(k == 7))
            r_ = nt()
            K.act(r_[:, 0:N], pst[:, 0:N], AF.Ln, bias=vecs_eps[:, 0:1], scale=1.0 / D)
            K.act(rstd[:, 0:N], r_[:, 0:N], AF.Exp, scale=-0.5)
            for k in range(8):
                t_ = nt()
                K.stt('dve' if k % 2 == 0 else 'pool', t_[:, 0:N], X[:, k, 0:N], A1[:, 2 * k + col:2 * k + col + 1], rstd[:, 0:N], ALU.mult, ALU.mult)
                K.act(H[:, k, 0:N], t_[:, 0:N], AF.Identity, bias=modv(k, col), scale=1.0)
            K.dma('sp', V(hT.apx[:, c0:c0 + N].rearrange('(k p) n -> p k n', p=128), [('hT', ti)]), H[:, :, 0:N])

            def proj(chunk, rows=128):
                p = nextp()
                for k in range(8):
                    K.mm(p[0:rows, 0:N], win[:, k, chunk * 128:chunk * 128 + rows], H[:, k, 0:N], start=(k == 0), stop=(k == 7))
                return p
            if lvl < 1:
                continue
            for c in range(2):
                pa = proj(c)
                pbb = proj(2 + c)
                sg = nt()
                K.act(sg[:, 0:N], pbb[:, 0:N], AF.Sigmoid)
                K.tt('dve', U[:, c, 0:N], pa[:, 0:N], sg[:, 0:N], ALU.mult)
            K.dma('sp', V(uT.apx[:, c0:c0 + N].rearrange('(k p) n -> p k n', p=128), [('uT', ti)]), U[:, :, 0:N])
            if lvl < 2:
                continue
            pq0 = proj(4)
            pq1 = proj(5, 64)
            r2 = rms_feat([(pq0[:, 0:N], 128), (pq1[0:64, 0:N], 64)], 'ones', 192, N)
            K.stt('dve', cqn[:, 0, 0:N], pq0[:, 0:N], vec('cq_g', 0), r2[:, 0:N], ALU.mult, ALU.mult)
            K.stt('dve', cqn[0:64, 1, 0:N], pq1[0:64, 0:N], vec('cq_g', 1, 64), r2[0:64, 0:N], ALU.mult, ALU.mult)
            pkv = proj(6)
            r2 = rms_feat([(pkv[:, 0:N], 128)], 'ones', 128, N)
            K.stt('dve', ckvn[:, 0:N], pkv[:, 0:N], vec('ckv_g', 0), r2[:, 0:N], ALU.mult, ALU.mult)
            QO, KO = qo['mla'][b], ko['mla'][b]
            def mk_q(h):
                def f(p):
                    K.mm(p[0:96, 0:N], wuq[:, 0, h * 96:(h + 1) * 96], cqn[:, 0, 0:N], start=True, stop=False)
                    K.mm(p[0:96, 0:N], wuq[0:64, 1, h * 96:(h + 1) * 96], cqn[0:64, 1, 0:N], start=False, stop=True)
                    return p[0:96, 0:N]
                return f

            def mk_k(h):
                def f(p):
                    K.mm(p[0:96, 0:N], wukv[:, h * 96:(h + 1) * 96], ckvn[:, 0:N], start=True, stop=False)
                    for k in range(8):
                        K.mm(p[0:96, 0:N], win[:, k, 1920:2016], H[:, k, 0:N], start=False, stop=(k == 7))
                    return p[0:96, 0:N]
                return f
            for hp in range(2):
                items = []
                for h in (2 * hp, 2 * hp + 1):
                    items.append((mk_q(h), 96, 'bd96', 96, vec('mqn_g', 0, 96), cb('RM', 0, 96, 0, 96), RP[0:96, 0, 0:N], RP[0:96, 1, 0:N], QO[0:96, h, 0:N]))
                    items.append((mk_k(h), 96, 'bd96', 96, vec('mkn_g', 0, 96), cb('RM', 0, 96, 0, 96), RP[0:96, 0, 0:N], RP[0:96, 1, 0:N], KO[0:96, h, 0:N]))
                chains(items, N)
            K.dma('sp', V(QT['mla'].apx[:, 0:96, c0:c0 + N].rearrange('h p n -> p h n'), [('QTm', ti)]), QO[0:96, :, 0:N])
            K.dma('sp', V(KT['mla'].apx[:, 0:96, c0:c0 + N].rearrange('h p n -> p h n'), [('KTm', ti)]), KO[0:96, :, 0:N])
            if lvl < 3:
                continue
            for (nm, ch0, bd, nf, gq, gk, rot, ci) in [('diff', 7, 'bd32', 32, 'dqn_g', 'dkn_g', cb('RD'), 2), ('na', 11, 'bd64', 64, 'nqn_g', 'nkn_g', None, None)]:
                if 'p1' not in NOBAR:
                    S.barrier()
                QO, KO = qo[nm][b], ko[nm][b]

                def mk_p(chunk):
                    def f(p):
                        for k in range(8):
                            K.mm(p[:, 0:N], win[:, k, chunk * 128:chunk * 128 + 128], H[:, k, 0:N], start=(k == 0), stop=(k == 7))
                        return p[:, 0:N]
                    return f
                items = []
                for c in range(2):
                    for (dst, cho, g) in [(QO, 0, gq), (KO, 2, gk)]:
                        items.append((mk_p(ch0 + cho + c), 128, bd, nf, vec(g, 0), rot,
                                      None if ci is None else RP[:, ci, 0:N], None if ci is None else RP[:, ci + 1, 0:N], dst[:, c, 0:N]))
                chains(items, N)
                sfx = 'd' if nm == 'diff' else 'n'
                K.dma('sp', V(QT[nm].apx[:, :, c0:c0 + N].rearrange('h p n -> p h n'), [('QT' + sfx, ti)]), QO[:, :, 0:N])
                K.dma('sp', V(KT[nm].apx[:, :, c0:c0 + N].rearrange('h p n -> p h n'), [('KT' + sfx, ti)]), KO[:, :, 0:N])
            if lvl < 4:
                continue
            nsub = N // 128
            for s_ in range(nsub):
                kt = c0 // 128 + s_
                VAm, VAd, VAn = va['mla'][kt % NB], va['diff'][kt % NB], va['na'][kt % NB]
                p = nextp()
                K.mm(p[:, 0:256], ckvn[:, s_ * 128:(s_ + 1) * 128], wukv[:, 384:640])
                for h4 in range(4):
                    K.copy('dve', VAm[:, h4, 0:64], p[:, h4 * 64:(h4 + 1) * 64])
                p = nextp()
                for k in range(8):
                    K.mm(p[:, 0:512], H[:, k, s_ * 128:(s_ + 1) * 128], win[:, k, 2016:2528], start=(k == 0), stop=(k == 7))
                for h4 in range(4):
                    K.copy('dve', VAd[:, h4, 0:64], p[:, h4 * 64:(h4 + 1) * 64])
                    K.copy('dve', VAn[:, h4, 0:64], p[:, 256 + h4 * 64:256 + (h4 + 1) * 64])
                for nm, t_ in [('mla', VAm), ('diff', VAd), ('na', VAn)]:
                    K.dma('sp', V(VA[nm].apx[kt, :, :].rearrange('p (h d) -> p h d', h=4), [('VA' + nm, kt)]), t_[:, :, :])
        if dbg and l == 0:
            S.barrier()
            for (dn, src) in [('d_hT', hT), ('d_uT', uT), ('d_QTm', QT['mla']), ('d_KTm', KT['mla']), ('d_QTd', QT['diff']),
                              ('d_KTd', KT['diff']), ('d_QTn', QT['na']), ('d_KTn', KT['na']), ('d_VAm', VA['mla'])]:
                K.dma('sp', V(dbg_out[dn].apx, [dn]), V(src.apx, [src.name + '_all']), is_out=True)
        end_phase(st)
        if stop_after == 'P1':
            break

        st = phase()
        accs = [K.sb('cacc%d' % c, [128, TOK], F32) for c in range(2)]
        o_cw, _ = VC['conv_w']
        Ucb = [K.sb('Ucb%d' % c, [128, 286], BF16) for c in range(2)]
        Ulb = [K.sb('Ulb%d' % c, [128, 4126], BF16) for c in range(2)]
        Dg = [K.sb('Dg%d' % c, [128, 31, 128], BF16) for c in range(2)]
        cvp = [K.ps('cvp%d' % i, [128, 512], F32) for i in range(4)]
        for c in range(2):
            K.memset('pool', Ucb[c][:, :], 0.0)
            K.memset('pool', Ulb[c][:, :], 0.0)
            K.dma('pool', Ucb[c][:, 15:271], V(uT.apx[c * 128:(c + 1) * 128, 0:256], ['uT_all']))
            K.dma('pool', Ulb[c][:, 15:4111], V(uT.apx[c * 128:(c + 1) * 128, 256:TOK], ['uT_all']))
            for wv in range(31):
                K.ts('dve', Dg[c][:, wv, :], cc('identf'), vecs[:, o_cw + c * 31 + wv:o_cw + c * 31 + wv + 1], None, ALU.mult)
        for ti, (c0, N) in enumerate(tiles):
            for c in range(2):
                src, off = (Ucb[c], 0) if ti == 0 else (Ulb[c], c0 - 256)
                pc = cvp[(ti * 2 + c) % 4]
                for wv in range(31):
                    K.mm(pc[:, 0:N], Dg[c][:, wv, :], src[:, off + wv:off + wv + N], start=(wv == 0), stop=(wv == 30))
                K.act(accs[c][:, c0:c0 + N], pc[:, 0:N], AF.Identity, bias=vec('conv_b', c), scale=1.0)
        ceps = K.sb('ceps', [128, 1], F32)
        K.memset('pool', ceps[:, :], EPS)
        ctmp = [K.sb('ctmp%d' % i, [128, 512], F32) for i in range(6)]
        zt = K.sb('zt', [128, 2, 512], BF16)
        cp1 = K.ps('cp1', [128, 512], F32)
        cp2 = K.ps('cp2', [128, 512], F32)
        for ti, (c0, N) in enumerate(tiles):
            for c in range(2):
                K.mm(cp1[:, 0:N], cc('ones'), accs[c][:, c0:c0 + N], start=(c == 0), stop=(c == 1))
            for c in range(2):
                K.act(ctmp[c][:, 0:N], accs[c][:, c0:c0 + N], AF.Square)
                K.mm(cp2[:, 0:N], cc('ones'), ctmp[c][:, 0:N], start=(c == 0), stop=(c == 1))
            mean, msq, var, sd, rs = ctmp[2], ctmp[3], ctmp[4], ctmp[5], ctmp[0]
            K.act(mean[:, 0:N], cp1[:, 0:N], AF.Copy, scale=1.0 / 256)
            K.tt('dve', msq[:, 0:N], mean[:, 0:N], mean[:, 0:N], ALU.mult)
            K.stt('dve', var[:, 0:N], cp2[:, 0:N], 1.0 / 256, msq[:, 0:N], ALU.mult, ALU.subtract)
            K.act(sd[:, 0:N], var[:, 0:N], AF.Ln, bias=ceps[:, 0:1], scale=1.0)
            K.act(rs[:, 0:N], sd[:, 0:N], AF.Exp, scale=-0.5)
            for c in range(2):
                t1 = ctmp[1]
                K.tt('dve', t1[:, 0:N], accs[c][:, c0:c0 + N], mean[:, 0:N], ALU.subtract)
                K.tt('dve', t1[:, 0:N], t1[:, 0:N], rs[:, 0:N], ALU.mult)
                K.act(zt[:, c, 0:N], t1[:, 0:N], AF.Silu, bias=vec('cln_b', c), scale=vec('cln_g', c))
            K.dma('sp', V(OT['conv'].apx[:, c0:c0 + N].rearrange('(k p) n -> p k n', p=128), [('OTconv', ti)]), zt[:, :, 0:N])
            if 'cv' not in NOBAR:
                S.barrier()
        end_phase(st)

        def attention(kind):
            st = phase()
            nshift = K.sb('nshift', [128, 1], F32)
            NPO = 4 if kind == 'na' else 2
            NPS = 7 - NPO
            NPT = 6
            psS = [K.ps('psS%d' % i, [128, 512], F32) for i in range(NPS)]
            psO = [K.ps('psO%d' % i, [128, 512], F32) for i in range(NPO)]
            psB = K.ps('psB', [128, 512], F32)
            Pt = [K.sb('Pt%d' % i, [128, 1024 if kind == 'na' else 512], BF16) for i in range(2 if kind == 'na' else NPT)]
            rcp = [K.sb('rcp%d' % i, [128, 512], F32) for i in range(2)]
            bcs = [K.sb('bcs%d' % i, [64, 512], F32) for i in range(2)]
            resb = [K.sb('resb%d' % i, [64, 512], BF16) for i in range(2)]
            VAs = K.sb('VAs', [128, NKT, 260], BF16)
            K.dma('sp', VAs[:, :, :], V(VA[kind].apx.rearrange('k p f -> p k f'), ['VA%s_all' % kind]))
            Qs = [K.sb('Qs%d' % i, [128, 4 if kind == 'mla' else 2, 512], BF16) for i in range(2)]
            if kind == 'mla':
                d = 96
                KTs = K.sb('KTs', [128, 4, TOK], BF16)
                K.dma('sp', KTs[:, :, :], V(KT['mla'].apx.rearrange('h p n -> p h n'), ['KTm_all']))
            elif kind == 'na':
                d = 64
                KTs = K.sb('KTs', [128, 4, TOK], BF16)
                K.memset('pool', KTs[:, :, :], 0.0)
                for h_ in range(4):
                    r0_ = 64 * (h_ % 2)
                    K.dma('sp', KTs[r0_:r0_ + 64, h_, :], V(KT['na'].apx[h_ // 2, r0_:r0_ + 64, :], ['KTn_all']))
                Bt = K.sb('Bt', [128, 20, 640], F32)
                Mt = K.sb('Mt', [128, 5, 640], F32)
                K.dma('sp', Bt[:, :, :], V(w['nab'].apx.rearrange('c h p n -> p (c h) n'), ['nab']))
                K.dma('sp', Mt[:, :, :], V(namask_d.apx.rearrange('c p n -> p c n'), ['namask']))
                for cs in range(5):
                    for h in range(4):
                        K.tt('dve', Bt[:, cs * 4 + h, :], Bt[:, cs * 4 + h, :], Mt[:, cs, :], ALU.add)
                sbs = [K.sb('sbs%d' % i, [128, 640], F32) for i in range(2)]
            else:
                d = 32
                KPs = K.sb('KPs', [128, 8, TOK], BF16)
                K.memset('pool', KPs[:, :, :], 0.0)
                for c in range(2):
                    for hh in range(2):
                        for m in range(2):
                            r0 = 64 * hh + 32 * m
                            K.dma('sp', KPs[r0:r0 + 32, (2 * c + hh) * 2 + m, :], V(KT['diff'].apx[c, r0:r0 + 32, :], ['KTd_all']))
                sub_g = K.sb('sub_g', [64, 1], F32)
                K.ts('dve', sub_g[:, :], vec('sub_g', 0, 64), 1.0 - lam_init, None, ALU.mult)
                dt0 = K.sb('dt0', [64, 512], F32)
                dt1 = K.sb('dt1', [64, 512], F32)
                dsq = K.sb('dsq', [64, 512], F32)
            scale = float(d) ** -0.5
            K.memset('pool', nshift[:, :], -math.sqrt(d))
            S.barrier()

            def qk_ops(h, m, Q):
                if kind == 'mla':
                    return (lambda kt: KTs[0:96, h, kt * 128:(kt + 1) * 128]), (lambda a, b_: Q[0:96, h, a:b_])
                c, hh = divmod(h, 2)
                r0 = 64 * hh
                if kind == 'na':
                    return (lambda kt: KTs[:, h, kt * 128:(kt + 1) * 128]), (lambda a, b_: Q[:, c, a:b_])
                return (lambda kt: KPs[:, h * 2 + m, kt * 128:(kt + 1) * 128]), (lambda a, b_: Q[:, c, a:b_])

            def dense_map(h, m, Q, N, kts, Ops):
                kf, qf = qk_ops(h, m, Q)
                LA = min(NPS - 2, len(kts) - 1)
                for a_ in range(LA + 1):
                    K.mm(psS[(sct[0] + a_) % NPS][:, 0:N], kf(kts[a_]), qf(0, N))
                for ii, kt in enumerate(kts):
                    ps = psS[(sct[0] + ii) % NPS]
                    if ii + LA + 1 < len(kts):
                        K.mm(psS[(sct[0] + ii + LA + 1) % NPS][:, 0:N], kf(kts[ii + LA + 1]), qf(0, N))
                    P = Pt[(pct[0] + ii) % len(Pt)]
                    K.act(P[:, 0:N], ps[:, 0:N], AF.Exp, bias=nshift[:, 0:1], scale=scale)
                    K.mm(Ops[0:65, 0:N], VAs[:, kt, h * 65:(h + 1) * 65], P[:, 0:N], start=(ii == 0), stop=(ii == len(kts) - 1))
                    if ii == min(2, len(kts) - 1) and apend:
                        apend.pop(0)()
                    if ii == min(8, len(kts) - 1) and apend:
                        apend.pop(0)()
                sct[0] += len(kts)
                pct[0] += len(kts)

            bci = [0]
            apend = []
            sct = [0]
            pct = [0]

            def bcast_recip(Ops, N, mult=None):
                i = bci[0] % 2
                bci[0] += 1
                r = rcp[i]
                K.recip(r[64:65, 0:N], Ops[64:65, 0:N])
                if mult is not None:
                    K.ts('dve', r[64:65, 0:N], r[64:65, 0:N], mult, None, ALU.mult)
                K.mm(psB[0:64, 0:N], cc('ones', 64, 65, 0, 64), r[64:65, 0:N])
                K.copy('act', bcs[i][:, 0:N], psB[0:64, 0:N])
                return bcs[i]

            ri = [0]

            def store_head(h, c0, N, resv, bi):
                K.dma('sp', V(OT[kind].apx[h * 64:(h + 1) * 64, c0:c0 + N], [('OT' + kind, bi, h)]), resv)

            blocks = ([(0, 256, [0, 1])] if not last else []) + [(256 + 512 * i, 512, list(range(NKT))) for i in range(8)]
            for bi, (c0, N, kts) in enumerate(blocks):
                Q = Qs[bi % 2]
                K.dma('sp', Q[:, :, 0:N], V(QT[kind].apx[:, :, c0:c0 + N].rearrange('h p n -> p h n'), ['QT%s_all' % kind]))
                if kind == 'na' and N == 512:
                    while apend:
                        apend.pop(0)()
                    for sb_ in range(4):
                        i = (c0 - 256) // 128 + sb_
                        js = na_slots(i)
                        cs = na_case(i)
                        qa, qb = sb_ * 128, (sb_ + 1) * 128
                        for h in range(4):
                            kf, qf = qk_ops(h, 0, Q)
                            P = Pt[h % 2]
                            SB = sbs[h % 2]
                            for ii, kt in enumerate([0, 1]):
                                K.mm(psS[0][:, ii * 128:(ii + 1) * 128], kf(kt), qf(qa, qb))
                            for ii, j in enumerate(js):
                                pp = psS[1] if ii < 4 else psS[2]
                                K.mm(pp[:, (ii % 4) * 128:(ii % 4 + 1) * 128], kf(2 + j), qf(qa, qb))
                            K.stt('dve', SB[:, 0:512], psS[1][:, 0:512], scale, Bt[:, cs * 4 + h, 0:512], ALU.mult, ALU.add)
                            K.stt('dve', SB[:, 512:640], psS[2][:, 0:128], scale, Bt[:, cs * 4 + h, 512:640], ALU.mult, ALU.add)
                            K.act(P[:, 0:256], psS[0][:, 0:256], AF.Exp, bias=nshift[:, 0:1], scale=scale)
                            K.act(P[:, 256:896], SB[:, 0:640], AF.Exp, bias=nshift[:, 0:1], scale=1.0)
                            ktl = [0, 1] + [2 + j for j in js]
                            for ii, kt in enumerate(ktl):
                                K.mm(psO[h][0:65, qa:qb], VAs[:, kt, h * 65:(h + 1) * 65], P[:, ii * 128:(ii + 1) * 128],
                                     start=(ii == 0), stop=(ii == 6), skip_group_check=True)
                    for h in range(4):
                        bc = bcast_recip(psO[h], N)
                        res = resb[h % 2]
                        K.tt('dve', res[:, 0:N], psO[h][0:64, 0:N], bc[:, 0:N], ALU.mult)
                        store_head(h, c0, N, res[:, 0:N], bi)
                    continue
                for h in range(4):
                    if kind == 'diff':
                        O0, O1 = psO[0], psO[1]
                        dense_map(h, 0, Q, N, kts, O0)
                        dense_map(h, 1, Q, N, kts, O1)

                        def fin(h=h, O0=O0, O1=O1, N=N, c0=c0, bi=bi):
                            res = resb[h % 2]
                            bc0 = bcast_recip(O0, N)
                            bc1 = bcast_recip(O1, N, mult=lamt[64:65, 0:1])
                            K.tt('dve', dt0[:, 0:N], O0[0:64, 0:N], bc0[:, 0:N], ALU.mult)
                            K.tt('dve', dt1[:, 0:N], O1[0:64, 0:N], bc1[:, 0:N], ALU.mult)
                            K.tt('dve', dt0[:, 0:N], dt0[:, 0:N], dt1[:, 0:N], ALU.add)
                            K.act(dsq[:, 0:N], dt0[:, 0:N], AF.Square)
                            K.mm(psB[0:64, 0:N], cc('ones', 0, 64, 0, 64), dsq[:, 0:N])
                            K.act(dt1[:, 0:N], psB[0:64, 0:N], AF.Ln, bias=vecs_eps2[0:64, 0:1], scale=1.0 / 64)
                            K.act(dt1[:, 0:N], dt1[:, 0:N], AF.Exp, scale=-0.5)
                            K.stt('dve', res[:, 0:N], dt0[:, 0:N], sub_g[:, 0:1], dt1[:, 0:N], ALU.mult, ALU.mult)
                            store_head(h, c0, N, res[:, 0:N], bi)
                    else:
                        Ops = psO[h % 2]
                        dense_map(h, 0, Q, N, kts, Ops)

                        hst = {}

                        def fin1(h=h, Ops=Ops, N=N, hst=hst):
                            i_ = bci[0] % 2
                            bci[0] += 1
                            hst['i'] = i_
                            K.recip(rcp[i_][64:65, 0:N], Ops[64:65, 0:N])

                        def fin(h=h, Ops=Ops, N=N, c0=c0, bi=bi, hst=hst):
                            i_ = hst['i']
                            res = resb[h % 2]
                            K.mm(psB[0:64, 0:N], cc('ones', 64, 65, 0, 64), rcp[i_][64:65, 0:N])
                            K.copy('act', bcs[i_][:, 0:N], psB[0:64, 0:N])
                            K.tt('dve', res[:, 0:N], Ops[0:64, 0:N], bcs[i_][:, 0:N], ALU.mult)
                            store_head(h, c0, N, res[:, 0:N], bi)
                    if kind == 'diff':
                        fin()
                    else:
                        apend.append(fin1)
                        apend.append(fin)
            while apend:
                apend.pop(0)()
            end_phase(st)

        for kind in ['mla', 'diff', 'na']:
            if stop_after == 'C':
                break
            attention(kind)
        if dbg and l == 0:
            S.barrier()
            for (dn, src) in [('d_OTconv', OT['conv']), ('d_OTmla', OT['mla']), ('d_OTdiff', OT['diff']), ('d_OTna', OT['na'])]:
                K.dma('sp', V(dbg_out[dn].apx, [dn]), V(src.apx, [src.name + '_all']), is_out=True)
            S.barrier()
        if stop_after in ('C', 'ATT'):
            break

        st = phase()
        bo = K.sb('bo', [128, 8, 1024], BF16)
        K.dma('pool', bo[:, :, :], V(w['bouts'].apx.rearrange('b (k p) n -> p (b k) n', p=128), ['bouts']))
        gwm = [K.sb('gwm%d' % m, [128, 8, 512], BF16) for m in range(8)]
        for m in range(8):
            K.dma('pool', gwm[m][:, :, :], V(w['gate_w'].apx[:, m * 512:(m + 1) * 512].rearrange('(k p) n -> p k n', p=128), ['gate_w']))
        wo = K.sb('wo', [128, 8, 1024], BF16)
        K.dma('pool', wo[:, :, :], V(w['w_o'].apx.rearrange('(k p) n -> p k n', p=128), ['w_o']))
        rw = K.sb('rw', [128, 8, 32], F32)
        K.dma('sp', rw[:, :, :], V(w['router_w'].apx.rearrange('(k p) n -> p k n', p=128), ['router_w']))
        Hm = K.sb('Hm', [128, 8, 512], BF16)
        B4 = K.sb('B4', [128, 8, 512], BF16)
        Xm = K.sb('Xm', [128, 8, 512], F32)
        X1 = K.sb('X1', [128, 8, 512], F32)
        ym = K.sb('ym', [128, 8, 512], BF16)
        H2f = K.sb('H2f', [128, 8, 512], F32)
        H2b = K.sb('H2b', [128, 8, 512], BF16)
        mt = [K.sb('mt%d' % i, [128, 512], F32) for i in range(6)]
        macc = K.sb('macc', [128, 512], F32)
        rstd2_t = K.sb('rstd2_t', [128, 512], F32)
        mpend = []
        meps = K.sb('meps', [128, 1], F32)
        K.memset('pool', meps[:, :], EPS)
        gTt = K.sb('gTt', [32, 512], F32)
        rlg = [K.sb('rlg%d' % i, [128, 32], F32) for i in range(4)]
        rmsk = [K.sb('rmsk%d' % i, [128, 32], F32) for i in range(4)]
        rex = [K.sb('rex%d' % i, [128, 32], F32) for i in range(4)]
        rgt = [K.sb('rgt%d' % i, [128, 32], F32) for i in range(4)]
        rr8 = [K.sb('rr8%d' % i, [128, 8], F32) for i in range(4)]
        rrs = [K.sb('rrs%d' % i, [128, 4], F32) for i in range(4)]
        mp = [K.ps('mp%d' % i, [128, 512], F32) for i in range(8)]
        mpi = [0]

        def mnext():
            p = mp[mpi[0] % 8]
            mpi[0] += 1
            return p
        mti = [0]

        def mnt():
            t = mt[mti[0] % 6]
            mti[0] += 1
            return t
        S.barrier(skip_pool_dma=True)
        for ti, (c0, N) in enumerate(tiles):
            if last and ti == 0:
                continue
            col = 1 if ti == 0 else 0
            K.dma('sp', Hm[:, :, 0:N], V(hT.apx[:, c0:c0 + N].rearrange('(k p) n -> p k n', p=128), ['hT_all']))
            for bi_, nm in enumerate(['conv', 'mla', 'diff', 'na']):
                K.dma('sp', B4[:, bi_ * 2:bi_ * 2 + 2, 0:N], V(OT[nm].apx[:, c0:c0 + N].rearrange('(k p) n -> p k n', p=128), ['OT_all' + nm]))
            K.dma('sp', Xm[:, :, 0:N], V(res_in.apx[:, c0:c0 + N].rearrange('(k p) n -> p k n', p=128), res_in[:, :].keys))
            for m in range(8):
                if m in (1, 3, 5) and mpend:
                    mpend.pop(0)()
                tms = []
                for b_ in range(4):
                    pg = mnext()
                    for k in range(8):
                        K.mm(pg[:, 0:N], gwm[m][:, k, b_ * 128:(b_ + 1) * 128], Hm[:, k, 0:N], start=(k == 0), stop=(k == 7))
                    sg = mnt()
                    K.act(sg[:, 0:N], pg[:, 0:N], AF.Sigmoid, bias=vec('gate_b', b_ * 8 + m), scale=1.0)
                    py = mnext()
                    for k in range(2):
                        K.mm(py[:, 0:N], bo[:, b_ * 2 + k, m * 128:(m + 1) * 128], B4[:, b_ * 2 + k, 0:N], start=(k == 0), stop=(k == 1))
                    K.tt('dve', sg[:, 0:N], sg[:, 0:N], py[:, 0:N], ALU.mult)
                    tms.append(sg)
                K.tt('dve', tms[0][:, 0:N], tms[0][:, 0:N], tms[1][:, 0:N], ALU.add)
                K.tt('dve', tms[2][:, 0:N], tms[2][:, 0:N], tms[3][:, 0:N], ALU.add)
                K.tt('dve', ym[:, m, 0:N], tms[0][:, 0:N], tms[2][:, 0:N], ALU.add)
            if 'mg' not in NOBAR:
                S.barrier()
            for m2 in range(8):
                po = mnext()
                for m in range(8):
                    K.mm(po[:, 0:N], wo[:, m, m2 * 128:(m2 + 1) * 128], ym[:, m, 0:N], start=(m == 0), stop=(m == 7))
                K.stt('dve', X1[:, m2, 0:N], po[:, 0:N], modv(16 + m2, col), Xm[:, m2, 0:N], ALU.mult, ALU.add)
            K.dma('sp', V(res1.apx[:, c0:c0 + N].rearrange('(k p) n -> p k n', p=128), [('res1', ti)]), X1[:, :, 0:N])
            def postA(ti=ti, c0=c0, N=N, col=col):
                pst = mnext()
                for k in range(8):
                    sq_ = mnt()
                    K.act(sq_[:, 0:N], X1[:, k, 0:N], AF.Square)
                    K.mm(pst[:, 0:N], cc('ones'), sq_[:, 0:N], start=(k == 0), stop=(k == 7))
                r_ = mnt()
                K.act(r_[:, 0:N], pst[:, 0:N], AF.Ln, bias=meps[:, 0:1], scale=1.0 / D)
                rstd2 = rstd2_t
                K.act(rstd2[:, 0:N], r_[:, 0:N], AF.Exp, scale=-0.5)
                for k in range(8):
                    t_ = mnt()
                    K.stt('dve', t_[:, 0:N], X1[:, k, 0:N], A2[:, 2 * k + col:2 * k + col + 1], rstd2[:, 0:N], ALU.mult, ALU.mult)
                    K.act(H2f[:, k, 0:N], t_[:, 0:N], AF.Identity, bias=modv(24 + k, col), scale=1.0)
                    K.copy('dve', H2b[:, k, 0:N], H2f[:, k, 0:N])
                K.dma('sp', V(h2T.apx[:, c0:c0 + N].rearrange('(k p) n -> p k n', p=128), [('h2T', ti)]), H2b[:, :, 0:N])

            def postB(ti=ti, c0=c0, N=N, col=col):
                nsb = N // 128
                prs = []
                for sb_ in range(nsb):
                    pr = mnext()
                    for k in range(8):
                        K.mm(pr[:, 0:32], H2f[:, k, sb_ * 128:(sb_ + 1) * 128], rw[:, k, :], start=(k == 0), stop=(k == 7))
                    prs.append(pr)
                for sb_ in range(nsb):
                    K.tt('dve', rlg[sb_][:, :], prs[sb_][:, 0:32], rowsb[:, 64:96], ALU.add)
                for sb_ in range(nsb):
                    K.vmax8(rr8[sb_][:, :], rlg[sb_][:, :])
                for sb_ in range(nsb):
                    K.ts('dve', rmsk[sb_][:, :], rlg[sb_][:, :], rr8[sb_][:, 3:4], None, ALU.is_ge)
                    K.ts('dve', rrs[sb_][:, 0:1], rr8[sb_][:, 0:1], -1.0, None, ALU.mult)
                for sb_ in range(nsb):
                    K.act(rex[sb_][:, :], rlg[sb_][:, :], AF.Exp, bias=rrs[sb_][:, 0:1], scale=1.0)
                for sb_ in range(nsb):
                    K.tt('dve', rex[sb_][:, :], rex[sb_][:, :], rmsk[sb_][:, :], ALU.mult)
                for sb_ in range(nsb):
                    K.S.add('dve', (lambda o, i: (lambda e: e.tensor_reduce(o, i, AX.X, ALU.add)))(rrs[sb_][:, 1:2].ap, rex[sb_][:, :].ap), rex[sb_][:, :].keys, rrs[sb_][:, :].keys)
                for sb_ in range(nsb):
                    K.recip(rrs[sb_][:, 2:3], rrs[sb_][:, 1:2])
                for sb_ in range(nsb):
                    K.ts('dve', rgt[sb_][:, :], rex[sb_][:, :], rrs[sb_][:, 2:3], None, ALU.mult)

            def postC(ti=ti, c0=c0, N=N, col=col):
                nsb = N // 128
                pts = []
                for sb_ in range(nsb):
                    pt_ = mnext()
                    K.tr(pt_[0:32, 0:128], rgt[sb_][:, :], cc('identf'))
                    pts.append(pt_)
                for sb_ in range(nsb):
                    K.copy('dve', gTt[:, sb_ * 128:(sb_ + 1) * 128], pts[sb_][0:32, 0:128])
                K.dma('sp', V(gT.apx[:, c0:c0 + N], [('gT', ti)]), gTt[:, 0:N])

            mpend.extend([postA, postB, postC])
            if 'mg' not in NOBAR:
                S.barrier()
        while mpend:
            mpend.pop(0)()
        S.barrier()
        if dbg and l == 0:
            for (dn, src) in [('d_res1', res1), ('d_h2T', h2T), ('d_gT', gT)]:
                K.dma('sp', V(dbg_out[dn].apx, [dn]), V(src.apx, [src.name + '_all']), is_out=True)
        end_phase(st)
        if stop_after == 'M':
            break

        st = phase()
        bd = K.sb('bd', [32, 1024], F32)
        K.dma('sp', bd[:, :], w['b_dn'][:, :])
        o_bg, _ = VC['bg']
        bup = K.sb('bup', [128, 512], F32)
        K.ts('dve', bup[:, :], vecs[:, o_bg:o_bg + 512], 1.0, None, ALU.add)
        EB = 1088
        H2 = K.sb('H2', [128, 8, EB], BF16)
        gts = K.sb('gts', [32, EB], F32)
        acc = K.sb('eacc', [128, 8, EB], F32)
        Wg = [K.sb('Wg%d' % i, [128, 8, 2048], BF16) for i in range(2)]
        Wd = [K.sb('Wd%d' % i, [128, 8, 1024], BF16) for i in range(2)]
        G = [K.sb('G%d' % i, [128, EB], F32) for i in range(2)]
        At = [K.sb('At%d' % i, [128, 8, 512], BF16) for i in range(2)]
        et = [K.sb('et%d' % i, [128, 512], F32) for i in range(6)]
        ep = [K.ps('ep%d' % i, [128, 512], F32) for i in range(8)]
        epi = [0]

        def enext():
            p = ep[epi[0] % 8]
            epi[0] += 1
            return p
        eti = [0]

        def ent():
            t = et[eti[0] % 6]
            eti[0] += 1
            return t
        S.barrier()
        eblocks = ([(256 + 1024 * i, 1024) for i in range(4)] if last else [(1088 * i, 1088) for i in range(4)])
        ai = 0
        for (c0, Nb) in eblocks:
            btiles = ([(0, 512), (512, 512)] if Nb == 1024 else ([(0, 256), (256, 416), (672, 416)] if c0 == 0 else [(0, 364), (364, 362), (726, 362)]))
            K.dma('sp', H2[:, :, 0:Nb], V(h2T.apx[:, c0:c0 + Nb].rearrange('(k p) n -> p k n', p=128), ['h2T_all']))
            K.dma('sp', gts[:, 0:Nb], V(gT.apx[:, c0:c0 + Nb], ['gT_all']))
            for m in range(8):
                for (t0, N) in btiles:
                    p = enext()
                    K.mm(p[:, 0:N], bd[0:32, m * 128:(m + 1) * 128], gts[0:32, t0:t0 + N])
                    K.copy('dve', acc[:, m, t0:t0 + N], p[:, 0:N])
            def load_w(e):
                K.dma('pool', Wg[e % 2][:, :, :], V(w['w_gu'].apx[e].rearrange('(k p) n -> p k n', p=128), ['w_gu']))
                K.dma('pool', Wd[e % 2][:, :, :], V(w['w_dn'].apx[e].rearrange('(k p) n -> p k n', p=128), ['w_dn']))
            load_w(0)
            pend = []
            for e in range(NE):
                WG, WD, GE = Wg[e % 2], Wd[e % 2], G[e % 2]
                K.dma('sp', GE[:, 0:Nb], V(gT.apx[e:e + 1, c0:c0 + Nb].partition_broadcast(128), ['gT_all']))
                K.act(GE[:, 0:Nb], GE[:, 0:Nb], AF.Copy, scale=1.0 / 1.702)
                for (t0, N) in btiles:
                    A = At[ai % 2]
                    ai += 1
                    for j in range(8):
                        pg = enext()
                        for k in range(8):
                            K.mm(pg[:, 0:N], WG[:, k, j * 128:(j + 1) * 128], H2[:, k, t0:t0 + N], start=(k == 0), stop=(k == 7))
                        pu = enext()
                        for k in range(8):
                            K.mm(pu[:, 0:N], WG[:, k, 1024 + j * 128:1024 + (j + 1) * 128], H2[:, k, t0:t0 + N], start=(k == 0), stop=(k == 7))
                        tg, sl, tu = ent(), ent(), ent()
                        tu2, p1 = tu, sl
                        K.ts('dve', tg[:, 0:N], pg[:, 0:N], vecs[:, o_bg + e * 16 + j:o_bg + e * 16 + j + 1], 7.0, ALU.add, ALU.min)
                        K.act(sl[:, 0:N], tg[:, 0:N], AF.Silu, scale=1.702)
                        K.act(tu[:, 0:N], pu[:, 0:N], AF.Identity, bias=bup[:, e * 16 + 8 + j:e * 16 + 8 + j + 1], scale=1.0)
                        K.ts('dve', tu2[:, 0:N], tu[:, 0:N], 8.0, -6.0, ALU.min, ALU.max)
                        K.tt('dve', p1[:, 0:N], sl[:, 0:N], tu2[:, 0:N], ALU.mult)
                        K.tt('dve', A[:, j, 0:N], p1[:, 0:N], GE[:, t0:t0 + N], ALU.mult)
                        if j == 1:
                            if pend:
                                pend.pop(0)()
                            if t0 == 0 and e + 1 < NE:
                                load_w(e + 1)
                    def down(A=A, WD=WD, t0=t0, N=N):
                        for m in range(8):
                            py = enext()
                            for j in range(8):
                                K.mm(py[:, 0:N], WD[:, j, m * 128:(m + 1) * 128], A[:, j, 0:N], start=(j == 0), stop=(j == 7))
                            K.tt('dve', acc[:, m, t0:t0 + N], acc[:, m, t0:t0 + N], py[:, 0:N], ALU.add)
                    pend.append(down)
                if e % 4 == 3 or e == NE - 1:
                    while pend:
                        pend.pop(0)()
                if e % 4 == 3:
                    S.barrier(skip_pool_dma=True)
            for m in range(8):
                for (t0, N) in btiles:
                    col = 1 if (c0 + t0) < 256 else 0
                    xo = ent()
                    K.dma('sp', xo[:, 0:N], V(res1.apx[m * 128:(m + 1) * 128, c0 + t0:c0 + t0 + N], ['res1_all']))
                    K.stt('dve', xo[:, 0:N], acc[:, m, t0:t0 + N], modv(40 + m, col), xo[:, 0:N], ALU.mult, ALU.add)
                    if last:
                        K.dma('sp', V(outT.apx[m * 128:(m + 1) * 128, c0 + t0 - 256:c0 + t0 - 256 + N], [('outT', m, c0, t0)]), xo[:, 0:N], is_out=True)
                    else:
                        K.dma('sp', V(res2.apx[m * 128:(m + 1) * 128, c0 + t0:c0 + t0 + N], [('res2', m, c0, t0)]), xo[:, 0:N])
            S.barrier()
        if dbg and l == 0:
            K.dma('sp', V(dbg_out['d_res2'].apx, ['d_res2']), V(res2.apx, ['res2_all']), is_out=True)
        end_phase(st)
        S.barrier()
        lst.close()
    K.emit()
    return nc


OFF_B, OFF_C, OFF_D = 512, 864, 1632


def _pk(v, n):
    return np.ascontiguousarray(np.asarray(v, np.float32).reshape(n, 128).T)


def prep_shared(inp, layers=DEPTH):
    sh = {}
    c, cb = make_consts()
    sh['consts'] = c
    sh['constb'] = cb
    sh['rope'] = rope_tables()
    idx_r, idx_c, mask = na_tables_cached()
    sh['namask'] = np.ascontiguousarray(mask.reshape(5, 128, 640))
    for l in range(layers):
        sh['ada_w%d' % l] = np.ascontiguousarray(inp['ada_w'][l])
        vec = np.zeros((128, NVC), np.float32)

        def put(name, arr):
            o, wd = VC[name]
            arr = np.asarray(arr, np.float32)
            vec[:arr.shape[0], o:o + arr.shape[1]] = arr
        ab = _pk(inp['ada_b'][l], 48)
        put('ada_b', np.repeat(ab, 2, axis=1))
        put('n1g', _pk(inp['norm1_g'][l], 8))
        put('n2g', _pk(inp['norm2_g'][l], 8))
        cw = inp['conv_w'][l]
        put('conv_w', np.concatenate([cw[:, 0:128].T, cw[:, 128:256].T], axis=1))
        put('conv_b', _pk(inp['conv_b'][l], 2))
        put('cln_g', _pk(inp['conv_ln_g'][l], 2))
        put('cln_b', _pk(inp['conv_ln_b'][l], 2))
        cq = np.zeros((128, 2), np.float32)
        cq[:, 0] = inp['mla_cq_g'][l][0:128]
        cq[0:64, 1] = inp['mla_cq_g'][l][128:192]
        put('cq_g', cq)
        put('ckv_g', inp['mla_ckv_g'][l][:, None])
        put('mqn_g', inp['mla_qn_g'][l][:, None])
        put('mkn_g', inp['mla_kn_g'][l][:, None])
        put('dqn_g', np.tile(inp['diff_qn_g'][l], 4)[:, None])
        put('dkn_g', np.tile(inp['diff_kn_g'][l], 4)[:, None])
        put('nqn_g', np.tile(inp['na_qn_g'][l], 2)[:, None])
        put('nkn_g', np.tile(inp['na_kn_g'][l], 2)[:, None])
        put('gate_b', _pk(inp['gate_b'][l], 32))
        put('sub_g', inp['diff_subln_g'][l][:, None])
        bgu = inp['exp_b_gu'][l]
        put('bg', np.ascontiguousarray(bgu.reshape(32, 16, 128).transpose(2, 0, 1).reshape(128, 512)))
        sh['vecs%d' % l] = vec
        sh['rows%d' % l] = np.concatenate([inp['diff_subln_g'][l], inp['router_b'][l], inp['diff_lam'][l].reshape(-1)])[None, :].astype(np.float32)
        wi = inp['w_in'][l]
        z64 = np.zeros((D, 64), np.float32)
        cols = [wi[:, 0:512], wi[:, 512:640], wi[:, 640:704], z64, wi[:, 704:832]]
        for off in (0, 64):
            for base in (OFF_C, OFF_D):
                for hp in range(2):
                    cols.append(np.concatenate([wi[:, base + h * 192 + off: base + h * 192 + off + 64] for h in (2 * hp, 2 * hp + 1)], axis=1))
        c5 = cols[5:]
        cols = cols[:5] + [c5[0], c5[1], c5[4], c5[5], c5[2], c5[3], c5[6], c5[7]]
        cols += [z64, wi[:, 832:864]]
        for base in (OFF_C, OFF_D):
            cols.append(np.concatenate([wi[:, base + h * 192 + 128: base + h * 192 + 192] for h in range(4)], axis=1))
        wr = np.ascontiguousarray(np.concatenate(cols, axis=1))
        assert wr.shape == (D, 2528), wr.shape
        sh['w_in%d' % l] = wr
        sh['w_uq%d' % l] = np.ascontiguousarray(inp['mla_w_uq'][l])
        wk = inp['mla_w_ukv'][l]
        z32 = np.zeros((128, 32), np.float32)
        kc = []
        for h in range(4):
            kc += [wk[:, h * 128:h * 128 + 64], z32]
        vc = [wk[:, h * 128 + 64:h * 128 + 128] for h in range(4)]
        sh['w_ukv%d' % l] = np.ascontiguousarray(np.concatenate(kc + vc, axis=1))
        sh['bouts%d' % l] = np.ascontiguousarray(np.stack([inp['conv_out'][l], inp['mla_out'][l], inp['diff_out'][l], inp['na_out'][l]], 0))
        sh['gate_w%d' % l] = np.ascontiguousarray(inp['gate_w'][l].reshape(D, 4, 8, 128).transpose(0, 2, 1, 3).reshape(D, 4 * D))
        sh['w_o%d' % l] = np.ascontiguousarray(inp['w_o'][l])
        sh['router_w%d' % l] = np.ascontiguousarray(inp['router_w'][l])
        sh['w_gu%d' % l] = np.ascontiguousarray(inp['exp_w_gu'][l])
        sh['w_dn%d' % l] = np.ascontiguousarray(inp['exp_w_down'][l])
        sh['b_dn%d' % l] = np.ascontiguousarray(inp['exp_b_down'][l])
        rpb = inp['na_rpb'][l]
        g = rpb[:, idx_r, idx_c]
        sh['nab%d' % l] = np.ascontiguousarray(g.transpose(1, 0, 2, 3, 4).reshape(5, 4, 128, 640)).astype(np.float32)
    return sh


def prep_core(inp, b):
    m = {}
    m['xT'] = np.ascontiguousarray(np.concatenate([inp['ctx'][b].T, inp['x'][b].T], axis=1)).astype(np.float32)
    cT = np.zeros((128, 16), np.float32)
    cT[:, 0::2] = _pk(inp['c'][b], 8)
    cT[:, 1::2] = _pk(inp['c_ctx'], 8)
    m['cT'] = cT
    return m


_NC_CACHE = {}


def kernel(**inputs):
    inp = {k: np.asarray(v) for k, v in inputs.items()}
    if 'nc' not in _NC_CACHE:
        _NC_CACHE['nc'] = build()
    nc = _NC_CACHE['nc']
    sh = prep_shared(inp)
    in_maps = []
    for b in range(8):
        m = dict(sh)
        m.update(prep_core(inp, b))
        in_maps.append(m)
    res = run_bass_kernel_spmd(nc, in_maps, core_ids=list(range(8)))
    out = np.stack([np.ascontiguousarray(res.results[b]['outT'].T) for b in range(8)], 0)
    return out.astype(np.float32)
```
